# Optimizing a Trainium2 kernel written in Bass

```python
import math
import jax
import jax.numpy as jnp
from jax import lax
import numpy as np

D_MODEL = 1024
BATCH = 4
SEQ = 4096
DEPTH = 2

CHUNK = 64

N_A_LAYERS = DEPTH // 2
N_B_LAYERS = DEPTH - N_A_LAYERS
N_DENSE_LAYERS = (DEPTH + 1) // 2
N_MOE_LAYERS = DEPTH // 2

A_HEAD_DIM = 128
A_HEADS = D_MODEL // A_HEAD_DIM
A_WIDTH = A_HEADS * A_HEAD_DIM
CONV_K = 4
A_IN_COLS = 4 * A_WIDTH + 2 * A_HEADS

B_HEAD_DIM = 64
B_HEADS = D_MODEL // B_HEAD_DIM
B_WIDTH = B_HEADS * B_HEAD_DIM
LEFT_CHUNKS = 8
BAND = (LEFT_CHUNKS + 1) * CHUNK
REL_CLIP = 128

D_FF_DENSE = ((8 * D_MODEL // 3 + 127) // 128) * 128
N_EXPERTS = 8
TOP_K = 2
D_FF_EXPERT = 7 * D_MODEL // 2

DEEPNORM_ALPHA = (2.0 * DEPTH) ** 0.25
DEEPNORM_BETA = (8.0 * DEPTH) ** -0.25

EPS = 1e-6
F32 = jnp.float32

kernel_name = "yoco_gdn_bandattn_moe_deepnorm"


def layer_norm(x, g, b):
    xf = x.astype(F32)
    mu = jnp.mean(xf, axis=-1, keepdims=True)
    xc = xf - mu
    var = jnp.mean(jnp.square(xc), axis=-1, keepdims=True)
    return (xc * lax.rsqrt(var + EPS) * g.astype(F32) + b.astype(F32)).astype(x.dtype)


def l2norm(t):
    return t * lax.rsqrt(jnp.sum(jnp.square(t), axis=-1, keepdims=True) + EPS)


def causal_depthwise_conv(u, w):
    c = u.shape[-1]
    return lax.conv_general_dilated(
        u, w.reshape(CONV_K, 1, c), window_strides=(1,), padding=[(CONV_K - 1, 0)],
        dimension_numbers=("NWC", "WIO", "NWC"), feature_group_count=c)


def chunk_gated_delta_rule(q, k, v, g, beta):
    b, t, h, dk = q.shape
    dv = v.shape[-1]
    nc = t // CHUNK

    def to_chunks(a):
        return a.reshape(b, nc, CHUNK, h, a.shape[-1]).transpose(0, 1, 3, 2, 4)

    q, k, v = to_chunks(q), to_chunks(k), to_chunks(v)
    g = g.reshape(b, nc, CHUNK, h).transpose(0, 1, 3, 2)
    beta = beta.reshape(b, nc, CHUNK, h).transpose(0, 1, 3, 2)
    g_cum = jnp.cumsum(g, axis=-1)

    causal = jnp.tril(jnp.ones((CHUNK, CHUNK), dtype=bool))
    strict = jnp.tril(jnp.ones((CHUNK, CHUNK), dtype=bool), k=-1)
    diff = g_cum[..., :, None] - g_cum[..., None, :]
    decay = jnp.where(causal, jnp.exp(jnp.where(causal, diff, 0.0)), 0.0)

    kk = jnp.einsum("bnhid,bnhjd->bnhij", k, k)
    a_mat = jnp.where(strict, beta[..., :, None] * kk * decay, 0.0)
    eye = jnp.eye(CHUNK, dtype=F32)
    t_mat = lax.linalg.triangular_solve(
        eye + a_mat, jnp.broadcast_to(eye, a_mat.shape),
        left_side=True, lower=True, unit_diagonal=True)
    u = t_mat @ (v * beta[..., None])
    w = t_mat @ (k * (beta * jnp.exp(g_cum))[..., None])
    qk = jnp.where(causal, jnp.einsum("bnhid,bnhjd->bnhij", q, k) * decay, 0.0)
    q_dec = q * jnp.exp(g_cum)[..., None]
    k_dec = k * jnp.exp(g_cum[..., -1:] - g_cum)[..., None]
    g_last = jnp.exp(g_cum[..., -1])

    def step(state, xs):
        u_c, w_c, qk_c, qd_c, kd_c, gl_c = xs
        v_new = u_c - w_c @ state
        o_c = qd_c @ state + qk_c @ v_new
        state = state * gl_c[..., None, None] + jnp.einsum("bhck,bhcv->bhkv", kd_c, v_new)
        return state, o_c

    xs = tuple(jnp.moveaxis(a, 1, 0) for a in (u, w, qk, q_dec, k_dec, g_last))
    s0 = jnp.zeros((b, h, dk, dv), F32)
    _, o = lax.scan(step, s0, xs)
    return o.transpose(1, 0, 3, 2, 4).reshape(b, t, h, dv)


def gated_deltanet(x, w_in, conv_w, a_log, dt_bias, norm_g, w_o):
    b, t, _ = x.shape
    proj = (x @ w_in).astype(F32)
    qkv, z, beta_logit, a_logit = jnp.split(
        proj, [3 * A_WIDTH, 4 * A_WIDTH, 4 * A_WIDTH + A_HEADS], axis=-1)
    qkv = jax.nn.silu(causal_depthwise_conv(qkv, conv_w.astype(F32)))
    q, k, v = (u.reshape(b, t, A_HEADS, A_HEAD_DIM) for u in jnp.split(qkv, 3, axis=-1))
    q = l2norm(q) * (A_HEAD_DIM ** -0.5)
    k = l2norm(k)
    beta = jax.nn.sigmoid(beta_logit)
    g = -jnp.exp(a_log.astype(F32)) * jax.nn.softplus(a_logit + dt_bias.astype(F32))
    o = chunk_gated_delta_rule(q, k, v, g, beta)
    o = o * lax.rsqrt(jnp.mean(jnp.square(o), axis=-1, keepdims=True) + EPS) * norm_g.astype(F32)
    o = o * jax.nn.silu(z).reshape(b, t, A_HEADS, A_HEAD_DIM)
    return o.reshape(b, t, A_WIDTH).astype(x.dtype) @ w_o


def band_attention(x, w_q, rel_bias, k_sh, v_sh, w_o):
    b, t, _ = x.shape
    nc = t // CHUNK
    pad = LEFT_CHUNKS * CHUNK
    q = (x @ w_q).reshape(b, nc, CHUNK, B_HEADS, B_HEAD_DIM) * (B_HEAD_DIM ** -0.5)
    q_chunks = jnp.moveaxis(q, 1, 0)
    k_pad = jnp.pad(k_sh, ((0, 0), (pad, 0), (0, 0), (0, 0)))
    v_pad = jnp.pad(v_sh, ((0, 0), (pad, 0), (0, 0), (0, 0)))
    rel = pad + jnp.arange(CHUNK)[:, None] - jnp.arange(BAND)[None, :]
    idx = jnp.clip(rel, -REL_CLIP, REL_CLIP) + REL_CLIP
    bias = rel_bias.astype(F32)[:, idx]
    neg = jnp.finfo(F32).min

    def one_chunk(args):
        c, qc = args
        start = c * CHUNK
        kb = lax.dynamic_slice_in_dim(k_pad, start, BAND, axis=1)
        vb = lax.dynamic_slice_in_dim(v_pad, start, BAND, axis=1)
        s = jnp.einsum("bqhd,bkhd->bhqk", qc, kb).astype(F32) + bias
        valid = (start + jnp.arange(BAND)) >= pad
        s = jnp.where(valid[None, None, None, :], s, neg)
        p = jax.nn.softmax(s, axis=-1).astype(vb.dtype)
        return jnp.einsum("bhqk,bkhd->bqhd", p, vb)

    o = lax.map(one_chunk, (jnp.arange(nc), q_chunks))
    o = jnp.moveaxis(o, 0, 1).reshape(b, t, B_WIDTH)
    return o @ w_o


def swiglu(x, w_up, w_down):
    gate, up = jnp.split(x @ w_up, 2, axis=-1)
    return (jax.nn.silu(gate) * up) @ w_down


def moe_swiglu(x, w_router, w_up, w_down):
    b, t, d = x.shape
    xt = x.reshape(b * t, d)
    logits = (xt @ w_router).astype(F32)
    top_val, top_idx = lax.top_k(logits, TOP_K)
    top_w = jax.nn.softmax(top_val, axis=-1)
    gates = jnp.sum(jax.nn.one_hot(top_idx, N_EXPERTS, dtype=F32) * top_w[..., None], axis=1)
    gates = gates.astype(x.dtype)
    y = jnp.zeros_like(xt)
    for e in range(N_EXPERTS):
        y = y + gates[:, e:e + 1] * swiglu(xt, w_up[e], w_down[e])
    return y.reshape(b, t, d)


def setup_inputs(seed: int = 0) -> dict:
    key = jax.random.key(seed)
    ks = jax.random.split(key, 20)
    d = D_MODEL
    nrm = jax.random.normal
    x = nrm(ks[0], (BATCH, SEQ, d), F32)
    a_w_in = nrm(ks[1], (N_A_LAYERS, d, A_IN_COLS), F32) * d ** -0.5
    a_conv_w = nrm(ks[2], (N_A_LAYERS, CONV_K, 3 * A_WIDTH), F32) * CONV_K ** -0.5
    a_A_log = jnp.log(jax.random.uniform(ks[3], (N_A_LAYERS, A_HEADS), F32, 1.0, 16.0))
    dt = jnp.exp(jax.random.uniform(ks[4], (N_A_LAYERS, A_HEADS), F32,
                                    math.log(1e-3), math.log(1e-1)))
    a_dt_bias = dt + jnp.log(-jnp.expm1(-dt))
    a_norm_g = 1.0 + 0.02 * nrm(ks[5], (N_A_LAYERS, A_HEAD_DIM), F32)
    a_w_o = nrm(ks[6], (N_A_LAYERS, A_WIDTH, d), F32) * A_WIDTH ** -0.5 * DEEPNORM_BETA
    kv_w = nrm(ks[7], (d, 2 * B_WIDTH), F32) * d ** -0.5
    b_w_q = nrm(ks[8], (N_B_LAYERS, d, B_WIDTH), F32) * d ** -0.5
    b_rel_bias = 0.2 * nrm(ks[9], (N_B_LAYERS, B_HEADS, 2 * REL_CLIP + 1), F32)
    b_w_o = nrm(ks[10], (N_B_LAYERS, B_WIDTH, d), F32) * B_WIDTH ** -0.5 * DEEPNORM_BETA
    ffn_w_up = nrm(ks[11], (N_DENSE_LAYERS, d, 2 * D_FF_DENSE), F32) * d ** -0.5
    ffn_w_down = nrm(ks[12], (N_DENSE_LAYERS, D_FF_DENSE, d), F32) * D_FF_DENSE ** -0.5 * DEEPNORM_BETA
    moe_router = nrm(ks[13], (N_MOE_LAYERS, d, N_EXPERTS), F32) * d ** -0.5
    moe_w_up = nrm(ks[14], (N_MOE_LAYERS, N_EXPERTS, d, 2 * D_FF_EXPERT), F32) * d ** -0.5
    moe_w_down = nrm(ks[15], (N_MOE_LAYERS, N_EXPERTS, D_FF_EXPERT, d), F32) * D_FF_EXPERT ** -0.5 * DEEPNORM_BETA
    ln1_g = 1.0 + 0.02 * nrm(ks[16], (DEPTH, d), F32)
    ln1_b = 0.02 * nrm(ks[17], (DEPTH, d), F32)
    ln2_g = 1.0 + 0.02 * nrm(ks[18], (DEPTH, d), F32)
    ln2_b = 0.02 * nrm(ks[19], (DEPTH, d), F32)
    return {"x": x, "a_w_in": a_w_in, "a_conv_w": a_conv_w, "a_A_log": a_A_log,
            "a_dt_bias": a_dt_bias, "a_norm_g": a_norm_g, "a_w_o": a_w_o, "kv_w": kv_w,
            "b_w_q": b_w_q, "b_rel_bias": b_rel_bias, "b_w_o": b_w_o,
            "ffn_w_up": ffn_w_up, "ffn_w_down": ffn_w_down, "moe_router": moe_router,
            "moe_w_up": moe_w_up, "moe_w_down": moe_w_down,
            "ln1_g": ln1_g, "ln1_b": ln1_b, "ln2_g": ln2_g, "ln2_b": ln2_b}


def reference(x, a_w_in, a_conv_w, a_A_log, a_dt_bias, a_norm_g, a_w_o, kv_w,
              b_w_q, b_rel_bias, b_w_o, ffn_w_up, ffn_w_down, moe_router,
              moe_w_up, moe_w_down, ln1_g, ln1_b, ln2_g, ln2_b):
    b, t, _ = x.shape
    k_sh = None
    v_sh = None
    for layer in range(DEPTH):
        if layer < N_A_LAYERS:
            h = gated_deltanet(x, a_w_in[layer], a_conv_w[layer], a_A_log[layer],
                               a_dt_bias[layer], a_norm_g[layer], a_w_o[layer])
        else:
            if layer == N_A_LAYERS:
                kv = x @ kv_w
                k_sh, v_sh = (u.reshape(b, t, B_HEADS, B_HEAD_DIM) for u in jnp.split(kv, 2, axis=-1))
            j = layer - N_A_LAYERS
            h = band_attention(x, b_w_q[j], b_rel_bias[j], k_sh, v_sh, b_w_o[j])
        x = layer_norm(DEEPNORM_ALPHA * x + h, ln1_g[layer], ln1_b[layer])
        if layer % 2 == 0:
            f = swiglu(x, ffn_w_up[layer // 2], ffn_w_down[layer // 2])
        else:
            f = moe_swiglu(x, moe_router[layer // 2], moe_w_up[layer // 2], moe_w_down[layer // 2])
        x = layer_norm(DEEPNORM_ALPHA * x + f, ln2_g[layer], ln2_b[layer])
    return x
```

```python
import contextlib
import numpy as np
import concourse.bass as bass
import concourse.mybir as mybir
from concourse.bass_utils import run_bass_kernel_spmd

F32 = mybir.dt.float32
BF16 = mybir.dt.bfloat16
ALU = mybir.AluOpType
AF = mybir.ActivationFunctionType
AX = mybir.AxisListType

ALPHA = 2.0 ** 0.5
EPS = 1e-6
NEG = -30000.0


class Buf:
    __slots__ = ("name", "t", "w", "r", "dsem", "dcnt", "dkey", "psum")

    def __init__(self, name, t):
        self.name = name
        self.t = t
        self.w = None
        self.r = []
        self.dsem = None
        self.dcnt = 0
        self.dkey = None
        self.psum = False

    def __getitem__(self, k):
        return self.t[k]


class Sched:
    ENG = ("pe", "act", "dve", "pool", "sp")

    def __init__(self, nc, stack):
        self.nc = nc
        self.stack = stack
        self.e = {"pe": nc.tensor, "act": nc.scalar, "dve": nc.vector,
                  "pool": nc.gpsimd, "sp": nc.sync}
        self.sem = {}
        self.cnt = {}
        for k in self.ENG:
            self.sem[k] = stack.enter_context(nc.semaphore("s_" + k))
            self.cnt[k] = 0
        self.seen = {k: {} for k in self.ENG}
        self.nbuf = 0
        self.dbufs = []

    def sbuf(self, name, shape, dtype):
        return Buf(name, self.stack.enter_context(self.nc.sbuf_tensor("sb_" + name, list(shape), dtype)))

    def psum(self, name, shape, dtype):
        b = Buf(name, self.stack.enter_context(self.nc.psum_tensor("ps_" + name, list(shape), dtype)))
        b.psum = True
        return b

    def view(self, name, ap):
        return Buf(name, ap)

    def _dsem(self, b):
        if b.dsem is None:
            self.nbuf += 1
            b.dsem = self.stack.enter_context(self.nc.semaphore("d%d" % self.nbuf))
            b.dkey = ("d", self.nbuf)
            self.sem[b.dkey] = b.dsem
            self.dbufs.append(b)
        return b.dkey

    def _wait(self, eng, key, val):
        if val <= 0:
            return
        seen = self.seen[eng]
        if seen.get(key, 0) >= val:
            return
        seen[key] = val
        self.e[eng].wait_ge(self.sem[key], val)

    def _deps(self, eng, reads, writes):
        for b in reads:
            if b.w is not None:
                self._wait(eng, b.w[0], b.w[1])
            if b.psum:
                for (k, v) in b.r:
                    if k != eng:
                        self._wait(eng, k, v)
        for b in writes:
            if b.w is not None:
                self._wait(eng, b.w[0], b.w[1])
            for (k, v) in b.r:
                self._wait(eng, k, v)

    def _mark(self, key, val, reads, writes):
        for b in writes:
            b.w = (key, val)
            b.r = []
        for b in reads:
            if b in writes:
                continue
            b.r = [(k, v) for (k, v) in b.r if k != key]
            b.r.append((key, val))

    def op(self, eng, fn, reads=(), writes=()):
        self._deps(eng, reads, writes)
        ins = fn(self.e[eng])
        self.cnt[eng] += 1
        ins.then_inc(self.sem[eng], 1)
        self._mark(eng, self.cnt[eng], reads, writes)
        return ins

    def group(self, eng, fns, reads=(), writes=()):
        self._deps(eng, reads, writes)
        ins = None
        for fn in fns:
            ins = fn(self.e[eng])
        self.cnt[eng] += 1
        ins.then_inc(self.sem[eng], 1)
        self._mark(eng, self.cnt[eng], reads, writes)
        return ins

    def dma(self, q, out_ap, in_ap, reads=(), writes=(), owner=None):
        self._deps(q, reads, writes)
        if owner is None:
            owner = writes[0] if writes else reads[0]
        key = self._dsem(owner)
        ins = self.e[q].dma_start(out=out_ap, in_=in_ap)
        ins.then_inc(self.sem[key], 16)
        owner.dcnt += 16
        self._mark(key, owner.dcnt, reads, writes)
        return ins

    def barrier(self):
        for eng in self.ENG:
            for k in self.ENG:
                self._wait(eng, k, self.cnt[k])
            for b in self.dbufs:
                self._wait(eng, b.dkey, b.dcnt)

    def drop_dbufs(self, keep):
        self.dbufs = [b for b in self.dbufs if b in keep]


class Rot:
    def __init__(self, bufs):
        self.bufs = bufs
        self.i = 0

    def next(self):
        b = self.bufs[self.i % len(self.bufs)]
        self.i += 1
        return b


def build(stage=99):
    nc = bass.Bass("TRN2", target_bir_lowering=False)

    def din(name, shape, dtype=F32):
        return nc.dram_tensor(name, list(shape), dtype, kind="ExternalInput").ap()

    x_win = din("x_win", [4096, 1024])
    w_in = din("w_in", [1024, 4112])
    cw_d = din("cw", [128, 24, 4])
    alog_d = din("alog", [128, 8])
    dtb_d = din("dtb", [128, 8])
    normg_d = din("normg", [128, 1])
    w_oa = din("w_oa", [1024, 1024])
    kv_w = din("kv_w", [1024, 2048])
    w_q = din("w_q", [1024, 1024])
    biasT = din("biasT", [16, 128, 640])
    wmask_d = din("wmask", [128, 640])
    kvalid_d = din("kvalid", [1, 2560])
    w_ob = din("w_ob", [1024, 1024])
    ffn_up = din("ffn_up", [1024, 5632])
    ffn_dn = din("ffn_dn", [2816, 1024])
    router_d = din("router", [1024, 8])
    if stage >= 20:
        moe_up = din("moe_up", [8, 1024, 7168])
        moe_dn = din("moe_dn", [8, 3584, 1024])
    lng_d = din("lng", [128, 4, 8])
    lnb_d = din("lnb", [128, 4, 8])
    cmat_d = din("cmat", [128, 6, 128])
    out_d = nc.dram_tensor("out", [2048, 1024], F32, kind="ExternalOutput").ap()
    dbg_d = nc.dram_tensor("dbg", [128, 8, 2560], F32, kind="ExternalOutput").ap() if stage < 20 else None

    with contextlib.ExitStack() as top:
        S = Sched(nc, top)
        XF = S.sbuf("XF", [128, 8, 2560], F32)
        CM = S.sbuf("CM", [128, 6, 128], F32)
        CMB = S.sbuf("CMB", [128, 2, 128], BF16)
        LNG = S.sbuf("LNG", [128, 4, 8], F32)
        LNB = S.sbuf("LNB", [128, 4, 8], F32)
        CST = S.view("CST", None)
        S.dma("sp", CM[:], cmat_d[:, :, :], writes=[CST])
        S.dma("sp", LNG[:], lng_d[:, :, :], writes=[CST], owner=CST)
        S.dma("sp", LNB[:], lnb_d[:, :, :], writes=[CST], owner=CST)
        S.op("dve", lambda e: e.tensor_copy(out=CMB[:, 0, :], in_=CM[:, 0, :]), reads=[CST], writes=[CST])
        S.op("dve", lambda e: e.tensor_copy(out=CMB[:, 1, :], in_=CM[:, 5, :]), reads=[CST], writes=[CST])
        if stage < 20:
            S.op("dve", lambda e: e.memset(XF[:], 0.0), writes=[])
        IDF, TRIF, BLKF, MBL, MBU, ONEF = (CM[:, i, :] for i in range(6))
        IDB, ONEB = CMB[:, 0, :], CMB[:, 1, :]
        XFv = [[S.view("XF%d_%d" % (k, t), None) for t in range(5)] for k in range(8)]

        def layer_norm(li, tt, xb_t=None, xbv=None):
            tok = slice(tt * 512, (tt + 1) * 512)
            ps_s = psB.next()
            ps_q = psB.next()
            fns = []
            S.group("pe", [lambda e, k=k: e.matmul(ps_s[:, :], lhsT=ONEF, rhs=XF[:, k, tok], start=(k == 0), stop=(k == 7))
                           for k in range(8)], reads=[CST] + [XFv[k][tt] for k in range(8)], writes=[ps_s])
            sqs = []
            for k in range(8):
                sq = lnsq.next()
                S.op("act", lambda e, sq=sq, k=k: e.activation(out=sq[:, :], in_=XF[:, k, tok], func=AF.Square),
                     reads=[XFv[k][tt]], writes=[sq])
                S.op("pe", lambda e, sq=sq, k=k: e.matmul(ps_q[:, :], lhsT=ONEF, rhs=sq[:, :], start=(k == 0), stop=(k == 7)),
                     reads=[CST, sq], writes=[ps_q])
            mean = lnst.next()
            rstd = lnst.next()
            S.op("act", lambda e: e.activation(out=mean[:, :], in_=ps_s[:, :], func=AF.Copy, scale=1.0 / 1024),
                 reads=[ps_s], writes=[mean])
            m2 = lnsq.next()
            S.op("dve", lambda e: e.tensor_tensor(out=m2[:, :], in0=mean[:, :], in1=mean[:, :], op=ALU.mult),
                 reads=[mean], writes=[m2])
            S.op("dve", lambda e: e.scalar_tensor_tensor(out=rstd[:, :], in0=ps_q[:, :], scalar=1.0 / 1024, in1=m2[:, :],
                                                        op0=ALU.mult, op1=ALU.subtract), reads=[ps_q, m2], writes=[rstd])
            S.op("act", lambda e: e.activation(out=rstd[:, :], in_=rstd[:, :], func=AF.Ln, bias=EPS, scale=1.0),
                 reads=[rstd], writes=[rstd])
            S.op("act", lambda e: e.activation(out=rstd[:, :], in_=rstd[:, :], func=AF.Exp, scale=-0.5),
                 reads=[rstd], writes=[rstd])
            for k in range(8):
                tmp = lnsq.next()
                S.op("dve", lambda e, k=k, tmp=tmp: e.tensor_tensor(out=tmp[:, :], in0=XF[:, k, tok], in1=mean[:, :], op=ALU.subtract),
                     reads=[XFv[k][tt], mean], writes=[tmp])
                S.op("dve", lambda e, tmp=tmp: e.tensor_tensor(out=tmp[:, :], in0=tmp[:, :], in1=rstd[:, :], op=ALU.mult),
                     reads=[tmp, rstd], writes=[tmp])
                S.op("act", lambda e, k=k, tmp=tmp: e.activation(out=XF[:, k, tok], in_=tmp[:, :], func=AF.Identity,
                                                                scale=LNG[:, li, k:k + 1], bias=LNB[:, li, k:k + 1]),
                     reads=[tmp, CST], writes=[XFv[k][tt]])
                if xb_t is not None:
                    S.op("dve", lambda e, k=k: e.tensor_copy(out=xb_t[:, k, tok], in_=XF[:, k, tok]),
                         reads=[XFv[k][tt]], writes=[xbv[k][tt]])

        with contextlib.ExitStack() as pa:
            S.stack = pa
            psA = Rot([S.psum("psA%d" % i, [128, 512], F32) for i in range(3)])
            psB = Rot([S.psum("psB%d" % i, [128, 512], F32) for i in range(2)])
            psW = S.psum("psW", [128, 1024], F32)
            psT_t = S.psum("psT", [128, 1024], BF16)
            psTv = S.view("psTv", psT_t[:, 0:512])
            psTv.psum = True
            psT = Rot([psTv])
            scr = Rot([S.sbuf("scr%d" % i, [128, 512], F32) for i in range(4)])
            lnsq = scr
            lnst = Rot([S.sbuf("lnst%d" % i, [128, 512], F32) for i in range(2)])
            xtm = Rot([S.sbuf("xtm%d" % i, [128, 1024], F32) for i in range(2)])
            xTb = S.sbuf("xTb", [128, 8, 512], BF16)
            wpan = Rot([S.sbuf("wpan%d" % i, [128, 8, 256], BF16) for i in range(3)])
            wba = S.sbuf("wba", [128, 8, 16], BF16)
            qT = S.sbuf("qT", [128, 8, 512], BF16)
            kT = S.sbuf("kT", [128, 8, 512], BF16)
            vT = S.sbuf("vT", [128, 8, 512], BF16)
            OT = vT
            OG = qT
            halo = S.sbuf("halo", [128, 24, 3], F32)
            cwt = S.sbuf("cwt", [128, 24, 4], F32)
            ubuf = Rot([S.sbuf("ubuf%d" % i, [128, 515], F32) for i in range(2)])
            ycv = scr
            sqb = Rot([S.sbuf("sqb%d" % i, [128, 512], BF16) for i in range(1)])
            alog = S.sbuf("alog", [128, 8], F32)
            dtb = S.sbuf("dtb", [128, 8], F32)
            negA = S.sbuf("negA", [128, 8], F32)
            normg = S.sbuf("normg", [128, 1], F32)
            PB = []
            for par in range(2):
                PB.append((S.sbuf("ba%d" % par, [128, 16], F32), S.sbuf("beta%d" % par, [128, 8], F32), S.sbuf("gt%d" % par, [128, 8], F32),
                           S.sbuf("gcum%d" % par, [128, 8], F32), S.sbuf("glast%d" % par, [128, 8], F32), S.sbuf("bexp%d" % par, [128, 8], F32),
                           S.sbuf("kdsc%d" % par, [128, 8], F32), S.sbuf("DECB%d" % par, [128, 8, 128], BF16), S.sbuf("DECT%d" % par, [128, 8, 128], BF16),
                           S.sbuf("EROW%d" % par, [128, 8, 128], BF16), S.sbuf("GLS%d" % par, [128, 8, 2], F32)))
            tmpW = S.sbuf("tmpW", [128, 8, 128], F32)
            Dm = tmpW
            Sf = [S.sbuf("Sf%d" % g, [128, 4, 128], F32) for g in range(2)]
            Sb = [S.sbuf("Sb%d" % g, [128, 4, 128], BF16) for g in range(2)]
            ppc = [[S.sbuf("pp%d_%d" % (g, i), [128, 4, 128], BF16) for i in range(4)] for g in range(2)]
            gbufc = [{n: S.sbuf("g%d_%s" % (g, n), [128, 4, 128], BF16) for n in ("Tt", "Xw", "kd", "Xu", "wT", "QKT", "qd", "VN0", "VN1")} for g in range(2)]
            ufc = [S.sbuf("uf%d" % g, [128, 4, 128], F32) for g in range(2)]

            def run_chains(gens):
                gens = list(gens)
                while gens:
                    for gc_ in list(gens):
                        try:
                            next(gc_)
                        except StopIteration:
                            gens.remove(gc_)

            S.dma("sp", cwt[:], cw_d[:, :, :], writes=[CST], owner=CST)
            S.dma("sp", alog[:], alog_d[:, :], writes=[CST], owner=CST)
            S.dma("sp", dtb[:], dtb_d[:, :], writes=[CST], owner=CST)
            S.dma("sp", normg[:], normg_d[:, :], writes=[CST], owner=CST)
            S.dma("pool", wba[:], w_in[:, 4096:4112].rearrange("(k p) m -> p k m", p=128), writes=[wba])
            S.op("act", lambda e: e.activation(out=negA[:], in_=alog[:], func=AF.Exp), reads=[CST], writes=[CST])
            S.op("dve", lambda e: e.tensor_scalar(out=negA[:], in0=negA[:], scalar1=-1.0, scalar2=None, op0=ALU.mult),
                 reads=[CST], writes=[CST])
            S.op("dve", lambda e: e.memset(halo[:], 0.0), writes=[halo])
            for g in range(2):
                S.op("dve", lambda e, g=g: e.memset(Sf[g][:], 0.0), writes=[Sf[g]])
                S.op("dve", lambda e, g=g: e.memset(Sb[g][:], 0.0), writes=[Sb[g]])

            def load_panel(col0, ncols=256, src=w_in):
                wp = wpan.next()
                S.dma("pool", wp[:, :, 0:ncols], src[:, col0:col0 + ncols].rearrange("(k p) m -> p k m", p=128), writes=[wp])
                return wp

            stop = False
            for st in range(8):
                if stop:
                    break
                full = st >= 3
                ft = st - 3
                for tq in range(4):
                    xt = xtm.next()
                    r0 = st * 512 + tq * 128
                    S.dma("sp", xt[:], x_win[r0:r0 + 128, :], writes=[xt])
                    for half in range(2):
                        ps = psA.next()
                        S.group("pe", [lambda e, j=j, ps=ps, xt=xt, half=half: e.transpose(
                            out=ps[:, j * 128:(j + 1) * 128], in_=xt[:, (half * 4 + j) * 128:(half * 4 + j + 1) * 128], identity=IDF)
                            for j in range(4)], reads=[xt, CST], writes=[ps])
                        S.op("act", lambda e, ps=ps, half=half, tq=tq: e.activation(
                            out=xTb[:, half * 4:half * 4 + 4, tq * 128:(tq + 1) * 128],
                            in_=ps[:, :].rearrange("p (j t) -> p j t", j=4), func=AF.Copy), reads=[ps], writes=[xTb])
                        if full:
                            S.op("dve", lambda e, ps=ps, half=half, tq=tq: e.tensor_scalar(
                                out=XF[:, half * 4:half * 4 + 4, ft * 512 + tq * 128:ft * 512 + (tq + 1) * 128],
                                in0=ps[:, :].rearrange("p (j t) -> p j t", j=4), scalar1=ALPHA, scalar2=None, op0=ALU.mult),
                                reads=[ps], writes=[XFv[k][ft] for k in range(half * 4, half * 4 + 4)])
                if stage == 1:
                    stop = True
                    continue
                secs = [("k", 1024, kT, 8), ("v", 2048, vT, 16)] + ([("q", 0, qT, 0)] if st >= 2 else [])
                pend = []
                for (nm, c0, dst, m0) in secs:
                    for pn in range(4):
                        wp = load_panel(c0 + pn * 256)
                        for j in range(2):
                            hh = pn * 2 + j
                            ps = psA.next()
                            S.group("pe", [lambda e, k=k, ps=ps, wp=wp, j=j: e.matmul(
                                ps[:, :], lhsT=wp[:, k, j * 128:(j + 1) * 128], rhs=xTb[:, k, :], start=(k == 0), stop=(k == 7))
                                for k in range(8)], reads=[wp, xTb], writes=[ps])
                            ub = ubuf.next()
                            m = m0 + hh
                            S.op("act", lambda e, ub=ub, ps=ps: e.activation(out=ub[:, 3:515], in_=ps[:, :], func=AF.Copy),
                                 reads=[ps], writes=[ub])
                            S.op("pool", lambda e, ub=ub, m=m: e.tensor_copy(out=ub[:, 0:3], in_=halo[:, m, :]),
                                 reads=[halo], writes=[ub])
                            S.op("pool", lambda e, ub=ub, m=m: e.tensor_copy(out=halo[:, m, :], in_=ub[:, 512:515]),
                                 reads=[ub], writes=[halo])
                            if nm == "q" and not full:
                                continue
                            y = ycv.next()
                            S.op("act", lambda e, ps=ps, y=y, m=m: e.activation(out=y[:, :], in_=ps[:, :], func=AF.Copy, scale=cwt[:, m, 3:4]),
                                 reads=[ps, CST], writes=[y])
                            for jj in range(3):
                                S.op("dve", lambda e, ub=ub, y=y, m=m, jj=jj: e.scalar_tensor_tensor(
                                    out=y[:, :], in0=ub[:, jj:jj + 512], scalar=cwt[:, m, jj:jj + 1], in1=y[:, :],
                                    op0=ALU.mult, op1=ALU.add), reads=[ub, y, CST], writes=[y])
                            pend.append((nm, dst, hh, y))
                            if len(pend) == 4:
                                for (nm2, dst2, h2, y2) in pend:
                                    if nm2 == "v":
                                        S.op("act", lambda e, dst2=dst2, h2=h2, y2=y2: e.activation(
                                            out=dst2[:, h2, :], in_=y2[:, :], func=AF.Silu), reads=[y2], writes=[dst2])
                                    else:
                                        S.op("act", lambda e, y2=y2: e.activation(out=y2[:, :], in_=y2[:, :], func=AF.Silu),
                                             reads=[y2], writes=[y2])
                                for (nm2, dst2, h2, y2) in pend:
                                    if nm2 == "v":
                                        continue
                                    sq = sqb.next()
                                    S.op("act", lambda e, sq=sq, y2=y2: e.activation(out=sq[:, :], in_=y2[:, :], func=AF.Square),
                                         reads=[y2], writes=[sq])
                                    pss = psB.next()
                                    S.op("pe", lambda e, sq=sq, pss=pss: e.matmul(pss[:, :], lhsT=ONEB, rhs=sq[:, :], start=True, stop=True),
                                         reads=[sq, CST], writes=[pss])
                                    rn = lnst.next()
                                    S.op("act", lambda e, rn=rn, pss=pss: e.activation(out=rn[:, :], in_=pss[:, :], func=AF.Ln, bias=EPS, scale=1.0),
                                         reads=[pss], writes=[rn])
                                    S.op("act", lambda e, rn=rn: e.activation(out=rn[:, :], in_=rn[:, :], func=AF.Exp, scale=-0.5),
                                         reads=[rn], writes=[rn])
                                    if nm2 == "q":
                                        S.op("dve", lambda e, dst2=dst2, h2=h2, y2=y2, rn=rn: e.scalar_tensor_tensor(
                                            out=dst2[:, h2, :], in0=y2[:, :], scalar=128.0 ** -0.5, in1=rn[:, :],
                                            op0=ALU.mult, op1=ALU.mult), reads=[y2, rn], writes=[dst2])
                                    else:
                                        S.op("dve", lambda e, dst2=dst2, h2=h2, y2=y2, rn=rn: e.tensor_tensor(
                                            out=dst2[:, h2, :], in0=y2[:, :], in1=rn[:, :], op=ALU.mult), reads=[y2, rn], writes=[dst2])
                                pend = []
                if stage == 2:
                    stop = True
                    continue
                def pre(tq):
                    ba, beta, gt, gcum, glast, bexp, kdsc, DECB, DECT, EROW, GLS = PB[tq % 2]
                    tcol = slice(tq * 128, (tq + 1) * 128)
                    psb = psB.next()
                    S.group("pe", [lambda e, k=k, psb=psb: e.matmul(psb[:, 0:16], lhsT=xTb[:, k, tcol], rhs=wba[:, k, :],
                                                                  start=(k == 0), stop=(k == 7)) for k in range(8)],
                            reads=[xTb, wba], writes=[psb])
                    S.op("act", lambda e, psb=psb: e.activation(out=ba[:, :], in_=psb[:, 0:16], func=AF.Copy), reads=[psb], writes=[ba])
                    S.op("act", lambda e: e.activation(out=beta[:, :], in_=ba[:, 0:8], func=AF.Exp, scale=-1.0), reads=[ba], writes=[beta])
                    S.op("dve", lambda e: e.tensor_scalar(out=beta[:, :], in0=beta[:, :], scalar1=1.0, scalar2=None, op0=ALU.add),
                         reads=[beta], writes=[beta])
                    S.op("dve", lambda e: e.reciprocal(out=beta[:, :], in_=beta[:, :]), reads=[beta], writes=[beta])
                    yield
                    S.op("dve", lambda e: e.tensor_tensor(out=gt[:, :], in0=ba[:, 8:16], in1=dtb[:, :], op=ALU.add), reads=[ba, CST], writes=[gt])
                    S.op("act", lambda e: e.activation(out=gt[:, :], in_=gt[:, :], func=AF.Exp), reads=[gt], writes=[gt])
                    S.op("act", lambda e: e.activation(out=gt[:, :], in_=gt[:, :], func=AF.Ln, bias=1.0, scale=1.0), reads=[gt], writes=[gt])
                    S.op("dve", lambda e: e.tensor_tensor(out=gt[:, :], in0=gt[:, :], in1=negA[:, :], op=ALU.mult), reads=[gt, CST], writes=[gt])
                    yield
                    psb = psB.next()
                    S.op("pe", lambda e, psb=psb: e.matmul(psb[:, 0:8], lhsT=TRIF, rhs=gt[:, :], start=True, stop=True), reads=[gt, CST], writes=[psb])
                    S.op("pe", lambda e, psb=psb: e.matmul(psb[:, 8:16], lhsT=BLKF, rhs=gt[:, :], start=True, stop=True), reads=[gt, CST, psb], writes=[psb])
                    S.op("act", lambda e, psb=psb: e.activation(out=gcum[:, :], in_=psb[:, 0:8], func=AF.Copy), reads=[psb], writes=[gcum])
                    S.op("act", lambda e, psb=psb: e.activation(out=glast[:, :], in_=psb[:, 8:16], func=AF.Copy), reads=[psb], writes=[glast])
                    yield
                    S.op("act", lambda e: e.activation(out=bexp[:, :], in_=gcum[:, :], func=AF.Exp), reads=[gcum], writes=[bexp])
                    S.op("dve", lambda e: e.tensor_tensor(out=bexp[:, :], in0=bexp[:, :], in1=beta[:, :], op=ALU.mult), reads=[bexp, beta], writes=[bexp])
                    S.op("dve", lambda e: e.tensor_tensor(out=kdsc[:, :], in0=glast[:, :], in1=gcum[:, :], op=ALU.subtract), reads=[glast, gcum], writes=[kdsc])
                    S.op("act", lambda e: e.activation(out=kdsc[:, :], in_=kdsc[:, :], func=AF.Exp), reads=[kdsc], writes=[kdsc])
                    yield
                    S.op("dve", lambda e: e.tensor_tensor(out=Dm[:, :, :], in0=gcum[:, :].unsqueeze(2).to_broadcast([128, 8, 128]),
                                                          in1=IDF.unsqueeze(1).to_broadcast([128, 8, 128]), op=ALU.mult),
                         reads=[gcum, CST], writes=[Dm])
                    S.group("pe", [lambda e, hf=hf: e.matmul(psW[:, hf * 512:(hf + 1) * 512], lhsT=ONEF,
                                                           rhs=Dm[:, hf * 4:hf * 4 + 4, :].rearrange("p h j -> p (h j)"), start=True, stop=True)
                                   for hf in range(2)], reads=[Dm, CST], writes=[psW])
                    R3 = psW[:, :].rearrange("p (h j) -> p h j", h=8)
                    gc_b = gcum[:, :].unsqueeze(2).to_broadcast([128, 8, 128])
                    S.op("dve", lambda e: e.tensor_tensor(out=tmpW[:, :, :], in0=gc_b, in1=R3, op=ALU.subtract), reads=[gcum, psW], writes=[tmpW])
                    S.op("dve", lambda e: e.tensor_tensor(out=tmpW[:, :, :], in0=tmpW[:, :, :], in1=MBL.unsqueeze(1).to_broadcast([128, 8, 128]), op=ALU.add),
                         reads=[tmpW, CST], writes=[tmpW])
                    S.op("act", lambda e: e.activation(out=tmpW[:, :, :], in_=tmpW[:, :, :], func=AF.Exp), reads=[tmpW], writes=[tmpW])
                    S.op("dve", lambda e: e.tensor_tensor(out=DECB[:, :, :], in0=tmpW[:, :, :], in1=beta[:, :].unsqueeze(2).to_broadcast([128, 8, 128]), op=ALU.mult),
                         reads=[tmpW, beta], writes=[DECB])
                    yield
                    S.op("dve", lambda e: e.tensor_tensor(out=tmpW[:, :, :], in0=R3, in1=gc_b, op=ALU.subtract), reads=[gcum, psW, tmpW], writes=[tmpW])
                    S.op("dve", lambda e: e.tensor_tensor(out=tmpW[:, :, :], in0=tmpW[:, :, :], in1=MBU.unsqueeze(1).to_broadcast([128, 8, 128]), op=ALU.add),
                         reads=[tmpW, CST], writes=[tmpW])
                    S.op("act", lambda e: e.activation(out=DECT[:, :, :], in_=tmpW[:, :, :], func=AF.Exp), reads=[tmpW], writes=[DECT])
                    yield
                    S.op("act", lambda e: e.activation(out=EROW[:, :, :], in_=R3, func=AF.Exp), reads=[psW], writes=[EROW])
                    S.op("act", lambda e: e.activation(out=GLS[:, :, :], in_=R3[:, :, 63:128:64], func=AF.Exp), reads=[psW], writes=[GLS])

                    yield

                def unit(g, tq):
                    tcol = slice(tq * 128, (tq + 1) * 128)
                    ba, beta, gt, gcum, glast, bexp, kdsc, DECB, DECT, EROW, GLS = PB[tq % 2]
                    pp = ppc[g]
                    gbuf = gbufc[g]
                    hs = slice(g * 4, g * 4 + 4)

                    def mm4(ps, lfn, rfn, reads):
                        S.group("pe", [lambda e, a=a: e.matmul(ps[:, a * 128:(a + 1) * 128], lhsT=lfn(a), rhs=rfn(a), start=True, stop=True)
                                       for a in range(4)], reads=reads, writes=[ps])

                    def tr4(ps, ifn, reads):
                        S.group("pe", [lambda e, a=a: e.transpose(out=ps[:, a * 128:(a + 1) * 128], in_=ifn(a), identity=IDB)
                                       for a in range(4)], reads=reads + [CST], writes=[ps])

                    def ev(dst, ps, eng="act"):
                        if eng == "act":
                            S.op("act", lambda e: e.activation(out=dst[:, :, :].rearrange("p a j -> p (a j)"), in_=ps[:, :], func=AF.Copy),
                                 reads=[ps], writes=[dst])
                        else:
                            S.op("dve", lambda e: e.tensor_copy(out=dst[:, :, :].rearrange("p a j -> p (a j)"), in_=ps[:, :]),
                                 reads=[ps], writes=[dst])

                    def p3(ps):
                        return ps[:, :].rearrange("p (a j) -> p a j", a=4)

                    ps = psA.next()
                    mm4(ps, lambda a: kT[:, g * 4 + a, tcol], lambda a: kT[:, g * 4 + a, tcol], [kT])
                    A = pp[0]
                    S.op("dve", lambda e: e.tensor_tensor(out=A[:, :, :], in0=p3(ps), in1=DECB[:, hs, :], op=ALU.mult), reads=[ps, DECB], writes=[A])
                    yield
                    pst = psT.next()
                    tr4(pst, lambda a: A[:, a, :], [A])
                    AT = pp[1]
                    ev(AT, pst, "act")
                    Tt = gbuf["Tt"]
                    S.op("dve", lambda e: e.tensor_tensor(out=Tt[:, :, :], in0=IDF.unsqueeze(1).to_broadcast([128, 4, 128]),
                                                          in1=pst[:, :].rearrange("p (a j) -> p a j", a=4), op=ALU.subtract),
                         reads=[pst, CST], writes=[Tt])
                    yield
                    P, PT = A, AT
                    for lvl in range(5):
                        ps = psA.next()
                        mm4(ps, lambda a: PT[:, a, :], lambda a: P[:, a, :], [P, PT])
                        P2 = pp[2] if P is pp[0] else pp[0]
                        ev(P2, ps, "act")
                        yield
                        P2T = None
                        if lvl < 4:
                            ps2 = psA.next()
                            mm4(ps2, lambda a: P[:, a, :], lambda a: PT[:, a, :], [P, PT])
                            P2T = pp[3] if PT is pp[1] else pp[1]
                            ev(P2T, ps2, "act")
                            yield
                        ps3 = psA.next()
                        mm4(ps3, lambda a: P2[:, a, :], lambda a: Tt[:, a, :], [P2, Tt])
                        S.op("dve", lambda e: e.tensor_tensor(out=Tt[:, :, :], in0=Tt[:, :, :], in1=p3(ps3), op=ALU.add), reads=[ps3, Tt], writes=[Tt])
                        yield
                        P, PT = P2, P2T
                    pst = psT.next()
                    tr4(pst, lambda a: kT[:, g * 4 + a, tcol], [kT])
                    Xw = gbuf["Xw"]
                    kd = gbuf["kd"]
                    pk3 = pst[:, :].rearrange("p (a j) -> p a j", a=4)
                    S.op("dve", lambda e: e.tensor_tensor(out=Xw[:, :, :], in0=pk3, in1=bexp[:, hs].unsqueeze(2).to_broadcast([128, 4, 128]), op=ALU.mult),
                         reads=[pst, bexp], writes=[Xw])
                    S.op("dve", lambda e: e.tensor_tensor(out=kd[:, :, :], in0=pk3, in1=kdsc[:, hs].unsqueeze(2).to_broadcast([128, 4, 128]), op=ALU.mult),
                         reads=[pst, kdsc], writes=[kd])
                    yield
                    pst = psT.next()
                    tr4(pst, lambda a: vT[:, g * 4 + a, tcol], [vT])
                    Xu = gbuf["Xu"]
                    pv3 = pst[:, :].rearrange("p (a j) -> p a j", a=4)
                    S.op("dve", lambda e: e.tensor_tensor(out=Xu[:, :, :], in0=pv3, in1=beta[:, hs].unsqueeze(2).to_broadcast([128, 4, 128]), op=ALU.mult),
                         reads=[pst, beta], writes=[Xu])
                    yield
                    ps = psA.next()
                    mm4(ps, lambda a: Tt[:, a, :], lambda a: Xu[:, a, :], [Tt, Xu])
                    u = ufc[g]
                    ev(u, ps, "act")
                    yield
                    ps = psA.next()
                    mm4(ps, lambda a: Xw[:, a, :], lambda a: Tt[:, a, :], [Tt, Xw])
                    wT = gbuf["wT"]
                    ev(wT, ps, "act")
                    yield
                    if full:
                        ps = psA.next()
                        mm4(ps, lambda a: kT[:, g * 4 + a, tcol], lambda a: qT[:, g * 4 + a, tcol], [kT, qT])
                        QKT = gbuf["QKT"]
                        S.op("dve", lambda e: e.tensor_tensor(out=QKT[:, :, :], in0=p3(ps), in1=DECT[:, hs, :], op=ALU.mult), reads=[ps, DECT], writes=[QKT])
                        qd = gbuf["qd"]
                        S.op("pool", lambda e: e.tensor_tensor(out=qd[:, :, :], in0=qT[:, hs, tcol], in1=EROW[:, hs, :], op=ALU.mult),
                             reads=[qT, EROW], writes=[qd])
                        yield
                    for c in range(2):
                        pr = slice(c * 64, c * 64 + 64)
                        ps = psA.next()
                        mm4(ps, lambda a: wT[:, a, :], lambda a: Sb[g][:, a, :], [wT, Sb[g]])
                        VN = gbuf["VN%d" % c]
                        S.op("dve", lambda e: e.tensor_tensor(out=VN[pr, :, :], in0=u[pr, :, :], in1=p3(ps)[pr, :, :], op=ALU.subtract),
                             reads=[u, ps], writes=[VN])
                        yield
                        if full:
                            pso = psB.next()
                            fns = []
                            for a in range(4):
                                fns.append(lambda e, a=a: e.matmul(pso[:, a * 64:(a + 1) * 64], lhsT=Sb[g][:, a, :], rhs=qd[:, a, pr], start=True, stop=False))
                                fns.append(lambda e, a=a: e.matmul(pso[:, a * 64:(a + 1) * 64], lhsT=VN[pr, a, :], rhs=QKT[pr, a, pr], start=False, stop=True))
                            S.group("pe", fns, reads=[Sb[g], qd, VN, QKT], writes=[pso])
                            S.op("act", lambda e: e.activation(
                                out=OT[:, hs, tq * 128 + c * 64:tq * 128 + c * 64 + 64],
                                in_=pso[:, 0:256].rearrange("p (a t) -> p a t", a=4), func=AF.Copy), reads=[pso], writes=[OT])
                            yield
                        ps = psA.next()
                        mm4(ps, lambda a: kd[pr, a, :], lambda a: VN[pr, a, :], [kd, VN])
                        gl = GLS[:, hs, c:c + 1].to_broadcast([128, 4, 128])
                        S.op("dve", lambda e: e.tensor_tensor(out=Sf[g][:, :, :], in0=Sf[g][:, :, :], in1=gl, op=ALU.mult), reads=[Sf[g], GLS], writes=[Sf[g]])
                        S.op("dve", lambda e: e.tensor_tensor(out=Sf[g][:, :, :], in0=Sf[g][:, :, :], in1=p3(ps), op=ALU.add), reads=[Sf[g], ps], writes=[Sf[g]])
                        S.op("act", lambda e: e.activation(out=Sb[g][:, :, :], in_=Sf[g][:, :, :], func=AF.Copy), reads=[Sf[g]], writes=[Sb[g]])
                        yield

                pre_done = [False] * 4
                unit_done = [[False] * 4 for _ in range(2)]

                def pre_all():
                    for tq in range(4):
                        while tq >= 2 and not (unit_done[0][tq - 2] and unit_done[1][tq - 2]):
                            yield
                        yield from pre(tq)
                        pre_done[tq] = True

                def units(g):
                    for tq in range(4):
                        while not pre_done[tq]:
                            yield
                        yield from unit(g, tq)
                        unit_done[g][tq] = True

                run_chains([pre_all(), units(0), units(1)])
                if stage == 5:
                    stop = True
                    continue
                if not full:
                    continue
                rstds = []
                for h in range(8):
                    sq = sqb.next()
                    S.op("act", lambda e, sq=sq, h=h: e.activation(out=sq[:, :], in_=OT[:, h, :], func=AF.Square), reads=[OT], writes=[sq])
                    pss = psB.next()
                    S.op("pe", lambda e, sq=sq, pss=pss: e.matmul(pss[:, :], lhsT=ONEB, rhs=sq[:, :], start=True, stop=True), reads=[sq, CST], writes=[pss])
                    rn = lnst.next()
                    S.op("act", lambda e, rn=rn, pss=pss: e.activation(out=rn[:, :], in_=pss[:, :], func=AF.Ln, bias=EPS, scale=1.0 / 128), reads=[pss], writes=[rn])
                    S.op("act", lambda e, rn=rn: e.activation(out=rn[:, :], in_=rn[:, :], func=AF.Exp, scale=-0.5), reads=[rn], writes=[rn])
                    S.op("dve", lambda e, rn=rn, h=h: e.scalar_tensor_tensor(out=OT[:, h, :], in0=OT[:, h, :], scalar=normg[:, 0:1], in1=rn[:, :],
                                                                          op0=ALU.mult, op1=ALU.mult), reads=[OT, rn, CST], writes=[OT])
                for pn in range(4):
                    wp = load_panel(3072 + pn * 256)
                    for j in range(2):
                        h = pn * 2 + j
                        ps = psA.next()
                        S.group("pe", [lambda e, k=k, ps=ps, wp=wp, j=j: e.matmul(
                            ps[:, :], lhsT=wp[:, k, j * 128:(j + 1) * 128], rhs=xTb[:, k, :], start=(k == 0), stop=(k == 7))
                            for k in range(8)], reads=[wp, xTb], writes=[ps])
                        zs = ycv.next()
                        S.op("act", lambda e, zs=zs, ps=ps: e.activation(out=zs[:, :], in_=ps[:, :], func=AF.Silu), reads=[ps], writes=[zs])
                        S.op("dve", lambda e, zs=zs, h=h: e.tensor_tensor(out=OG[:, h, :], in0=OT[:, h, :], in1=zs[:, :], op=ALU.mult),
                             reads=[OT, zs], writes=[OG])
                tok = slice(ft * 512, (ft + 1) * 512)
                for pn in range(4):
                    wp = load_panel(pn * 256, src=w_oa)
                    for j in range(2):
                        m = pn * 2 + j
                        ps = psA.next()
                        S.group("pe", [lambda e, k=k, ps=ps, wp=wp, j=j: e.matmul(
                            ps[:, :], lhsT=wp[:, k, j * 128:(j + 1) * 128], rhs=OG[:, k, :], start=(k == 0), stop=(k == 7))
                            for k in range(8)], reads=[wp, OG], writes=[ps])
                        S.op("dve", lambda e, ps=ps, m=m: e.tensor_tensor(out=XF[:, m, tok], in0=XF[:, m, tok], in1=ps[:, :], op=ALU.add),
                             reads=[ps, XFv[m][ft]], writes=[XFv[m][ft]])
                layer_norm(0, ft)
                if stage == 6:
                    stop = True
            S.barrier()
        S.stack = top


        def finish_dbg():
            allx = [XFv[k][t] for k in range(8) for t in range(5)]
            S.dma("sp", dbg_d[:, :, :], XF[:], reads=allx, owner=CST)
            S._wait("sp", CST.dkey, CST.dcnt)

        if stage < 10:
            finish_dbg()
            return nc

        XB = S.sbuf("XB", [128, 8, 2560], BF16)
        XBv = [[S.view("XB%d_%d" % (k, t), None) for t in range(5)] for k in range(8)]

        def tokc(t):
            return slice(t * 512, (t + 1) * 512)

        def make_xb(tts):
            for t in tts:
                for k in range(8):
                    if k % 2 == 0:
                        S.op("act", lambda e, k=k, t=t: e.activation(out=XB[:, k, tokc(t)], in_=XF[:, k, tokc(t)], func=AF.Copy),
                             reads=[XFv[k][t]], writes=[XBv[k][t]])
                    else:
                        S.op("dve", lambda e, k=k, t=t: e.tensor_copy(out=XB[:, k, tokc(t)], in_=XF[:, k, tokc(t)]),
                             reads=[XFv[k][t]], writes=[XBv[k][t]])

        def scale_xf(tts):
            for t in tts:
                for k in range(8):
                    S.op("dve", lambda e, k=k, t=t: e.tensor_scalar(out=XF[:, k, tokc(t)], in0=XF[:, k, tokc(t)], scalar1=ALPHA, scalar2=None, op0=ALU.mult),
                         reads=[XFv[k][t]], writes=[XFv[k][t]])

        def expert(up_src, dn_src, F, tts, gate=None, hook=None):
            nf = F // 128
            for c0 in range(0, nf, 4):
                cf = min(4, nf - c0)
                for p0 in range(c0, c0 + cf, 2):
                    npn = min(2, c0 + cf - p0)
                    wp = uppan.next()
                    S.dma("pool", wp[:, :, 0:npn * 128], up_src[:, p0 * 128:(p0 + npn) * 128].rearrange("(k p) m -> p k m", p=128), writes=[wp])
                    S.dma("pool", wp[:, :, 256:256 + npn * 128], up_src[:, F + p0 * 128:F + (p0 + npn) * 128].rearrange("(k p) m -> p k m", p=128), writes=[wp])
                    for ti, t in enumerate(tts):
                        for j in range(npn):
                            fj = p0 + j - c0
                            psg = psA.next()
                            S.group("pe", [lambda e, k=k, psg=psg, wp=wp, j=j, t=t: e.matmul(
                                psg[:, :], lhsT=wp[:, k, j * 128:(j + 1) * 128], rhs=XB[:, k, tokc(t)], start=(k == 0), stop=(k == 7))
                                for k in range(8)], reads=[wp] + [XBv[k][t] for k in range(8)], writes=[psg])
                            psu = psA.next()
                            S.group("pe", [lambda e, k=k, psu=psu, wp=wp, j=j, t=t: e.matmul(
                                psu[:, :], lhsT=wp[:, k, 256 + j * 128:256 + (j + 1) * 128], rhs=XB[:, k, tokc(t)], start=(k == 0), stop=(k == 7))
                                for k in range(8)], reads=[wp] + [XBv[k][t] for k in range(8)], writes=[psu])
                            sg = sgp.next()
                            S.op("act", lambda e, sg=sg, psg=psg: e.activation(out=sg[:, :], in_=psg[:, :], func=AF.Silu), reads=[psg], writes=[sg])
                            if gate is not None:
                                S.op("dve", lambda e, sg=sg, ti=ti: e.tensor_tensor(out=sg[:, :], in0=sg[:, :], in1=gate[0][:, ti * 512:(ti + 1) * 512], op=ALU.mult),
                                     reads=[sg, gate[1][ti]], writes=[sg])
                            S.op("dve", lambda e, sg=sg, psu=psu, fj=fj, ti=ti: e.tensor_tensor(
                                out=ACT_[:, fj, ti * 512:(ti + 1) * 512], in0=sg[:, :], in1=psu[:, :], op=ALU.mult),
                                reads=[sg, psu], writes=[ACTv[fj][ti]])
                wd = dnpan.next()
                S.dma("pool", wd[:, 0:cf, :], dn_src[c0 * 128:(c0 + cf) * 128, :].rearrange("(f p) m -> p f m", p=128), writes=[wd])
                for ti, t in enumerate(tts):
                    for m in range(8):
                        ps = psB.next()
                        S.group("pe", [lambda e, j=j, ps=ps, wd=wd, m=m, ti=ti: e.matmul(
                            ps[:, :], lhsT=wd[:, j, m * 128:(m + 1) * 128], rhs=ACT_[:, j, ti * 512:(ti + 1) * 512], start=(j == 0), stop=(j == cf - 1))
                            for j in range(cf)], reads=[wd] + [ACTv[j][ti] for j in range(cf)], writes=[ps])
                        S.op("dve", lambda e, ps=ps, m=m, t=t: e.tensor_tensor(out=XF[:, m, tokc(t)], in0=XF[:, m, tokc(t)], in1=ps[:, :], op=ALU.add),
                             reads=[ps, XFv[m][t]], writes=[XFv[m][t]])
                if c0 == 0 and hook is not None:
                    hook()

        with contextlib.ExitStack() as pb:
            S.stack = pb
            psA = Rot([S.psum("bpsA%d" % i, [128, 512], F32) for i in range(4)])
            psB = Rot([S.psum("bpsB%d" % i, [128, 512], F32) for i in range(4)])
            scr = Rot([S.sbuf("bscr%d" % i, [128, 512], F32) for i in range(4)])
            lnsq = scr
            lnst = Rot([S.sbuf("blnst%d" % i, [128, 512], F32) for i in range(3)])
            uppan = Rot([S.sbuf("buppan%d" % i, [128, 8, 512], BF16) for i in range(2)])
            dnpan = Rot([S.sbuf("bdnpan%d" % i, [128, 4, 1024], BF16) for i in range(2)])
            sgp = Rot([S.sbuf("bsg%d" % i, [128, 512], F32) for i in range(2)])
            ACT_ = S.sbuf("bact", [128, 4, 2560], BF16)
            ACTv = [[S.view("bact%d_%d" % (j, t), None) for t in range(5)] for j in range(4)]
            make_xb(range(5))
            scale_xf(range(5))
            expert(ffn_up, ffn_dn, 2816, list(range(5)))
            for t in range(5):
                layer_norm(1, t, xb_t=XB, xbv=XBv)
            S.barrier()
        S.stack = top
        if stage == 10:
            finish_dbg()
            return nc

        with contextlib.ExitStack() as pc:
            S.stack = pc
            scale_xf(range(1, 5))
            psS = Rot([S.psum("cpsS%d" % i, [128, 1024], F32) for i in range(3)])
            psT_t = S.psum("cpsT", [128, 1024], BF16)
            psTv = S.view("cpsTv", psT_t[:, 0:640])
            psTv.psum = True
            psO1 = S.psum("cpsO", [128, 512], F32)
            psO = psS
            cpan = Rot([S.sbuf("cpan%d" % i, [128, 8, 256], BF16) for i in range(3)])
            KT2 = S.sbuf("KT2", [128, 2, 2560], BF16)
            QT2 = S.sbuf("QT2", [128, 2, 2048], BF16)
            VT = S.sbuf("VT", [128, 20, 256], BF16)
            BM = S.sbuf("BM", [128, 4, 640], BF16)
            WM = S.sbuf("WM", [128, 640], BF16)
            KV1 = S.sbuf("KV1", [1, 2560], BF16)
            ONE1 = S.sbuf("ONE1", [1, 128], BF16)
            pexp = Rot([S.sbuf("cpe%d" % i, [128, 640], BF16) for i in range(3)])
            PTs = Rot([S.sbuf("cPT%d" % i, [128, 5, 128], BF16) for i in range(2)])
            OTM = Rot([S.sbuf("cOTM%d" % i, [128, 256], BF16) for i in range(2)])
            OTF = S.sbuf("cOTF", [128, 2, 2048], BF16)
            wob = S.sbuf("cwob", [128, 2, 1024], BF16)
            st4 = Rot([S.sbuf("cst%d" % i, [128, 4], F32) for i in range(6)])
            S.dma("pool", WM[:], wmask_d[:, :], writes=[WM])
            S.dma("pool", KV1[:], kvalid_d[:, :], writes=[KV1])
            S.op("dve", lambda e: e.memset(ONE1[:], 1.0), writes=[ONE1])
            for hg in range(4):
                wk = cpan.next()
                S.dma("pool", wk[:], kv_w[:, hg * 256:(hg + 1) * 256].rearrange("(k p) m -> p k m", p=128), writes=[wk])
                wv = cpan.next()
                S.dma("pool", wv[:], kv_w[:, 1024 + hg * 256:1024 + (hg + 1) * 256].rearrange("(k p) m -> p k m", p=128), writes=[wv])
                wq = cpan.next()
                S.dma("pool", wq[:], w_q[:, hg * 256:(hg + 1) * 256].rearrange("(k p) m -> p k m", p=128), writes=[wq])
                S.dma("pool", wob[:], w_ob[hg * 256:(hg + 1) * 256, :].rearrange("(f p) m -> p f m", p=128), writes=[wob])
                S.dma("pool", BM[:], biasT[hg * 4:(hg + 1) * 4, :, :].rearrange("a q k -> q a k"), writes=[BM])
                S.op("dve", lambda e: e.tensor_tensor(out=BM[:, :, :], in0=BM[:, :, :], in1=WM[:, :].unsqueeze(1).to_broadcast([128, 4, 640]), op=ALU.add),
                     reads=[BM, WM], writes=[BM])
                for t in range(5):
                    for pi in range(2):
                        ps = psO.next()
                        S.group("pe", [lambda e, k=k, ps=ps, pi=pi, t=t: e.matmul(ps[:, 0:512], lhsT=wk[:, k, pi * 128:(pi + 1) * 128], rhs=XB[:, k, tokc(t)],
                                                                                start=(k == 0), stop=(k == 7)) for k in range(8)],
                                reads=[wk] + [XBv[k][t] for k in range(8)], writes=[ps])
                        S.op("act", lambda e, ps=ps, pi=pi, t=t: e.activation(out=KT2[:, pi, tokc(t)], in_=ps[:, 0:512], func=AF.Copy), reads=[ps], writes=[KT2])
                        if t >= 1:
                            ps = psO.next()
                            S.group("pe", [lambda e, k=k, ps=ps, pi=pi, t=t: e.matmul(ps[:, 0:512], lhsT=wq[:, k, pi * 128:(pi + 1) * 128], rhs=XB[:, k, tokc(t)],
                                                                                    start=(k == 0), stop=(k == 7)) for k in range(8)],
                                    reads=[wq] + [XBv[k][t] for k in range(8)], writes=[ps])
                            S.op("act", lambda e, ps=ps, pi=pi, t=t: e.activation(out=QT2[:, pi, tokc(t - 1)], in_=ps[:, 0:512], func=AF.Copy, scale=0.125),
                                 reads=[ps], writes=[QT2])
                    for q4 in range(4):
                        kt = t * 4 + q4
                        ps = psO.next()
                        S.group("pe", [lambda e, k=k, ps=ps, kt=kt: e.matmul(ps[:, 0:256], lhsT=XB[:, k, kt * 128:(kt + 1) * 128], rhs=wv[:, k, :],
                                                                            start=(k == 0), stop=(k == 7)) for k in range(8)],
                                reads=[wv] + [XBv[k][t] for k in range(8)], writes=[ps])
                        S.op("dve", lambda e, ps=ps, kt=kt: e.tensor_copy(out=VT[:, kt, :], in_=ps[:, 0:256]), reads=[ps], writes=[VT])
                chains = [(T, a) for T in range(16) for a in range(4)]
                cst_ = {}
                otms = {}

                def s1(T, a):
                    pi = a // 2
                    pr = slice((a % 2) * 64, (a % 2) * 64 + 64)
                    ps2 = psS.next()
                    qs = slice(T * 128, (T + 1) * 128)
                    fns = []
                    for (c0, c1) in ((0, 512), (512, 640)):
                        ks = slice(T * 128 + c0, T * 128 + c1)
                        fns.append(lambda e, c0=c0, c1=c1, ks=ks: e.matmul(ps2[:, c0:c1], lhsT=QT2[pr, pi, qs], rhs=KT2[pr, pi, ks], start=True, stop=False))
                        if T < 4:
                            fns.append(lambda e, c0=c0, c1=c1, ks=ks: e.matmul(ps2[:, c0:c1], lhsT=ONE1[0:1, :], rhs=KV1[0:1, ks], start=False, stop=False))
                        fns.append(lambda e, c0=c0, c1=c1: e.matmul(ps2[:, c0:c1], lhsT=IDB, rhs=BM[:, a, c0:c1], start=False, stop=True))
                    S.group("pe", fns, reads=[QT2, KT2, ONE1, KV1, BM, CST], writes=[ps2])
                    stt_ = st4.next()
                    S.op("dve", lambda e: e.tensor_reduce(out=stt_[:, 1:2], in_=ps2[:, 0:640], axis=AX.X, op=ALU.max, negate=True), reads=[ps2], writes=[stt_])
                    pe_ = pexp.next()
                    S.op("act", lambda e: e.activation(out=pe_[:, :], in_=ps2[:, 0:640], func=AF.Exp, bias=stt_[:, 1:2], scale=1.0, accum_out=stt_[:, 2:3]),
                         reads=[ps2, stt_], writes=[pe_, stt_])
                    S.op("dve", lambda e: e.reciprocal(out=stt_[:, 3:4], in_=stt_[:, 2:3]), reads=[stt_], writes=[stt_])
                    cst_[(T, a)] = [pe_, stt_, None]

                def s2(T, a):
                    pe_, stt_, _ = cst_[(T, a)]
                    S.group("pe", [lambda e, kt=kt: e.transpose(out=psTv[:, kt * 128:(kt + 1) * 128], in_=pe_[:, kt * 128:(kt + 1) * 128], identity=IDB)
                                   for kt in range(5)], reads=[pe_, CST], writes=[psTv])
                    pt = PTs.next()
                    S.op("dve", lambda e: e.tensor_copy(out=pt[:, :, :].rearrange("p k q -> p (k q)"), in_=psTv[:, :]), reads=[psTv], writes=[pt])
                    cst_[(T, a)][2] = pt

                def s3(T, a):
                    pe_, stt_, pt = cst_.pop((T, a))
                    if a == 0:
                        otms[T] = OTM.next()
                    otm = otms[T]
                    pso = psO1
                    S.group("pe", [lambda e, kt=kt: e.matmul(pso[:, 0:64], lhsT=pt[:, kt, :], rhs=VT[:, T + kt, a * 64:(a + 1) * 64], start=(kt == 0), stop=(kt == 4))
                                   for kt in range(5)], reads=[pt, VT], writes=[pso])
                    S.op("act", lambda e: e.activation(out=otm[:, a * 64:(a + 1) * 64], in_=pso[:, 0:64], func=AF.Copy, scale=stt_[:, 3:4]),
                         reads=[pso, stt_], writes=[otm])
                    if a == 3:
                        S.group("pe", [lambda e, j=j: e.transpose(out=psTv[:, j * 128:(j + 1) * 128], in_=otm[:, j * 128:(j + 1) * 128], identity=IDB)
                                       for j in range(2)], reads=[otm, CST], writes=[psTv])
                        S.op("act", lambda e: e.activation(out=OTF[:, :, T * 128:(T + 1) * 128], in_=psTv[:, 0:256].rearrange("p (j q) -> p j q", j=2), func=AF.Copy),
                             reads=[psTv], writes=[OTF])

                s1(*chains[0])
                s1(*chains[1])
                for ci in range(len(chains)):
                    s2(*chains[ci])
                    if ci + 2 < len(chains):
                        s1(*chains[ci + 2])
                    s3(*chains[ci])
                for t in range(1, 5):
                    for m in range(8):
                        ps = psO.next()
                        S.group("pe", [lambda e, j=j, ps=ps, m=m, t=t: e.matmul(ps[:, 0:512], lhsT=wob[:, j, m * 128:(m + 1) * 128], rhs=OTF[:, j, tokc(t - 1)],
                                                                               start=(j == 0), stop=(j == 1)) for j in range(2)], reads=[wob, OTF], writes=[ps])
                        S.op("dve", lambda e, ps=ps, m=m, t=t: e.tensor_tensor(out=XF[:, m, tokc(t)], in0=XF[:, m, tokc(t)], in1=ps[:, 0:512], op=ALU.add),
                             reads=[ps, XFv[m][t]], writes=[XFv[m][t]])
            S.barrier()
        S.stack = top

        with contextlib.ExitStack() as pd:
            S.stack = pd
            psA = Rot([S.psum("dpsA%d" % i, [128, 512], F32) for i in range(4)])
            psB = Rot([S.psum("dpsB%d" % i, [128, 512], F32) for i in range(4)])
            scr = Rot([S.sbuf("dscr%d" % i, [128, 512], F32) for i in range(4)])
            lnsq = scr
            lnst = Rot([S.sbuf("dlnst%d" % i, [128, 512], F32) for i in range(3)])
            for t in range(1, 5):
                layer_norm(2, t, xb_t=XB, xbv=XBv)
            if stage == 11:
                S.barrier()
                S.stack = top
                finish_dbg()
                return nc
            uppan = Rot([S.sbuf("duppan%d" % i, [128, 8, 512], BF16) for i in range(2)])
            dnpan = Rot([S.sbuf("ddnpan%d" % i, [128, 4, 1024], BF16) for i in range(2)])
            sgp = Rot([S.sbuf("dsg%d" % i, [128, 512], F32) for i in range(2)])
            ACT_ = S.sbuf("dact", [128, 4, 2048], BF16)
            ACTv = [[S.view("dact%d_%d" % (j, t), None) for t in range(4)] for j in range(4)]
            GB = S.sbuf("GB", [128, 2048], F32)
            GBv = [S.view("GB%d" % t, None) for t in range(4)]
            GTM = S.sbuf("GTM", [128, 16, 8], F32)
            WR = S.sbuf("WR", [128, 8, 8], F32)
            rt = Rot([S.sbuf("drt%d" % i, [128, 8], F32) for i in range(6)])
            rs = Rot([S.sbuf("drs%d" % i, [128, 8], F32) for i in range(4)])
            Dg = Rot([S.sbuf("dDg%d" % i, [128, 128], F32) for i in range(2)])
            S.dma("sp", WR[:], router_d.rearrange("(k p) e -> p k e", p=128), writes=[WR])
            for i in range(16):
                t, q4 = 1 + i // 4, i % 4
                ts_ = slice(t * 512 + q4 * 128, t * 512 + (q4 + 1) * 128)
                ps = psA.next()
                S.group("pe", [lambda e, k=k, ps=ps, ts_=ts_: e.matmul(ps[:, 0:8], lhsT=XF[:, k, ts_], rhs=WR[:, k, :], start=(k == 0), stop=(k == 7))
                               for k in range(8)], reads=[WR] + [XFv[k][t] for k in range(8)], writes=[ps])
                lg = rt.next()
                S.op("act", lambda e, lg=lg, ps=ps: e.activation(out=lg[:, :], in_=ps[:, 0:8], func=AF.Copy), reads=[ps], writes=[lg])
                sc_ = rs.next()
                S.op("dve", lambda e, lg=lg, sc_=sc_: e.tensor_reduce(out=sc_[:, 0:1], in_=lg[:, :], axis=AX.X, op=ALU.max), reads=[lg], writes=[sc_])
                eq1 = rt.next()
                S.op("dve", lambda e, lg=lg, sc_=sc_, eq1=eq1: e.tensor_scalar(out=eq1[:, :], in0=lg[:, :], scalar1=sc_[:, 0:1], scalar2=None, op0=ALU.is_equal),
                     reads=[lg, sc_], writes=[eq1])
                l2 = rt.next()
                S.op("dve", lambda e, lg=lg, eq1=eq1, l2=l2: e.scalar_tensor_tensor(out=l2[:, :], in0=eq1[:, :], scalar=-1e30, in1=lg[:, :], op0=ALU.mult, op1=ALU.add),
                     reads=[lg, eq1], writes=[l2])
                S.op("dve", lambda e, l2=l2, sc_=sc_: e.tensor_reduce(out=sc_[:, 1:2], in_=l2[:, :], axis=AX.X, op=ALU.max), reads=[l2, sc_], writes=[sc_])
                eq2 = rt.next()
                S.op("dve", lambda e, l2=l2, sc_=sc_, eq2=eq2: e.tensor_scalar(out=eq2[:, :], in0=l2[:, :], scalar1=sc_[:, 1:2], scalar2=None, op0=ALU.is_equal),
                     reads=[l2, sc_], writes=[eq2])
                S.op("dve", lambda e, sc_=sc_: e.tensor_tensor(out=sc_[:, 2:3], in0=sc_[:, 1:2], in1=sc_[:, 0:1], op=ALU.subtract), reads=[sc_], writes=[sc_])
                S.op("act", lambda e, sc_=sc_: e.activation(out=sc_[:, 2:3], in_=sc_[:, 2:3], func=AF.Exp), reads=[sc_], writes=[sc_])
                S.op("dve", lambda e, sc_=sc_: e.tensor_scalar(out=sc_[:, 3:4], in0=sc_[:, 2:3], scalar1=1.0, scalar2=None, op0=ALU.add), reads=[sc_], writes=[sc_])
                S.op("dve", lambda e, sc_=sc_: e.reciprocal(out=sc_[:, 3:4], in_=sc_[:, 3:4]), reads=[sc_], writes=[sc_])
                S.op("dve", lambda e, sc_=sc_: e.tensor_tensor(out=sc_[:, 4:5], in0=sc_[:, 2:3], in1=sc_[:, 3:4], op=ALU.mult), reads=[sc_], writes=[sc_])
                S.op("dve", lambda e, eq1=eq1, sc_=sc_: e.tensor_scalar(out=eq1[:, :], in0=eq1[:, :], scalar1=sc_[:, 3:4], scalar2=None, op0=ALU.mult),
                     reads=[eq1, sc_], writes=[eq1])
                S.op("dve", lambda e, eq1=eq1, eq2=eq2, sc_=sc_, i=i: e.scalar_tensor_tensor(out=GTM[:, i, :], in0=eq2[:, :], scalar=sc_[:, 4:5], in1=eq1[:, :],
                                                                                           op0=ALU.mult, op1=ALU.add), reads=[eq1, eq2, sc_], writes=[GTM])
            scale_xf(range(1, 5))
            GB2 = S.sbuf("GB2", [128, 2048], F32)
            GBs = [GB, GB2]
            GBvs = [GBv, [S.view("GB2_%d" % t, None) for t in range(4)]]

            def make_gb(ex):
                gb, gbv = GBs[ex % 2], GBvs[ex % 2]
                for t4 in range(4):
                    ps = psA.next()
                    for q4 in range(4):
                        i = t4 * 4 + q4
                        dg = Dg.next()
                        S.op("dve", lambda e, dg=dg, i=i: e.tensor_scalar(out=dg[:, :], in0=IDF, scalar1=GTM[:, i, ex:ex + 1], scalar2=None, op0=ALU.mult),
                             reads=[GTM, CST], writes=[dg])
                        S.op("pe", lambda e, dg=dg, ps=ps, q4=q4: e.matmul(ps[:, q4 * 128:(q4 + 1) * 128], lhsT=ONEF, rhs=dg[:, :], start=True, stop=True),
                             reads=[dg, CST, ps], writes=[ps])
                    S.op("act", lambda e, ps=ps, t4=t4: e.activation(out=gb[:, t4 * 512:(t4 + 1) * 512], in_=ps[:, :], func=AF.Copy), reads=[ps], writes=[gbv[t4]])

            make_gb(0)
            for ex in range(8):
                hook = (lambda ex=ex: make_gb(ex + 1)) if ex < 7 else None
                expert(moe_up[ex], moe_dn[ex], 3584, [1, 2, 3, 4], gate=(GBs[ex % 2], GBvs[ex % 2]), hook=hook)
            for t in range(1, 5):
                layer_norm(3, t)
            S.barrier()
            xo = Rot([S.view("dxo%d" % i, GB[:, i * 1024:(i + 1) * 1024]) for i in range(2)])
            OUTB = S.view("OUTB", None)
            for i in range(16):
                t, q4 = 1 + i // 4, i % 4
                ts_ = slice(t * 512 + q4 * 128, t * 512 + (q4 + 1) * 128)
                xb_ = xo.next()
                for half in range(2):
                    ps = psA.next()
                    S.group("pe", [lambda e, j=j, ps=ps, half=half, ts_=ts_: e.transpose(out=ps[:, j * 128:(j + 1) * 128], in_=XF[:, half * 4 + j, ts_], identity=IDF)
                                   for j in range(4)], reads=[CST] + [XFv[k][t] for k in range(half * 4, half * 4 + 4)], writes=[ps])
                    if half == 0:
                        S.op("act", lambda e, ps=ps, xb_=xb_: e.activation(out=xb_[:, 0:512], in_=ps[:, :], func=AF.Copy), reads=[ps], writes=[xb_])
                    else:
                        S.op("dve", lambda e, ps=ps, xb_=xb_: e.tensor_copy(out=xb_[:, 512:1024], in_=ps[:, :]), reads=[ps], writes=[xb_])
                S.dma("sp", out_d[i * 128:(i + 1) * 128, :], xb_[:, :], reads=[xb_], owner=xb_)
            S.barrier()
        S.stack = top
        S.barrier()
    return nc


def _consts():
    i = np.arange(128)
    same = (i[:, None] // 64) == (i[None, :] // 64)
    ident = np.eye(128, dtype=np.float32)
    tri = (same & (i[:, None] <= i[None, :])).astype(np.float32)
    blk = same.astype(np.float32)
    mbl = np.where(same & (i[None, :] < i[:, None]), 0.0, -1e5).astype(np.float32)
    mbu = np.where(same & (i[None, :] >= i[:, None]), 0.0, -1e5).astype(np.float32)
    ones = np.ones((128, 128), np.float32)
    return np.ascontiguousarray(np.stack([ident, tri, blk, mbl, mbu, ones], axis=1))


def make_in_maps(inputs):
    f = lambda a: np.ascontiguousarray(np.asarray(a, dtype=np.float32))
    x = f(inputs["x"])
    q = np.arange(128)[:, None]
    k = np.arange(640)[None, :]
    idx = np.clip(512 + q - k, -128, 128) + 128
    biasT = f(np.asarray(inputs["b_rel_bias"])[0][:, idx])
    cq = q // 64
    wmask = np.where((k >= 64 * cq) & (k < 64 * cq + 576), 0.0, NEG).astype(np.float32)
    lng = np.stack([np.asarray(inputs[n])[l].reshape(8, 128).T for (n, l) in
                    (("ln1_g", 0), ("ln2_g", 0), ("ln1_g", 1), ("ln2_g", 1))], axis=1)
    lnb = np.stack([np.asarray(inputs[n])[l].reshape(8, 128).T for (n, l) in
                    (("ln1_b", 0), ("ln2_b", 0), ("ln1_b", 1), ("ln2_b", 1))], axis=1)
    shared = {
        "w_in": f(inputs["a_w_in"][0]),
        "cw": f(np.asarray(inputs["a_conv_w"])[0].reshape(4, 24, 128).transpose(2, 1, 0)),
        "alog": f(np.broadcast_to(np.asarray(inputs["a_A_log"])[0][None, :], (128, 8))),
        "dtb": f(np.broadcast_to(np.asarray(inputs["a_dt_bias"])[0][None, :], (128, 8))),
        "normg": f(np.asarray(inputs["a_norm_g"])[0].reshape(128, 1)),
        "w_oa": f(inputs["a_w_o"][0]),
        "kv_w": f(inputs["kv_w"]),
        "w_q": f(inputs["b_w_q"][0]),
        "biasT": biasT,
        "wmask": wmask,
        "w_ob": f(inputs["b_w_o"][0]),
        "ffn_up": f(inputs["ffn_w_up"][0]),
        "ffn_dn": f(inputs["ffn_w_down"][0]),
        "router": f(inputs["moe_router"][0]),
        "moe_up": f(inputs["moe_w_up"][0]),
        "moe_dn": f(inputs["moe_w_down"][0]),
        "lng": f(lng),
        "lnb": f(lnb),
        "cmat": _consts(),
    }
    maps = []
    for c in range(8):
        b, half = c // 2, c % 2
        if half == 0:
            xw = np.concatenate([np.zeros((2048, 1024), np.float32), x[b, 0:2048]], axis=0)
            kvalid = np.concatenate([np.full((1, 512), NEG, np.float32), np.zeros((1, 2048), np.float32)], axis=1)
        else:
            xw = x[b]
            kvalid = np.zeros((1, 2560), np.float32)
        m = dict(shared)
        m["x_win"] = np.ascontiguousarray(xw)
        m["kvalid"] = kvalid
        maps.append(m)
    return maps


def kernel(**inputs):
    nc = build()
    maps = make_in_maps(inputs)
    res = run_bass_kernel_spmd(nc, maps, core_ids=list(range(8)))
    out = np.zeros((4, 4096, 1024), np.float32)
    for c in range(8):
        b, half = c // 2, c % 2
        out[b, half * 2048:(half + 1) * 2048] = res.results[c]["out"]
    return out
```

```python
import contextlib
import numpy as np
import concourse.bass as bass
import concourse.mybir as mybir
from concourse.bass_utils import run_bass_kernel_spmd

F32 = mybir.dt.float32
BF16 = mybir.dt.bfloat16
ALU = mybir.AluOpType
AF = mybir.ActivationFunctionType
AX = mybir.AxisListType

ALPHA = 2.0 ** 0.5
EPS = 1e-6
NEG = -30000.0


class Buf:
    __slots__ = ("name", "t", "w", "r", "dsem", "dcnt", "dkey", "psum")

    def __init__(self, name, t):
        self.name = name
        self.t = t
        self.w = None
        self.r = []
        self.dsem = None
        self.dcnt = 0
        self.dkey = None
        self.psum = False

    def __getitem__(self, k):
        return self.t[k]


class Sched:
    ENG = ("pe", "act", "dve", "pool", "sp")

    def __init__(self, nc, stack):
        self.nc = nc
        self.stack = stack
        self.e = {"pe": nc.tensor, "act": nc.scalar, "dve": nc.vector,
                  "pool": nc.gpsimd, "sp": nc.sync}
        self.sem = {}
        self.cnt = {}
        for k in self.ENG:
            self.sem[k] = stack.enter_context(nc.semaphore("s_" + k))
            self.cnt[k] = 0
        self.seen = {k: {} for k in self.ENG}
        self.nbuf = 0
        self.dbufs = []
        self.in_barrier = False

    def sbuf(self, name, shape, dtype):
        return Buf(name, self.stack.enter_context(self.nc.sbuf_tensor("sb_" + name, list(shape), dtype)))

    def psum(self, name, shape, dtype):
        b = Buf(name, self.stack.enter_context(self.nc.psum_tensor("ps_" + name, list(shape), dtype)))
        b.psum = True
        return b

    def view(self, name, ap):
        return Buf(name, ap)

    def _dsem(self, b):
        if b.dsem is None:
            self.nbuf += 1
            b.dsem = self.stack.enter_context(self.nc.semaphore("d%d" % self.nbuf))
            b.dkey = ("d", self.nbuf)
            self.sem[b.dkey] = b.dsem
            self.dbufs.append(b)
        return b.dkey

    def _wait(self, eng, key, val):
        if val <= 0:
            return
        if eng == "pe" and key == "pe" and not self.in_barrier:
            return
        seen = self.seen[eng]
        if seen.get(key, 0) >= val:
            return
        seen[key] = val
        self.e[eng].wait_ge(self.sem[key], val)

    def _deps(self, eng, reads, writes):
        for b in reads:
            if b.w is not None:
                self._wait(eng, b.w[0], b.w[1])
            if b.psum:
                for (k, v) in b.r:
                    if k != eng:
                        self._wait(eng, k, v)
        for b in writes:
            if b.w is not None:
                self._wait(eng, b.w[0], b.w[1])
            for (k, v) in b.r:
                self._wait(eng, k, v)

    def _mark(self, key, val, reads, writes):
        for b in writes:
            b.w = (key, val)
            b.r = []
        for b in reads:
            if b in writes:
                continue
            b.r = [(k, v) for (k, v) in b.r if k != key]
            b.r.append((key, val))

    def op(self, eng, fn, reads=(), writes=()):
        self._deps(eng, reads, writes)
        ins = fn(self.e[eng])
        self.cnt[eng] += 1
        ins.then_inc(self.sem[eng], 1)
        self._mark(eng, self.cnt[eng], reads, writes)
        return ins

    def group(self, eng, fns, reads=(), writes=()):
        self._deps(eng, reads, writes)
        ins = None
        for fn in fns:
            ins = fn(self.e[eng])
        self.cnt[eng] += 1
        ins.then_inc(self.sem[eng], 1)
        self._mark(eng, self.cnt[eng], reads, writes)
        return ins

    def dma(self, q, out_ap, in_ap, reads=(), writes=(), owner=None):
        self._deps(q, reads, writes)
        if owner is None:
            owner = writes[0] if writes else reads[0]
        key = self._dsem(owner)
        ins = self.e[q].dma_start(out=out_ap, in_=in_ap)
        ins.then_inc(self.sem[key], 16)
        owner.dcnt += 16
        self._mark(key, owner.dcnt, reads, writes)
        return ins

    def barrier(self):
        self.in_barrier = True
        for eng in self.ENG:
            for k in self.ENG:
                self._wait(eng, k, self.cnt[k])
            for b in self.dbufs:
                self._wait(eng, b.dkey, b.dcnt)
        self.in_barrier = False

    def drop_dbufs(self, keep):
        self.dbufs = [b for b in self.dbufs if b in keep]


class Rot:
    def __init__(self, bufs):
        self.bufs = bufs
        self.i = 0

    def next(self):
        b = self.bufs[self.i % len(self.bufs)]
        self.i += 1
        return b


def build(stage=99):
    nc = bass.Bass("TRN2", target_bir_lowering=False)

    def din(name, shape, dtype=F32):
        return nc.dram_tensor(name, list(shape), dtype, kind="ExternalInput").ap()

    x_win = din("x_win", [4096, 1024])
    w_in = din("w_in", [1024, 4112])
    cw_d = din("cw", [128, 24, 4])
    alog_d = din("alog", [128, 8])
    dtb_d = din("dtb", [128, 8])
    normg_d = din("normg", [128, 1])
    w_oa = din("w_oa", [1024, 1024])
    kv_w = din("kv_w", [1024, 2048])
    w_q = din("w_q", [1024, 1024])
    biasT = din("biasT", [16, 128, 640])
    wmask_d = din("wmask", [128, 640])
    kvalid_d = din("kvalid", [1, 2560])
    w_ob = din("w_ob", [1024, 1024])
    ffn_up = din("ffn_up", [1024, 5632])
    ffn_dn = din("ffn_dn", [2816, 1024])
    router_d = din("router", [1024, 8])
    if stage >= 20:
        moe_up = din("moe_up", [8, 1024, 7168])
        moe_dn = din("moe_dn", [8, 3584, 1024])
    lng_d = din("lng", [128, 4, 8])
    lnb_d = din("lnb", [128, 4, 8])
    cmat_d = din("cmat", [128, 6, 128])
    out_d = nc.dram_tensor("out", [2048, 1024], F32, kind="ExternalOutput").ap()
    dbg_d = nc.dram_tensor("dbg", [128, 8, 2560], F32, kind="ExternalOutput").ap() if stage < 20 else None

    with contextlib.ExitStack() as top:
        S = Sched(nc, top)
        XF = S.sbuf("XF", [128, 8, 2560], F32)
        CM = S.sbuf("CM", [128, 6, 128], F32)
        CMB = S.sbuf("CMB", [128, 2, 128], BF16)
        LNG = S.sbuf("LNG", [128, 4, 8], F32)
        LNB = S.sbuf("LNB", [128, 4, 8], F32)
        CST = S.view("CST", None)
        S.dma("sp", CM[:], cmat_d[:, :, :], writes=[CST])
        S.dma("sp", LNG[:], lng_d[:, :, :], writes=[CST], owner=CST)
        S.dma("sp", LNB[:], lnb_d[:, :, :], writes=[CST], owner=CST)
        S.op("dve", lambda e: e.tensor_copy(out=CMB[:, 0, :], in_=CM[:, 0, :]), reads=[CST], writes=[CST])
        S.op("dve", lambda e: e.tensor_copy(out=CMB[:, 1, :], in_=CM[:, 5, :]), reads=[CST], writes=[CST])
        if stage < 20:
            S.op("dve", lambda e: e.memset(XF[:], 0.0), writes=[])
        IDF, TRIF, BLKF, MBL, MBU, ONEF = (CM[:, i, :] for i in range(6))
        IDB, ONEB = CMB[:, 0, :], CMB[:, 1, :]
        XFv = [[S.view("XF%d_%d" % (k, t), None) for t in range(5)] for k in range(8)]

        def layer_norm(li, tt, xb_t=None, xbv=None):
            tok = slice(tt * 512, (tt + 1) * 512)
            ps_s = psB.next()
            ps_q = psB.next()
            fns = []
            S.group("pe", [lambda e, k=k: e.matmul(ps_s[:, :], lhsT=ONEF, rhs=XF[:, k, tok], start=(k == 0), stop=(k == 7))
                           for k in range(8)], reads=[CST] + [XFv[k][tt] for k in range(8)], writes=[ps_s])
            sqs = []
            for k in range(8):
                sq = lnsq.next()
                S.op("act", lambda e, sq=sq, k=k: e.activation(out=sq[:, :], in_=XF[:, k, tok], func=AF.Square),
                     reads=[XFv[k][tt]], writes=[sq])
                S.op("pe", lambda e, sq=sq, k=k: e.matmul(ps_q[:, :], lhsT=ONEF, rhs=sq[:, :], start=(k == 0), stop=(k == 7)),
                     reads=[CST, sq], writes=[ps_q])
            mean = lnst.next()
            rstd = lnst.next()
            S.op("act", lambda e: e.activation(out=mean[:, :], in_=ps_s[:, :], func=AF.Copy, scale=1.0 / 1024),
                 reads=[ps_s], writes=[mean])
            m2 = lnsq.next()
            S.op("dve", lambda e: e.tensor_tensor(out=m2[:, :], in0=mean[:, :], in1=mean[:, :], op=ALU.mult),
                 reads=[mean], writes=[m2])
            S.op("dve", lambda e: e.scalar_tensor_tensor(out=rstd[:, :], in0=ps_q[:, :], scalar=1.0 / 1024, in1=m2[:, :],
                                                        op0=ALU.mult, op1=ALU.subtract), reads=[ps_q, m2], writes=[rstd])
            S.op("act", lambda e: e.activation(out=rstd[:, :], in_=rstd[:, :], func=AF.Ln, bias=EPS, scale=1.0),
                 reads=[rstd], writes=[rstd])
            S.op("act", lambda e: e.activation(out=rstd[:, :], in_=rstd[:, :], func=AF.Exp, scale=-0.5),
                 reads=[rstd], writes=[rstd])
            for k in range(8):
                tmp = lnsq.next()
                S.op("dve", lambda e, k=k, tmp=tmp: e.tensor_tensor(out=tmp[:, :], in0=XF[:, k, tok], in1=mean[:, :], op=ALU.subtract),
                     reads=[XFv[k][tt], mean], writes=[tmp])
                S.op("dve", lambda e, tmp=tmp: e.tensor_tensor(out=tmp[:, :], in0=tmp[:, :], in1=rstd[:, :], op=ALU.mult),
                     reads=[tmp, rstd], writes=[tmp])
                S.op("act", lambda e, k=k, tmp=tmp: e.activation(out=XF[:, k, tok], in_=tmp[:, :], func=AF.Identity,
                                                                scale=LNG[:, li, k:k + 1], bias=LNB[:, li, k:k + 1]),
                     reads=[tmp, CST], writes=[XFv[k][tt]])
                if xb_t is not None:
                    S.op("dve", lambda e, k=k: e.tensor_copy(out=xb_t[:, k, tok], in_=XF[:, k, tok]),
                         reads=[XFv[k][tt]], writes=[xbv[k][tt]])

        with contextlib.ExitStack() as pa:
            S.stack = pa
            psA = Rot([S.psum("psA%d" % i, [128, 512], F32) for i in range(3)])
            psB = Rot([S.psum("psB%d" % i, [128, 512], F32) for i in range(2)])
            psW = S.psum("psW", [128, 1024], F32)
            psT_t = S.psum("psT", [128, 1024], BF16)
            psTv = S.view("psTv", psT_t[:, 0:512])
            psTv.psum = True
            psT = Rot([psTv])
            scr = Rot([S.sbuf("scr%d" % i, [128, 512], F32) for i in range(4)])
            lnsq = scr
            lnst = Rot([S.sbuf("lnst%d" % i, [128, 512], F32) for i in range(2)])
            xtm = Rot([S.sbuf("xtm%d" % i, [128, 1024], F32) for i in range(2)])
            xTb = S.sbuf("xTb", [128, 8, 512], BF16)
            wpan = Rot([S.sbuf("wpan%d" % i, [128, 8, 256], BF16) for i in range(3)])
            wba = S.sbuf("wba", [128, 8, 16], BF16)
            qT = S.sbuf("qT", [128, 8, 512], BF16)
            kT = S.sbuf("kT", [128, 8, 512], BF16)
            vT = S.sbuf("vT", [128, 8, 512], BF16)
            OT = vT
            OG = qT
            halo = S.sbuf("halo", [128, 24, 3], F32)
            cwt = S.sbuf("cwt", [128, 24, 4], F32)
            ubuf = Rot([S.sbuf("ubuf%d" % i, [128, 515], F32) for i in range(2)])
            ycv = scr
            sqb = Rot([S.sbuf("sqb%d" % i, [128, 512], BF16) for i in range(1)])
            alog = S.sbuf("alog", [128, 8], F32)
            dtb = S.sbuf("dtb", [128, 8], F32)
            negA = S.sbuf("negA", [128, 8], F32)
            normg = S.sbuf("normg", [128, 1], F32)
            PB = []
            for par in range(2):
                PB.append((S.sbuf("ba%d" % par, [128, 16], F32), S.sbuf("beta%d" % par, [128, 8], F32), S.sbuf("gt%d" % par, [128, 8], F32),
                           S.sbuf("gcum%d" % par, [128, 8], F32), S.sbuf("glast%d" % par, [128, 8], F32), S.sbuf("bexp%d" % par, [128, 8], F32),
                           S.sbuf("kdsc%d" % par, [128, 8], F32), S.sbuf("DECB%d" % par, [128, 8, 128], BF16), S.sbuf("DECT%d" % par, [128, 8, 128], BF16),
                           S.sbuf("EROW%d" % par, [128, 8, 128], BF16), S.sbuf("GLS%d" % par, [128, 8, 2], F32)))
            tmpW = S.sbuf("tmpW", [128, 8, 128], F32)
            Dm = tmpW
            Sf = [S.sbuf("Sf%d" % g, [128, 4, 128], F32) for g in range(2)]
            Sb = [S.sbuf("Sb%d" % g, [128, 4, 128], BF16) for g in range(2)]
            ppc = [[S.sbuf("pp%d_%d" % (g, i), [128, 4, 128], BF16) for i in range(4)] for g in range(2)]
            gbufc = [{n: S.sbuf("g%d_%s" % (g, n), [128, 4, 128], BF16) for n in ("Tt", "Xw", "kd", "Xu", "wT", "QKT", "qd", "VN0", "VN1")} for g in range(2)]
            ufc = [S.sbuf("uf%d" % g, [128, 4, 128], F32) for g in range(2)]

            def run_chains(gens):
                gens = list(gens)
                while gens:
                    for gc_ in list(gens):
                        try:
                            next(gc_)
                        except StopIteration:
                            gens.remove(gc_)

            S.dma("sp", cwt[:], cw_d[:, :, :], writes=[CST], owner=CST)
            S.dma("sp", alog[:], alog_d[:, :], writes=[CST], owner=CST)
            S.dma("sp", dtb[:], dtb_d[:, :], writes=[CST], owner=CST)
            S.dma("sp", normg[:], normg_d[:, :], writes=[CST], owner=CST)
            S.dma("pool", wba[:], w_in[:, 4096:4112].rearrange("(k p) m -> p k m", p=128), writes=[wba])
            S.op("act", lambda e: e.activation(out=negA[:], in_=alog[:], func=AF.Exp), reads=[CST], writes=[CST])
            S.op("dve", lambda e: e.tensor_scalar(out=negA[:], in0=negA[:], scalar1=-1.0, scalar2=None, op0=ALU.mult),
                 reads=[CST], writes=[CST])
            S.op("dve", lambda e: e.memset(halo[:], 0.0), writes=[halo])
            for g in range(2):
                S.op("dve", lambda e, g=g: e.memset(Sf[g][:], 0.0), writes=[Sf[g]])
                S.op("dve", lambda e, g=g: e.memset(Sb[g][:], 0.0), writes=[Sb[g]])

            def load_panel(col0, ncols=256, src=w_in):
                wp = wpan.next()
                S.dma("pool", wp[:, :, 0:ncols], src[:, col0:col0 + ncols].rearrange("(k p) m -> p k m", p=128), writes=[wp])
                return wp

            stop = False
            for st in range(8):
                if stop:
                    break
                full = st >= 3
                ft = st - 3
                for tq in range(4):
                    xt = xtm.next()
                    r0 = st * 512 + tq * 128
                    S.dma("sp", xt[:], x_win[r0:r0 + 128, :], writes=[xt])
                    for half in range(2):
                        ps = psA.next()
                        S.group("pe", [lambda e, j=j, ps=ps, xt=xt, half=half: e.transpose(
                            out=ps[:, j * 128:(j + 1) * 128], in_=xt[:, (half * 4 + j) * 128:(half * 4 + j + 1) * 128], identity=IDF)
                            for j in range(4)], reads=[xt, CST], writes=[ps])
                        S.op("act", lambda e, ps=ps, half=half, tq=tq: e.activation(
                            out=xTb[:, half * 4:half * 4 + 4, tq * 128:(tq + 1) * 128],
                            in_=ps[:, :].rearrange("p (j t) -> p j t", j=4), func=AF.Copy), reads=[ps], writes=[xTb])
                        if full:
                            S.op("dve", lambda e, ps=ps, half=half, tq=tq: e.tensor_scalar(
                                out=XF[:, half * 4:half * 4 + 4, ft * 512 + tq * 128:ft * 512 + (tq + 1) * 128],
                                in0=ps[:, :].rearrange("p (j t) -> p j t", j=4), scalar1=ALPHA, scalar2=None, op0=ALU.mult),
                                reads=[ps], writes=[XFv[k][ft] for k in range(half * 4, half * 4 + 4)])
                if stage == 1:
                    stop = True
                    continue
                secs = [("k", 1024, kT, 8), ("v", 2048, vT, 16)] + ([("q", 0, qT, 0)] if st >= 2 else [])
                pend = []
                for (nm, c0, dst, m0) in secs:
                    for pn in range(4):
                        wp = load_panel(c0 + pn * 256)
                        for j in range(2):
                            hh = pn * 2 + j
                            ps = psA.next()
                            S.group("pe", [lambda e, k=k, ps=ps, wp=wp, j=j: e.matmul(
                                ps[:, :], lhsT=wp[:, k, j * 128:(j + 1) * 128], rhs=xTb[:, k, :], start=(k == 0), stop=(k == 7))
                                for k in range(8)], reads=[wp, xTb], writes=[ps])
                            ub = ubuf.next()
                            m = m0 + hh
                            S.op("act", lambda e, ub=ub, ps=ps: e.activation(out=ub[:, 3:515], in_=ps[:, :], func=AF.Copy),
                                 reads=[ps], writes=[ub])
                            S.op("pool", lambda e, ub=ub, m=m: e.tensor_copy(out=ub[:, 0:3], in_=halo[:, m, :]),
                                 reads=[halo], writes=[ub])
                            S.op("pool", lambda e, ub=ub, m=m: e.tensor_copy(out=halo[:, m, :], in_=ub[:, 512:515]),
                                 reads=[ub], writes=[halo])
                            if nm == "q" and not full:
                                continue
                            y = ycv.next()
                            S.op("act", lambda e, ps=ps, y=y, m=m: e.activation(out=y[:, :], in_=ps[:, :], func=AF.Copy, scale=cwt[:, m, 3:4]),
                                 reads=[ps, CST], writes=[y])
                            for jj in range(3):
                                S.op("dve", lambda e, ub=ub, y=y, m=m, jj=jj: e.scalar_tensor_tensor(
                                    out=y[:, :], in0=ub[:, jj:jj + 512], scalar=cwt[:, m, jj:jj + 1], in1=y[:, :],
                                    op0=ALU.mult, op1=ALU.add), reads=[ub, y, CST], writes=[y])
                            pend.append((nm, dst, hh, y))
                            if len(pend) == 4:
                                for (nm2, dst2, h2, y2) in pend:
                                    if nm2 == "v":
                                        S.op("act", lambda e, dst2=dst2, h2=h2, y2=y2: e.activation(
                                            out=dst2[:, h2, :], in_=y2[:, :], func=AF.Silu), reads=[y2], writes=[dst2])
                                    else:
                                        S.op("act", lambda e, y2=y2: e.activation(out=y2[:, :], in_=y2[:, :], func=AF.Silu),
                                             reads=[y2], writes=[y2])
                                for (nm2, dst2, h2, y2) in pend:
                                    if nm2 == "v":
                                        continue
                                    sq = sqb.next()
                                    S.op("act", lambda e, sq=sq, y2=y2: e.activation(out=sq[:, :], in_=y2[:, :], func=AF.Square),
                                         reads=[y2], writes=[sq])
                                    pss = psB.next()
                                    S.op("pe", lambda e, sq=sq, pss=pss: e.matmul(pss[:, :], lhsT=ONEB, rhs=sq[:, :], start=True, stop=True),
                                         reads=[sq, CST], writes=[pss])
                                    rn = lnst.next()
                                    S.op("act", lambda e, rn=rn, pss=pss: e.activation(out=rn[:, :], in_=pss[:, :], func=AF.Ln, bias=EPS, scale=1.0),
                                         reads=[pss], writes=[rn])
                                    S.op("act", lambda e, rn=rn: e.activation(out=rn[:, :], in_=rn[:, :], func=AF.Exp, scale=-0.5),
                                         reads=[rn], writes=[rn])
                                    if nm2 == "q":
                                        S.op("dve", lambda e, dst2=dst2, h2=h2, y2=y2, rn=rn: e.scalar_tensor_tensor(
                                            out=dst2[:, h2, :], in0=y2[:, :], scalar=128.0 ** -0.5, in1=rn[:, :],
                                            op0=ALU.mult, op1=ALU.mult), reads=[y2, rn], writes=[dst2])
                                    else:
                                        S.op("dve", lambda e, dst2=dst2, h2=h2, y2=y2, rn=rn: e.tensor_tensor(
                                            out=dst2[:, h2, :], in0=y2[:, :], in1=rn[:, :], op=ALU.mult), reads=[y2, rn], writes=[dst2])
                                pend = []
                if stage == 2:
                    stop = True
                    continue
                def pre(tq):
                    ba, beta, gt, gcum, glast, bexp, kdsc, DECB, DECT, EROW, GLS = PB[tq % 2]
                    tcol = slice(tq * 128, (tq + 1) * 128)
                    psb = psB.next()
                    S.group("pe", [lambda e, k=k, psb=psb: e.matmul(psb[:, 0:16], lhsT=xTb[:, k, tcol], rhs=wba[:, k, :],
                                                                  start=(k == 0), stop=(k == 7)) for k in range(8)],
                            reads=[xTb, wba], writes=[psb])
                    S.op("act", lambda e, psb=psb: e.activation(out=ba[:, :], in_=psb[:, 0:16], func=AF.Copy), reads=[psb], writes=[ba])
                    S.op("act", lambda e: e.activation(out=beta[:, :], in_=ba[:, 0:8], func=AF.Exp, scale=-1.0), reads=[ba], writes=[beta])
                    S.op("dve", lambda e: e.tensor_scalar(out=beta[:, :], in0=beta[:, :], scalar1=1.0, scalar2=None, op0=ALU.add),
                         reads=[beta], writes=[beta])
                    S.op("dve", lambda e: e.reciprocal(out=beta[:, :], in_=beta[:, :]), reads=[beta], writes=[beta])
                    yield
                    S.op("dve", lambda e: e.tensor_tensor(out=gt[:, :], in0=ba[:, 8:16], in1=dtb[:, :], op=ALU.add), reads=[ba, CST], writes=[gt])
                    S.op("act", lambda e: e.activation(out=gt[:, :], in_=gt[:, :], func=AF.Exp), reads=[gt], writes=[gt])
                    S.op("act", lambda e: e.activation(out=gt[:, :], in_=gt[:, :], func=AF.Ln, bias=1.0, scale=1.0), reads=[gt], writes=[gt])
                    S.op("dve", lambda e: e.tensor_tensor(out=gt[:, :], in0=gt[:, :], in1=negA[:, :], op=ALU.mult), reads=[gt, CST], writes=[gt])
                    yield
                    psb = psB.next()
                    S.op("pe", lambda e, psb=psb: e.matmul(psb[:, 0:8], lhsT=TRIF, rhs=gt[:, :], start=True, stop=True), reads=[gt, CST], writes=[psb])
                    S.op("pe", lambda e, psb=psb: e.matmul(psb[:, 8:16], lhsT=BLKF, rhs=gt[:, :], start=True, stop=True), reads=[gt, CST, psb], writes=[psb])
                    S.op("act", lambda e, psb=psb: e.activation(out=gcum[:, :], in_=psb[:, 0:8], func=AF.Copy), reads=[psb], writes=[gcum])
                    S.op("act", lambda e, psb=psb: e.activation(out=glast[:, :], in_=psb[:, 8:16], func=AF.Copy), reads=[psb], writes=[glast])
                    yield
                    S.op("act", lambda e: e.activation(out=bexp[:, :], in_=gcum[:, :], func=AF.Exp), reads=[gcum], writes=[bexp])
                    S.op("dve", lambda e: e.tensor_tensor(out=bexp[:, :], in0=bexp[:, :], in1=beta[:, :], op=ALU.mult), reads=[bexp, beta], writes=[bexp])
                    S.op("dve", lambda e: e.tensor_tensor(out=kdsc[:, :], in0=glast[:, :], in1=gcum[:, :], op=ALU.subtract), reads=[glast, gcum], writes=[kdsc])
                    S.op("act", lambda e: e.activation(out=kdsc[:, :], in_=kdsc[:, :], func=AF.Exp), reads=[kdsc], writes=[kdsc])
                    yield
                    S.op("dve", lambda e: e.tensor_tensor(out=Dm[:, :, :], in0=gcum[:, :].unsqueeze(2).to_broadcast([128, 8, 128]),
                                                          in1=IDF.unsqueeze(1).to_broadcast([128, 8, 128]), op=ALU.mult),
                         reads=[gcum, CST], writes=[Dm])
                    S.group("pe", [lambda e, hf=hf: e.matmul(psW[:, hf * 512:(hf + 1) * 512], lhsT=ONEF,
                                                           rhs=Dm[:, hf * 4:hf * 4 + 4, :].rearrange("p h j -> p (h j)"), start=True, stop=True)
                                   for hf in range(2)], reads=[Dm, CST], writes=[psW])
                    R3 = psW[:, :].rearrange("p (h j) -> p h j", h=8)
                    gc_b = gcum[:, :].unsqueeze(2).to_broadcast([128, 8, 128])
                    S.op("dve", lambda e: e.tensor_tensor(out=tmpW[:, :, :], in0=gc_b, in1=R3, op=ALU.subtract), reads=[gcum, psW], writes=[tmpW])
                    S.op("dve", lambda e: e.tensor_tensor(out=tmpW[:, :, :], in0=tmpW[:, :, :], in1=MBL.unsqueeze(1).to_broadcast([128, 8, 128]), op=ALU.add),
                         reads=[tmpW, CST], writes=[tmpW])
                    S.op("act", lambda e: e.activation(out=tmpW[:, :, :], in_=tmpW[:, :, :], func=AF.Exp), reads=[tmpW], writes=[tmpW])
                    S.op("dve", lambda e: e.tensor_tensor(out=DECB[:, :, :], in0=tmpW[:, :, :], in1=beta[:, :].unsqueeze(2).to_broadcast([128, 8, 128]), op=ALU.mult),
                         reads=[tmpW, beta], writes=[DECB])
                    yield
                    S.op("dve", lambda e: e.tensor_tensor(out=tmpW[:, :, :], in0=R3, in1=gc_b, op=ALU.subtract), reads=[gcum, psW, tmpW], writes=[tmpW])
                    S.op("dve", lambda e: e.tensor_tensor(out=tmpW[:, :, :], in0=tmpW[:, :, :], in1=MBU.unsqueeze(1).to_broadcast([128, 8, 128]), op=ALU.add),
                         reads=[tmpW, CST], writes=[tmpW])
                    S.op("act", lambda e: e.activation(out=DECT[:, :, :], in_=tmpW[:, :, :], func=AF.Exp), reads=[tmpW], writes=[DECT])
                    yield
                    S.op("act", lambda e: e.activation(out=EROW[:, :, :], in_=R3, func=AF.Exp), reads=[psW], writes=[EROW])
                    S.op("act", lambda e: e.activation(out=GLS[:, :, :], in_=R3[:, :, 63:128:64], func=AF.Exp), reads=[psW], writes=[GLS])

                    yield

                def unit(g, tq):
                    tcol = slice(tq * 128, (tq + 1) * 128)
                    ba, beta, gt, gcum, glast, bexp, kdsc, DECB, DECT, EROW, GLS = PB[tq % 2]
                    pp = ppc[g]
                    gbuf = gbufc[g]
                    hs = slice(g * 4, g * 4 + 4)

                    def mm4(ps, lfn, rfn, reads):
                        S.group("pe", [lambda e, a=a: e.matmul(ps[:, a * 128:(a + 1) * 128], lhsT=lfn(a), rhs=rfn(a), start=True, stop=True)
                                       for a in range(4)], reads=reads, writes=[ps])

                    def tr4(ps, ifn, reads):
                        S.group("pe", [lambda e, a=a: e.transpose(out=ps[:, a * 128:(a + 1) * 128], in_=ifn(a), identity=IDB)
                                       for a in range(4)], reads=reads + [CST], writes=[ps])

                    def ev(dst, ps, eng="act"):
                        if eng == "act":
                            S.op("act", lambda e: e.activation(out=dst[:, :, :].rearrange("p a j -> p (a j)"), in_=ps[:, :], func=AF.Copy),
                                 reads=[ps], writes=[dst])
                        else:
                            S.op("dve", lambda e: e.tensor_copy(out=dst[:, :, :].rearrange("p a j -> p (a j)"), in_=ps[:, :]),
                                 reads=[ps], writes=[dst])

                    def p3(ps):
                        return ps[:, :].rearrange("p (a j) -> p a j", a=4)

                    ps = psA.next()
                    mm4(ps, lambda a: kT[:, g * 4 + a, tcol], lambda a: kT[:, g * 4 + a, tcol], [kT])
                    A = pp[0]
                    S.op("dve", lambda e: e.tensor_tensor(out=A[:, :, :], in0=p3(ps), in1=DECB[:, hs, :], op=ALU.mult), reads=[ps, DECB], writes=[A])
                    yield
                    pst = psT.next()
                    tr4(pst, lambda a: A[:, a, :], [A])
                    AT = pp[1]
                    ev(AT, pst, "act")
                    Tt = gbuf["Tt"]
                    S.op("dve", lambda e: e.tensor_tensor(out=Tt[:, :, :], in0=IDF.unsqueeze(1).to_broadcast([128, 4, 128]),
                                                          in1=pst[:, :].rearrange("p (a j) -> p a j", a=4), op=ALU.subtract),
                         reads=[pst, CST], writes=[Tt])
                    yield
                    P, PT = A, AT
                    for lvl in range(5):
                        ps = psA.next()
                        mm4(ps, lambda a: PT[:, a, :], lambda a: P[:, a, :], [P, PT])
                        P2 = pp[2] if P is pp[0] else pp[0]
                        ev(P2, ps, "act")
                        yield
                        P2T = None
                        if lvl < 4:
                            ps2 = psA.next()
                            mm4(ps2, lambda a: P[:, a, :], lambda a: PT[:, a, :], [P, PT])
                            P2T = pp[3] if PT is pp[1] else pp[1]
                            ev(P2T, ps2, "act")
                            yield
                        ps3 = psA.next()
                        mm4(ps3, lambda a: P2[:, a, :], lambda a: Tt[:, a, :], [P2, Tt])
                        S.op("dve", lambda e: e.tensor_tensor(out=Tt[:, :, :], in0=Tt[:, :, :], in1=p3(ps3), op=ALU.add), reads=[ps3, Tt], writes=[Tt])
                        yield
                        P, PT = P2, P2T
                    pst = psT.next()
                    tr4(pst, lambda a: kT[:, g * 4 + a, tcol], [kT])
                    Xw = gbuf["Xw"]
                    kd = gbuf["kd"]
                    pk3 = pst[:, :].rearrange("p (a j) -> p a j", a=4)
                    S.op("dve", lambda e: e.tensor_tensor(out=Xw[:, :, :], in0=pk3, in1=bexp[:, hs].unsqueeze(2).to_broadcast([128, 4, 128]), op=ALU.mult),
                         reads=[pst, bexp], writes=[Xw])
                    S.op("dve", lambda e: e.tensor_tensor(out=kd[:, :, :], in0=pk3, in1=kdsc[:, hs].unsqueeze(2).to_broadcast([128, 4, 128]), op=ALU.mult),
                         reads=[pst, kdsc], writes=[kd])
                    yield
                    pst = psT.next()
                    tr4(pst, lambda a: vT[:, g * 4 + a, tcol], [vT])
                    Xu = gbuf["Xu"]
                    pv3 = pst[:, :].rearrange("p (a j) -> p a j", a=4)
                    S.op("dve", lambda e: e.tensor_tensor(out=Xu[:, :, :], in0=pv3, in1=beta[:, hs].unsqueeze(2).to_broadcast([128, 4, 128]), op=ALU.mult),
                         reads=[pst, beta], writes=[Xu])
                    yield
                    ps = psA.next()
                    mm4(ps, lambda a: Tt[:, a, :], lambda a: Xu[:, a, :], [Tt, Xu])
                    u = ufc[g]
                    ev(u, ps, "act")
                    yield
                    ps = psA.next()
                    mm4(ps, lambda a: Xw[:, a, :], lambda a: Tt[:, a, :], [Tt, Xw])
                    wT = gbuf["wT"]
                    ev(wT, ps, "act")
                    yield
                    if full:
                        ps = psA.next()
                        mm4(ps, lambda a: kT[:, g * 4 + a, tcol], lambda a: qT[:, g * 4 + a, tcol], [kT, qT])
                        QKT = gbuf["QKT"]
                        S.op("dve", lambda e: e.tensor_tensor(out=QKT[:, :, :], in0=p3(ps), in1=DECT[:, hs, :], op=ALU.mult), reads=[ps, DECT], writes=[QKT])
                        qd = gbuf["qd"]
                        S.op("pool", lambda e: e.tensor_tensor(out=qd[:, :, :], in0=qT[:, hs, tcol], in1=EROW[:, hs, :], op=ALU.mult),
                             reads=[qT, EROW], writes=[qd])
                        yield
                    for c in range(2):
                        pr = slice(c * 64, c * 64 + 64)
                        ps = psA.next()
                        mm4(ps, lambda a: wT[:, a, :], lambda a: Sb[g][:, a, :], [wT, Sb[g]])
                        VN = gbuf["VN%d" % c]
                        S.op("dve", lambda e: e.tensor_tensor(out=VN[pr, :, :], in0=u[pr, :, :], in1=p3(ps)[pr, :, :], op=ALU.subtract),
                             reads=[u, ps], writes=[VN])
                        yield
                        if full:
                            pso = psB.next()
                            fns = []
                            for a in range(4):
                                fns.append(lambda e, a=a: e.matmul(pso[:, a * 64:(a + 1) * 64], lhsT=Sb[g][:, a, :], rhs=qd[:, a, pr], start=True, stop=False))
                                fns.append(lambda e, a=a: e.matmul(pso[:, a * 64:(a + 1) * 64], lhsT=VN[pr, a, :], rhs=QKT[pr, a, pr], start=False, stop=True))
                            S.group("pe", fns, reads=[Sb[g], qd, VN, QKT], writes=[pso])
                            S.op("act", lambda e: e.activation(
                                out=OT[:, hs, tq * 128 + c * 64:tq * 128 + c * 64 + 64],
                                in_=pso[:, 0:256].rearrange("p (a t) -> p a t", a=4), func=AF.Copy), reads=[pso], writes=[OT])
                            yield
                        ps = psA.next()
                        mm4(ps, lambda a: kd[pr, a, :], lambda a: VN[pr, a, :], [kd, VN])
                        gl = GLS[:, hs, c:c + 1].to_broadcast([128, 4, 128])
                        S.op("dve", lambda e: e.tensor_tensor(out=Sf[g][:, :, :], in0=Sf[g][:, :, :], in1=gl, op=ALU.mult), reads=[Sf[g], GLS], writes=[Sf[g]])
                        S.op("dve", lambda e: e.tensor_tensor(out=Sf[g][:, :, :], in0=Sf[g][:, :, :], in1=p3(ps), op=ALU.add), reads=[Sf[g], ps], writes=[Sf[g]])
                        S.op("act", lambda e: e.activation(out=Sb[g][:, :, :], in_=Sf[g][:, :, :], func=AF.Copy), reads=[Sf[g]], writes=[Sb[g]])
                        yield

                pre_done = [False] * 4
                unit_done = [[False] * 4 for _ in range(2)]

                def pre_all():
                    for tq in range(4):
                        while tq >= 2 and not (unit_done[0][tq - 2] and unit_done[1][tq - 2]):
                            yield
                        yield from pre(tq)
                        pre_done[tq] = True

                def units(g):
                    for tq in range(4):
                        while not pre_done[tq]:
                            yield
                        yield from unit(g, tq)
                        unit_done[g][tq] = True

                run_chains([pre_all(), units(0), units(1)])
                if stage == 5:
                    stop = True
                    continue
                if not full:
                    continue
                rstds = []
                for h in range(8):
                    sq = sqb.next()
                    S.op("act", lambda e, sq=sq, h=h: e.activation(out=sq[:, :], in_=OT[:, h, :], func=AF.Square), reads=[OT], writes=[sq])
                    pss = psB.next()
                    S.op("pe", lambda e, sq=sq, pss=pss: e.matmul(pss[:, :], lhsT=ONEB, rhs=sq[:, :], start=True, stop=True), reads=[sq, CST], writes=[pss])
                    rn = lnst.next()
                    S.op("act", lambda e, rn=rn, pss=pss: e.activation(out=rn[:, :], in_=pss[:, :], func=AF.Ln, bias=EPS, scale=1.0 / 128), reads=[pss], writes=[rn])
                    S.op("act", lambda e, rn=rn: e.activation(out=rn[:, :], in_=rn[:, :], func=AF.Exp, scale=-0.5), reads=[rn], writes=[rn])
                    S.op("dve", lambda e, rn=rn, h=h: e.scalar_tensor_tensor(out=OT[:, h, :], in0=OT[:, h, :], scalar=normg[:, 0:1], in1=rn[:, :],
                                                                          op0=ALU.mult, op1=ALU.mult), reads=[OT, rn, CST], writes=[OT])
                for pn in range(4):
                    wp = load_panel(3072 + pn * 256)
                    for j in range(2):
                        h = pn * 2 + j
                        ps = psA.next()
                        S.group("pe", [lambda e, k=k, ps=ps, wp=wp, j=j: e.matmul(
                            ps[:, :], lhsT=wp[:, k, j * 128:(j + 1) * 128], rhs=xTb[:, k, :], start=(k == 0), stop=(k == 7))
                            for k in range(8)], reads=[wp, xTb], writes=[ps])
                        zs = ycv.next()
                        S.op("act", lambda e, zs=zs, ps=ps: e.activation(out=zs[:, :], in_=ps[:, :], func=AF.Silu), reads=[ps], writes=[zs])
                        S.op("dve", lambda e, zs=zs, h=h: e.tensor_tensor(out=OG[:, h, :], in0=OT[:, h, :], in1=zs[:, :], op=ALU.mult),
                             reads=[OT, zs], writes=[OG])
                tok = slice(ft * 512, (ft + 1) * 512)
                for pn in range(4):
                    wp = load_panel(pn * 256, src=w_oa)
                    for j in range(2):
                        m = pn * 2 + j
                        ps = psA.next()
                        S.group("pe", [lambda e, k=k, ps=ps, wp=wp, j=j: e.matmul(
                            ps[:, :], lhsT=wp[:, k, j * 128:(j + 1) * 128], rhs=OG[:, k, :], start=(k == 0), stop=(k == 7))
                            for k in range(8)], reads=[wp, OG], writes=[ps])
                        S.op("dve", lambda e, ps=ps, m=m: e.tensor_tensor(out=XF[:, m, tok], in0=XF[:, m, tok], in1=ps[:, :], op=ALU.add),
                             reads=[ps, XFv[m][ft]], writes=[XFv[m][ft]])
                layer_norm(0, ft)
                if stage == 6:
                    stop = True
            S.barrier()
        S.stack = top


        def finish_dbg():
            allx = [XFv[k][t] for k in range(8) for t in range(5)]
            S.dma("sp", dbg_d[:, :, :], XF[:], reads=allx, owner=CST)
            S._wait("sp", CST.dkey, CST.dcnt)

        if stage < 10:
            finish_dbg()
            return nc

        XB = S.sbuf("XB", [128, 8, 2560], BF16)
        XBv = [[S.view("XB%d_%d" % (k, t), None) for t in range(5)] for k in range(8)]

        def tokc(t):
            return slice(t * 512, (t + 1) * 512)

        def make_xb(tts):
            for t in tts:
                for k in range(8):
                    if k % 2 == 0:
                        S.op("act", lambda e, k=k, t=t: e.activation(out=XB[:, k, tokc(t)], in_=XF[:, k, tokc(t)], func=AF.Copy),
                             reads=[XFv[k][t]], writes=[XBv[k][t]])
                    else:
                        S.op("dve", lambda e, k=k, t=t: e.tensor_copy(out=XB[:, k, tokc(t)], in_=XF[:, k, tokc(t)]),
                             reads=[XFv[k][t]], writes=[XBv[k][t]])

        def scale_xf(tts):
            for t in tts:
                for k in range(8):
                    S.op("dve", lambda e, k=k, t=t: e.tensor_scalar(out=XF[:, k, tokc(t)], in0=XF[:, k, tokc(t)], scalar1=ALPHA, scalar2=None, op0=ALU.mult),
                         reads=[XFv[k][t]], writes=[XFv[k][t]])

        def expert(up_src, dn_src, F, tts, gate=None, hook=None):
            nf = F // 128
            for c0 in range(0, nf, 4):
                cf = min(4, nf - c0)
                for p0 in range(c0, c0 + cf, 2):
                    npn = min(2, c0 + cf - p0)
                    wp = uppan.next()
                    S.dma("pool", wp[:, :, 0:npn * 128], up_src[:, p0 * 128:(p0 + npn) * 128].rearrange("(k p) m -> p k m", p=128), writes=[wp])
                    S.dma("pool", wp[:, :, 256:256 + npn * 128], up_src[:, F + p0 * 128:F + (p0 + npn) * 128].rearrange("(k p) m -> p k m", p=128), writes=[wp])
                    for ti, t in enumerate(tts):
                        for j in range(npn):
                            fj = p0 + j - c0
                            psg = psA.next()
                            S.group("pe", [lambda e, k=k, psg=psg, wp=wp, j=j, t=t: e.matmul(
                                psg[:, :], lhsT=wp[:, k, j * 128:(j + 1) * 128], rhs=XB[:, k, tokc(t)], start=(k == 0), stop=(k == 7))
                                for k in range(8)], reads=[wp] + [XBv[k][t] for k in range(8)], writes=[psg])
                            psu = psA.next()
                            S.group("pe", [lambda e, k=k, psu=psu, wp=wp, j=j, t=t: e.matmul(
                                psu[:, :], lhsT=wp[:, k, 256 + j * 128:256 + (j + 1) * 128], rhs=XB[:, k, tokc(t)], start=(k == 0), stop=(k == 7))
                                for k in range(8)], reads=[wp] + [XBv[k][t] for k in range(8)], writes=[psu])
                            sg = sgp.next()
                            S.op("act", lambda e, sg=sg, psg=psg: e.activation(out=sg[:, :], in_=psg[:, :], func=AF.Silu), reads=[psg], writes=[sg])
                            if gate is not None:
                                S.op("dve", lambda e, sg=sg, ti=ti: e.tensor_tensor(out=sg[:, :], in0=sg[:, :], in1=gate[0][:, ti * 512:(ti + 1) * 512], op=ALU.mult),
                                     reads=[sg, gate[1][ti]], writes=[sg])
                            S.op("dve", lambda e, sg=sg, psu=psu, fj=fj, ti=ti: e.tensor_tensor(
                                out=ACT_[:, fj, ti * 512:(ti + 1) * 512], in0=sg[:, :], in1=psu[:, :], op=ALU.mult),
                                reads=[sg, psu], writes=[ACTv[fj][ti]])
                wd = dnpan.next()
                S.dma("pool", wd[:, 0:cf, :], dn_src[c0 * 128:(c0 + cf) * 128, :].rearrange("(f p) m -> p f m", p=128), writes=[wd])
                for ti, t in enumerate(tts):
                    for m in range(8):
                        ps = psB.next()
                        S.group("pe", [lambda e, j=j, ps=ps, wd=wd, m=m, ti=ti: e.matmul(
                            ps[:, :], lhsT=wd[:, j, m * 128:(m + 1) * 128], rhs=ACT_[:, j, ti * 512:(ti + 1) * 512], start=(j == 0), stop=(j == cf - 1))
                            for j in range(cf)], reads=[wd] + [ACTv[j][ti] for j in range(cf)], writes=[ps])
                        S.op("dve", lambda e, ps=ps, m=m, t=t: e.tensor_tensor(out=XF[:, m, tokc(t)], in0=XF[:, m, tokc(t)], in1=ps[:, :], op=ALU.add),
                             reads=[ps, XFv[m][t]], writes=[XFv[m][t]])
                if c0 == 0 and hook is not None:
                    hook()

        with contextlib.ExitStack() as pb:
            S.stack = pb
            psA = Rot([S.psum("bpsA%d" % i, [128, 512], F32) for i in range(4)])
            psB = Rot([S.psum("bpsB%d" % i, [128, 512], F32) for i in range(4)])
            scr = Rot([S.sbuf("bscr%d" % i, [128, 512], F32) for i in range(4)])
            lnsq = scr
            lnst = Rot([S.sbuf("blnst%d" % i, [128, 512], F32) for i in range(3)])
            uppan = Rot([S.sbuf("buppan%d" % i, [128, 8, 512], BF16) for i in range(2)])
            dnpan = Rot([S.sbuf("bdnpan%d" % i, [128, 4, 1024], BF16) for i in range(2)])
            sgp = Rot([S.sbuf("bsg%d" % i, [128, 512], F32) for i in range(2)])
            ACT_ = S.sbuf("bact", [128, 4, 2560], BF16)
            ACTv = [[S.view("bact%d_%d" % (j, t), None) for t in range(5)] for j in range(4)]
            make_xb(range(5))
            scale_xf(range(5))
            expert(ffn_up, ffn_dn, 2816, list(range(5)))
            for t in range(5):
                layer_norm(1, t, xb_t=XB, xbv=XBv)
            S.barrier()
        S.stack = top
        if stage == 10:
            finish_dbg()
            return nc

        with contextlib.ExitStack() as pc:
            S.stack = pc
            scale_xf(range(1, 5))
            psS = Rot([S.psum("cpsS%d" % i, [128, 1024], F32) for i in range(3)])
            psT_t = S.psum("cpsT", [128, 1024], BF16)
            psTv = S.view("cpsTv", psT_t[:, 0:640])
            psTv.psum = True
            psO1 = S.psum("cpsO", [128, 512], F32)
            psO = psS
            cpan = Rot([S.sbuf("cpan%d" % i, [128, 8, 256], BF16) for i in range(3)])
            KT2 = S.sbuf("KT2", [128, 2, 2560], BF16)
            QT2 = S.sbuf("QT2", [128, 2, 2048], BF16)
            VT = S.sbuf("VT", [128, 20, 256], BF16)
            BM = S.sbuf("BM", [128, 4, 640], BF16)
            WM = S.sbuf("WM", [128, 640], BF16)
            KV1 = S.sbuf("KV1", [1, 2560], BF16)
            ONE1 = S.sbuf("ONE1", [1, 128], BF16)
            pexp = Rot([S.sbuf("cpe%d" % i, [128, 640], BF16) for i in range(3)])
            PTs = Rot([S.sbuf("cPT%d" % i, [128, 5, 128], BF16) for i in range(2)])
            OTM = Rot([S.sbuf("cOTM%d" % i, [128, 256], BF16) for i in range(2)])
            OTF = S.sbuf("cOTF", [128, 2, 2048], BF16)
            wob = S.sbuf("cwob", [128, 2, 1024], BF16)
            st4 = Rot([S.sbuf("cst%d" % i, [128, 4], F32) for i in range(6)])
            S.dma("pool", WM[:], wmask_d[:, :], writes=[WM])
            S.dma("pool", KV1[:], kvalid_d[:, :], writes=[KV1])
            S.op("dve", lambda e: e.memset(ONE1[:], 1.0), writes=[ONE1])
            for hg in range(4):
                wk = cpan.next()
                S.dma("pool", wk[:], kv_w[:, hg * 256:(hg + 1) * 256].rearrange("(k p) m -> p k m", p=128), writes=[wk])
                wv = cpan.next()
                S.dma("pool", wv[:], kv_w[:, 1024 + hg * 256:1024 + (hg + 1) * 256].rearrange("(k p) m -> p k m", p=128), writes=[wv])
                wq = cpan.next()
                S.dma("pool", wq[:], w_q[:, hg * 256:(hg + 1) * 256].rearrange("(k p) m -> p k m", p=128), writes=[wq])
                S.dma("pool", wob[:], w_ob[hg * 256:(hg + 1) * 256, :].rearrange("(f p) m -> p f m", p=128), writes=[wob])
                S.dma("pool", BM[:], biasT[hg * 4:(hg + 1) * 4, :, :].rearrange("a q k -> q a k"), writes=[BM])
                S.op("dve", lambda e: e.tensor_tensor(out=BM[:, :, :], in0=BM[:, :, :], in1=WM[:, :].unsqueeze(1).to_broadcast([128, 4, 640]), op=ALU.add),
                     reads=[BM, WM], writes=[BM])
                for t in range(5):
                    for pi in range(2):
                        ps = psO.next()
                        S.group("pe", [lambda e, k=k, ps=ps, pi=pi, t=t: e.matmul(ps[:, 0:512], lhsT=wk[:, k, pi * 128:(pi + 1) * 128], rhs=XB[:, k, tokc(t)],
                                                                                start=(k == 0), stop=(k == 7)) for k in range(8)],
                                reads=[wk] + [XBv[k][t] for k in range(8)], writes=[ps])
                        S.op("act", lambda e, ps=ps, pi=pi, t=t: e.activation(out=KT2[:, pi, tokc(t)], in_=ps[:, 0:512], func=AF.Copy), reads=[ps], writes=[KT2])
                        if t >= 1:
                            ps = psO.next()
                            S.group("pe", [lambda e, k=k, ps=ps, pi=pi, t=t: e.matmul(ps[:, 0:512], lhsT=wq[:, k, pi * 128:(pi + 1) * 128], rhs=XB[:, k, tokc(t)],
                                                                                    start=(k == 0), stop=(k == 7)) for k in range(8)],
                                    reads=[wq] + [XBv[k][t] for k in range(8)], writes=[ps])
                            S.op("act", lambda e, ps=ps, pi=pi, t=t: e.activation(out=QT2[:, pi, tokc(t - 1)], in_=ps[:, 0:512], func=AF.Copy, scale=0.125),
                                 reads=[ps], writes=[QT2])
                    for q4 in range(4):
                        kt = t * 4 + q4
                        ps = psO.next()
                        S.group("pe", [lambda e, k=k, ps=ps, kt=kt: e.matmul(ps[:, 0:256], lhsT=XB[:, k, kt * 128:(kt + 1) * 128], rhs=wv[:, k, :],
                                                                            start=(k == 0), stop=(k == 7)) for k in range(8)],
                                reads=[wv] + [XBv[k][t] for k in range(8)], writes=[ps])
                        S.op("dve", lambda e, ps=ps, kt=kt: e.tensor_copy(out=VT[:, kt, :], in_=ps[:, 0:256]), reads=[ps], writes=[VT])
                chains = [(T, a) for T in range(16) for a in range(4)]
                cst_ = {}
                otms = {}

                def s1(T, a):
                    pi = a // 2
                    pr = slice((a % 2) * 64, (a % 2) * 64 + 64)
                    ps2 = psS.next()
                    qs = slice(T * 128, (T + 1) * 128)
                    fns = []
                    for (c0, c1) in ((0, 512), (512, 640)):
                        ks = slice(T * 128 + c0, T * 128 + c1)
                        fns.append(lambda e, c0=c0, c1=c1, ks=ks: e.matmul(ps2[:, c0:c1], lhsT=QT2[pr, pi, qs], rhs=KT2[pr, pi, ks], start=True, stop=False))
                        if T < 4:
                            fns.append(lambda e, c0=c0, c1=c1, ks=ks: e.matmul(ps2[:, c0:c1], lhsT=ONE1[0:1, :], rhs=KV1[0:1, ks], start=False, stop=False))
                        fns.append(lambda e, c0=c0, c1=c1: e.matmul(ps2[:, c0:c1], lhsT=IDB, rhs=BM[:, a, c0:c1], start=False, stop=True))
                    S.group("pe", fns, reads=[QT2, KT2, ONE1, KV1, BM, CST], writes=[ps2])
                    stt_ = st4.next()
                    S.op("dve", lambda e: e.tensor_reduce(out=stt_[:, 1:2], in_=ps2[:, 0:640], axis=AX.X, op=ALU.max, negate=True), reads=[ps2], writes=[stt_])
                    pe_ = pexp.next()
                    S.op("act", lambda e: e.activation(out=pe_[:, :], in_=ps2[:, 0:640], func=AF.Exp, bias=stt_[:, 1:2], scale=1.0, accum_out=stt_[:, 2:3]),
                         reads=[ps2, stt_], writes=[pe_, stt_])
                    S.op("dve", lambda e: e.reciprocal(out=stt_[:, 3:4], in_=stt_[:, 2:3]), reads=[stt_], writes=[stt_])
                    cst_[(T, a)] = [pe_, stt_, None]

                def s2(T, a):
                    pe_, stt_, _ = cst_[(T, a)]
                    S.group("pe", [lambda e, kt=kt: e.transpose(out=psTv[:, kt * 128:(kt + 1) * 128], in_=pe_[:, kt * 128:(kt + 1) * 128], identity=IDB)
                                   for kt in range(5)], reads=[pe_, CST], writes=[psTv])
                    pt = PTs.next()
                    S.op("dve", lambda e: e.tensor_copy(out=pt[:, :, :].rearrange("p k q -> p (k q)"), in_=psTv[:, :]), reads=[psTv], writes=[pt])
                    cst_[(T, a)][2] = pt

                def s3(T, a):
                    pe_, stt_, pt = cst_.pop((T, a))
                    if a == 0:
                        otms[T] = OTM.next()
                    otm = otms[T]
                    pso = psO1
                    S.group("pe", [lambda e, kt=kt: e.matmul(pso[:, 0:64], lhsT=pt[:, kt, :], rhs=VT[:, T + kt, a * 64:(a + 1) * 64], start=(kt == 0), stop=(kt == 4))
                                   for kt in range(5)], reads=[pt, VT], writes=[pso])
                    S.op("act", lambda e: e.activation(out=otm[:, a * 64:(a + 1) * 64], in_=pso[:, 0:64], func=AF.Copy, scale=stt_[:, 3:4]),
                         reads=[pso, stt_], writes=[otm])
                    if a == 3:
                        S.group("pe", [lambda e, j=j: e.transpose(out=psTv[:, j * 128:(j + 1) * 128], in_=otm[:, j * 128:(j + 1) * 128], identity=IDB)
                                       for j in range(2)], reads=[otm, CST], writes=[psTv])
                        S.op("act", lambda e: e.activation(out=OTF[:, :, T * 128:(T + 1) * 128], in_=psTv[:, 0:256].rearrange("p (j q) -> p j q", j=2), func=AF.Copy),
                             reads=[psTv], writes=[OTF])

                s1(*chains[0])
                s1(*chains[1])
                for ci in range(len(chains)):
                    s2(*chains[ci])
                    if ci + 2 < len(chains):
                        s1(*chains[ci + 2])
                    s3(*chains[ci])
                for t in range(1, 5):
                    for m in range(8):
                        ps = psO.next()
                        S.group("pe", [lambda e, j=j, ps=ps, m=m, t=t: e.matmul(ps[:, 0:512], lhsT=wob[:, j, m * 128:(m + 1) * 128], rhs=OTF[:, j, tokc(t - 1)],
                                                                               start=(j == 0), stop=(j == 1)) for j in range(2)], reads=[wob, OTF], writes=[ps])
                        S.op("dve", lambda e, ps=ps, m=m, t=t: e.tensor_tensor(out=XF[:, m, tokc(t)], in0=XF[:, m, tokc(t)], in1=ps[:, 0:512], op=ALU.add),
                             reads=[ps, XFv[m][t]], writes=[XFv[m][t]])
            S.barrier()
        S.stack = top

        with contextlib.ExitStack() as pd:
            S.stack = pd
            psA = Rot([S.psum("dpsA%d" % i, [128, 512], F32) for i in range(4)])
            psB = Rot([S.psum("dpsB%d" % i, [128, 512], F32) for i in range(4)])
            scr = Rot([S.sbuf("dscr%d" % i, [128, 512], F32) for i in range(4)])
            lnsq = scr
            lnst = Rot([S.sbuf("dlnst%d" % i, [128, 512], F32) for i in range(3)])
            for t in range(1, 5):
                layer_norm(2, t, xb_t=XB, xbv=XBv)
            if stage == 11:
                S.barrier()
                S.stack = top
                finish_dbg()
                return nc
            uppan = Rot([S.sbuf("duppan%d" % i, [128, 8, 512], BF16) for i in range(2)])
            dnpan = Rot([S.sbuf("ddnpan%d" % i, [128, 4, 1024], BF16) for i in range(2)])
            sgp = Rot([S.sbuf("dsg%d" % i, [128, 512], F32) for i in range(2)])
            ACT_ = S.sbuf("dact", [128, 4, 2048], BF16)
            ACTv = [[S.view("dact%d_%d" % (j, t), None) for t in range(4)] for j in range(4)]
            GB = S.sbuf("GB", [128, 2048], F32)
            GBv = [S.view("GB%d" % t, None) for t in range(4)]
            GTM = S.sbuf("GTM", [128, 16, 8], F32)
            WR = S.sbuf("WR", [128, 8, 8], F32)
            rt = Rot([S.sbuf("drt%d" % i, [128, 8], F32) for i in range(6)])
            rs = Rot([S.sbuf("drs%d" % i, [128, 8], F32) for i in range(4)])
            Dg = Rot([S.sbuf("dDg%d" % i, [128, 128], F32) for i in range(2)])
            S.dma("sp", WR[:], router_d.rearrange("(k p) e -> p k e", p=128), writes=[WR])
            for i in range(16):
                t, q4 = 1 + i // 4, i % 4
                ts_ = slice(t * 512 + q4 * 128, t * 512 + (q4 + 1) * 128)
                ps = psA.next()
                S.group("pe", [lambda e, k=k, ps=ps, ts_=ts_: e.matmul(ps[:, 0:8], lhsT=XF[:, k, ts_], rhs=WR[:, k, :], start=(k == 0), stop=(k == 7))
                               for k in range(8)], reads=[WR] + [XFv[k][t] for k in range(8)], writes=[ps])
                lg = rt.next()
                S.op("act", lambda e, lg=lg, ps=ps: e.activation(out=lg[:, :], in_=ps[:, 0:8], func=AF.Copy), reads=[ps], writes=[lg])
                sc_ = rs.next()
                S.op("dve", lambda e, lg=lg, sc_=sc_: e.tensor_reduce(out=sc_[:, 0:1], in_=lg[:, :], axis=AX.X, op=ALU.max), reads=[lg], writes=[sc_])
                eq1 = rt.next()
                S.op("dve", lambda e, lg=lg, sc_=sc_, eq1=eq1: e.tensor_scalar(out=eq1[:, :], in0=lg[:, :], scalar1=sc_[:, 0:1], scalar2=None, op0=ALU.is_equal),
                     reads=[lg, sc_], writes=[eq1])
                l2 = rt.next()
                S.op("dve", lambda e, lg=lg, eq1=eq1, l2=l2: e.scalar_tensor_tensor(out=l2[:, :], in0=eq1[:, :], scalar=-1e30, in1=lg[:, :], op0=ALU.mult, op1=ALU.add),
                     reads=[lg, eq1], writes=[l2])
                S.op("dve", lambda e, l2=l2, sc_=sc_: e.tensor_reduce(out=sc_[:, 1:2], in_=l2[:, :], axis=AX.X, op=ALU.max), reads=[l2, sc_], writes=[sc_])
                eq2 = rt.next()
                S.op("dve", lambda e, l2=l2, sc_=sc_, eq2=eq2: e.tensor_scalar(out=eq2[:, :], in0=l2[:, :], scalar1=sc_[:, 1:2], scalar2=None, op0=ALU.is_equal),
                     reads=[l2, sc_], writes=[eq2])
                S.op("dve", lambda e, sc_=sc_: e.tensor_tensor(out=sc_[:, 2:3], in0=sc_[:, 1:2], in1=sc_[:, 0:1], op=ALU.subtract), reads=[sc_], writes=[sc_])
                S.op("act", lambda e, sc_=sc_: e.activation(out=sc_[:, 2:3], in_=sc_[:, 2:3], func=AF.Exp), reads=[sc_], writes=[sc_])
                S.op("dve", lambda e, sc_=sc_: e.tensor_scalar(out=sc_[:, 3:4], in0=sc_[:, 2:3], scalar1=1.0, scalar2=None, op0=ALU.add), reads=[sc_], writes=[sc_])
                S.op("dve", lambda e, sc_=sc_: e.reciprocal(out=sc_[:, 3:4], in_=sc_[:, 3:4]), reads=[sc_], writes=[sc_])
                S.op("dve", lambda e, sc_=sc_: e.tensor_tensor(out=sc_[:, 4:5], in0=sc_[:, 2:3], in1=sc_[:, 3:4], op=ALU.mult), reads=[sc_], writes=[sc_])
                S.op("dve", lambda e, eq1=eq1, sc_=sc_: e.tensor_scalar(out=eq1[:, :], in0=eq1[:, :], scalar1=sc_[:, 3:4], scalar2=None, op0=ALU.mult),
                     reads=[eq1, sc_], writes=[eq1])
                S.op("dve", lambda e, eq1=eq1, eq2=eq2, sc_=sc_, i=i: e.scalar_tensor_tensor(out=GTM[:, i, :], in0=eq2[:, :], scalar=sc_[:, 4:5], in1=eq1[:, :],
                                                                                           op0=ALU.mult, op1=ALU.add), reads=[eq1, eq2, sc_], writes=[GTM])
            scale_xf(range(1, 5))
            GB2 = S.sbuf("GB2", [128, 2048], F32)
            GBs = [GB, GB2]
            GBvs = [GBv, [S.view("GB2_%d" % t, None) for t in range(4)]]

            def make_gb(ex):
                gb, gbv = GBs[ex % 2], GBvs[ex % 2]
                for t4 in range(4):
                    ps = psA.next()
                    for q4 in range(4):
                        i = t4 * 4 + q4
                        dg = Dg.next()
                        S.op("dve", lambda e, dg=dg, i=i: e.tensor_scalar(out=dg[:, :], in0=IDF, scalar1=GTM[:, i, ex:ex + 1], scalar2=None, op0=ALU.mult),
                             reads=[GTM, CST], writes=[dg])
                        S.op("pe", lambda e, dg=dg, ps=ps, q4=q4: e.matmul(ps[:, q4 * 128:(q4 + 1) * 128], lhsT=ONEF, rhs=dg[:, :], start=True, stop=True),
                             reads=[dg, CST, ps], writes=[ps])
                    S.op("act", lambda e, ps=ps, t4=t4: e.activation(out=gb[:, t4 * 512:(t4 + 1) * 512], in_=ps[:, :], func=AF.Copy), reads=[ps], writes=[gbv[t4]])

            make_gb(0)
            for ex in range(8):
                hook = (lambda ex=ex: make_gb(ex + 1)) if ex < 7 else None
                expert(moe_up[ex], moe_dn[ex], 3584, [1, 2, 3, 4], gate=(GBs[ex % 2], GBvs[ex % 2]), hook=hook)
            for t in range(1, 5):
                layer_norm(3, t)
            S.barrier()
            xo = Rot([S.view("dxo%d" % i, GB[:, i * 1024:(i + 1) * 1024]) for i in range(2)])
            OUTB = S.view("OUTB", None)
            for i in range(16):
                t, q4 = 1 + i // 4, i % 4
                ts_ = slice(t * 512 + q4 * 128, t * 512 + (q4 + 1) * 128)
                xb_ = xo.next()
                for half in range(2):
                    ps = psA.next()
                    S.group("pe", [lambda e, j=j, ps=ps, half=half, ts_=ts_: e.transpose(out=ps[:, j * 128:(j + 1) * 128], in_=XF[:, half * 4 + j, ts_], identity=IDF)
                                   for j in range(4)], reads=[CST] + [XFv[k][t] for k in range(half * 4, half * 4 + 4)], writes=[ps])
                    if half == 0:
                        S.op("act", lambda e, ps=ps, xb_=xb_: e.activation(out=xb_[:, 0:512], in_=ps[:, :], func=AF.Copy), reads=[ps], writes=[xb_])
                    else:
                        S.op("dve", lambda e, ps=ps, xb_=xb_: e.tensor_copy(out=xb_[:, 512:1024], in_=ps[:, :]), reads=[ps], writes=[xb_])
                S.dma("sp", out_d[i * 128:(i + 1) * 128, :], xb_[:, :], reads=[xb_], owner=xb_)
            S.barrier()
        S.stack = top
        S.barrier()
    return nc


def _consts():
    i = np.arange(128)
    same = (i[:, None] // 64) == (i[None, :] // 64)
    ident = np.eye(128, dtype=np.float32)
    tri = (same & (i[:, None] <= i[None, :])).astype(np.float32)
    blk = same.astype(np.float32)
    mbl = np.where(same & (i[None, :] < i[:, None]), 0.0, -1e5).astype(np.float32)
    mbu = np.where(same & (i[None, :] >= i[:, None]), 0.0, -1e5).astype(np.float32)
    ones = np.ones((128, 128), np.float32)
    return np.ascontiguousarray(np.stack([ident, tri, blk, mbl, mbu, ones], axis=1))


def make_in_maps(inputs):
    f = lambda a: np.ascontiguousarray(np.asarray(a, dtype=np.float32))
    x = f(inputs["x"])
    q = np.arange(128)[:, None]
    k = np.arange(640)[None, :]
    idx = np.clip(512 + q - k, -128, 128) + 128
    biasT = f(np.asarray(inputs["b_rel_bias"])[0][:, idx])
    cq = q // 64
    wmask = np.where((k >= 64 * cq) & (k < 64 * cq + 576), 0.0, NEG).astype(np.float32)
    lng = np.stack([np.asarray(inputs[n])[l].reshape(8, 128).T for (n, l) in
                    (("ln1_g", 0), ("ln2_g", 0), ("ln1_g", 1), ("ln2_g", 1))], axis=1)
    lnb = np.stack([np.asarray(inputs[n])[l].reshape(8, 128).T for (n, l) in
                    (("ln1_b", 0), ("ln2_b", 0), ("ln1_b", 1), ("ln2_b", 1))], axis=1)
    shared = {
        "w_in": f(inputs["a_w_in"][0]),
        "cw": f(np.asarray(inputs["a_conv_w"])[0].reshape(4, 24, 128).transpose(2, 1, 0)),
        "alog": f(np.broadcast_to(np.asarray(inputs["a_A_log"])[0][None, :], (128, 8))),
        "dtb": f(np.broadcast_to(np.asarray(inputs["a_dt_bias"])[0][None, :], (128, 8))),
        "normg": f(np.asarray(inputs["a_norm_g"])[0].reshape(128, 1)),
        "w_oa": f(inputs["a_w_o"][0]),
        "kv_w": f(inputs["kv_w"]),
        "w_q": f(inputs["b_w_q"][0]),
        "biasT": biasT,
        "wmask": wmask,
        "w_ob": f(inputs["b_w_o"][0]),
        "ffn_up": f(inputs["ffn_w_up"][0]),
        "ffn_dn": f(inputs["ffn_w_down"][0]),
        "router": f(inputs["moe_router"][0]),
        "moe_up": f(inputs["moe_w_up"][0]),
        "moe_dn": f(inputs["moe_w_down"][0]),
        "lng": f(lng),
        "lnb": f(lnb),
        "cmat": _consts(),
    }
    maps = []
    for c in range(8):
        b, half = c // 2, c % 2
        if half == 0:
            xw = np.concatenate([np.zeros((2048, 1024), np.float32), x[b, 0:2048]], axis=0)
            kvalid = np.concatenate([np.full((1, 512), NEG, np.float32), np.zeros((1, 2048), np.float32)], axis=1)
        else:
            xw = x[b]
            kvalid = np.zeros((1, 2560), np.float32)
        m = dict(shared)
        m["x_win"] = np.ascontiguousarray(xw)
        m["kvalid"] = kvalid
        maps.append(m)
    return maps


def kernel(**inputs):
    nc = build()
    maps = make_in_maps(inputs)
    res = run_bass_kernel_spmd(nc, maps, core_ids=list(range(8)))
    out = np.zeros((4, 4096, 1024), np.float32)
    for c in range(8):
        b, half = c // 2, c % 2
        out[b, half * 2048:(half + 1) * 2048] = res.results[c]["out"]
    return out
```

```python
import contextlib
import numpy as np
import concourse.bass as bass
import concourse.mybir as mybir
from concourse.bass_utils import run_bass_kernel_spmd

F32 = mybir.dt.float32
BF16 = mybir.dt.bfloat16
ALU = mybir.AluOpType
AF = mybir.ActivationFunctionType
AX = mybir.AxisListType

ALPHA = 2.0 ** 0.5
EPS = 1e-6
NEG = -30000.0


class Buf:
    __slots__ = ("name", "t", "w", "r", "dsem", "dcnt", "dkey", "psum")

    def __init__(self, name, t):
        self.name = name
        self.t = t
        self.w = None
        self.r = []
        self.dsem = None
        self.dcnt = 0
        self.dkey = None
        self.psum = False

    def __getitem__(self, k):
        return self.t[k]


class Sched:
    ENG = ("pe", "act", "dve", "pool", "sp")

    def __init__(self, nc, stack):
        self.nc = nc
        self.stack = stack
        self.e = {"pe": nc.tensor, "act": nc.scalar, "dve": nc.vector,
                  "pool": nc.gpsimd, "sp": nc.sync}
        self.sem = {}
        self.cnt = {}
        for k in self.ENG:
            self.sem[k] = stack.enter_context(nc.semaphore("s_" + k))
            self.cnt[k] = 0
        self.seen = {k: {} for k in self.ENG}
        self.nbuf = 0
        self.dbufs = []

    def sbuf(self, name, shape, dtype):
        return Buf(name, self.stack.enter_context(self.nc.sbuf_tensor("sb_" + name, list(shape), dtype)))

    def psum(self, name, shape, dtype):
        b = Buf(name, self.stack.enter_context(self.nc.psum_tensor("ps_" + name, list(shape), dtype)))
        b.psum = True
        return b

    def view(self, name, ap):
        return Buf(name, ap)

    def _dsem(self, b):
        if b.dsem is None:
            self.nbuf += 1
            b.dsem = self.stack.enter_context(self.nc.semaphore("d%d" % self.nbuf))
            b.dkey = ("d", self.nbuf)
            self.sem[b.dkey] = b.dsem
            self.dbufs.append(b)
        return b.dkey

    def _wait(self, eng, key, val):
        if val <= 0:
            return
        seen = self.seen[eng]
        if seen.get(key, 0) >= val:
            return
        seen[key] = val
        self.e[eng].wait_ge(self.sem[key], val)

    def _deps(self, eng, reads, writes):
        for b in reads:
            if b.w is not None:
                self._wait(eng, b.w[0], b.w[1])
            if b.psum:
                for (k, v) in b.r:
                    if k != eng:
                        self._wait(eng, k, v)
        for b in writes:
            if b.w is not None:
                self._wait(eng, b.w[0], b.w[1])
            for (k, v) in b.r:
                self._wait(eng, k, v)

    def _mark(self, key, val, reads, writes):
        for b in writes:
            b.w = (key, val)
            b.r = []
        for b in reads:
            if b in writes:
                continue
            b.r = [(k, v) for (k, v) in b.r if k != key]
            b.r.append((key, val))

    def op(self, eng, fn, reads=(), writes=()):
        self._deps(eng, reads, writes)
        ins = fn(self.e[eng])
        self.cnt[eng] += 1
        ins.then_inc(self.sem[eng], 1)
        self._mark(eng, self.cnt[eng], reads, writes)
        return ins

    def group(self, eng, fns, reads=(), writes=()):
        self._deps(eng, reads, writes)
        ins = None
        for fn in fns:
            ins = fn(self.e[eng])
        self.cnt[eng] += 1
        ins.then_inc(self.sem[eng], 1)
        self._mark(eng, self.cnt[eng], reads, writes)
        return ins

    def dma(self, q, out_ap, in_ap, reads=(), writes=(), owner=None):
        self._deps(q, reads, writes)
        if owner is None:
            owner = writes[0] if writes else reads[0]
        key = self._dsem(owner)
        ins = self.e[q].dma_start(out=out_ap, in_=in_ap)
        ins.then_inc(self.sem[key], 16)
        owner.dcnt += 16
        self._mark(key, owner.dcnt, reads, writes)
        return ins

    def barrier(self):
        for eng in self.ENG:
            for k in self.ENG:
                self._wait(eng, k, self.cnt[k])
            for b in self.dbufs:
                self._wait(eng, b.dkey, b.dcnt)

    def drop_dbufs(self, keep):
        self.dbufs = [b for b in self.dbufs if b in keep]


class Rot:
    def __init__(self, bufs):
        self.bufs = bufs
        self.i = 0

    def next(self):
        b = self.bufs[self.i % len(self.bufs)]
        self.i += 1
        return b


def build(stage=99):
    nc = bass.Bass("TRN2", target_bir_lowering=False)

    def din(name, shape, dtype=F32):
        return nc.dram_tensor(name, list(shape), dtype, kind="ExternalInput").ap()

    x_win = din("x_win", [4096, 1024])
    w_in = din("w_in", [1024, 4112])
    cw_d = din("cw", [128, 24, 4])
    alog_d = din("alog", [128, 8])
    dtb_d = din("dtb", [128, 8])
    normg_d = din("normg", [128, 1])
    w_oa = din("w_oa", [1024, 1024])
    kv_w = din("kv_w", [1024, 2048])
    w_q = din("w_q", [1024, 1024])
    biasT = din("biasT", [16, 128, 640])
    wmask_d = din("wmask", [128, 640])
    kvalid_d = din("kvalid", [1, 2560])
    w_ob = din("w_ob", [1024, 1024])
    ffn_up = din("ffn_up", [1024, 5632])
    ffn_dn = din("ffn_dn", [2816, 1024])
    router_d = din("router", [1024, 8])
    if stage >= 20:
        moe_up = din("moe_up", [8, 1024, 7168])
        moe_dn = din("moe_dn", [8, 3584, 1024])
    lng_d = din("lng", [128, 4, 8])
    lnb_d = din("lnb", [128, 4, 8])
    cmat_d = din("cmat", [128, 6, 128])
    out_d = nc.dram_tensor("out", [2048, 1024], F32, kind="ExternalOutput").ap()
    dbg_d = nc.dram_tensor("dbg", [128, 8, 2560], F32, kind="ExternalOutput").ap() if stage < 20 else None

    with contextlib.ExitStack() as top:
        S = Sched(nc, top)
        XF = S.sbuf("XF", [128, 8, 2560], F32)
        CM = S.sbuf("CM", [128, 6, 128], F32)
        CMB = S.sbuf("CMB", [128, 2, 128], BF16)
        LNG = S.sbuf("LNG", [128, 4, 8], F32)
        LNB = S.sbuf("LNB", [128, 4, 8], F32)
        CST = S.view("CST", None)
        S.dma("sp", CM[:], cmat_d[:, :, :], writes=[CST])
        S.dma("sp", LNG[:], lng_d[:, :, :], writes=[CST], owner=CST)
        S.dma("sp", LNB[:], lnb_d[:, :, :], writes=[CST], owner=CST)
        S.op("dve", lambda e: e.tensor_copy(out=CMB[:, 0, :], in_=CM[:, 0, :]), reads=[CST], writes=[CST])
        S.op("dve", lambda e: e.tensor_copy(out=CMB[:, 1, :], in_=CM[:, 5, :]), reads=[CST], writes=[CST])
        if stage < 20:
            S.op("dve", lambda e: e.memset(XF[:], 0.0), writes=[])
        IDF, TRIF, BLKF, MBL, MBU, ONEF = (CM[:, i, :] for i in range(6))
        IDB, ONEB = CMB[:, 0, :], CMB[:, 1, :]
        XFv = [[S.view("XF%d_%d" % (k, t), None) for t in range(5)] for k in range(8)]

        def layer_norm(li, tt, xb_t=None, xbv=None):
            tok = slice(tt * 512, (tt + 1) * 512)
            ps_s = psB.next()
            ps_q = psB.next()
            fns = []
            S.group("pe", [lambda e, k=k: e.matmul(ps_s[:, :], lhsT=ONEF, rhs=XF[:, k, tok], start=(k == 0), stop=(k == 7))
                           for k in range(8)], reads=[CST] + [XFv[k][tt] for k in range(8)], writes=[ps_s])
            sqs = []
            for k in range(8):
                sq = lnsq.next()
                S.op("act", lambda e, sq=sq, k=k: e.activation(out=sq[:, :], in_=XF[:, k, tok], func=AF.Square),
                     reads=[XFv[k][tt]], writes=[sq])
                S.op("pe", lambda e, sq=sq, k=k: e.matmul(ps_q[:, :], lhsT=ONEF, rhs=sq[:, :], start=(k == 0), stop=(k == 7)),
                     reads=[CST, sq], writes=[ps_q])
            mean = lnst.next()
            rstd = lnst.next()
            S.op("act", lambda e: e.activation(out=mean[:, :], in_=ps_s[:, :], func=AF.Copy, scale=1.0 / 1024),
                 reads=[ps_s], writes=[mean])
            m2 = lnsq.next()
            S.op("dve", lambda e: e.tensor_tensor(out=m2[:, :], in0=mean[:, :], in1=mean[:, :], op=ALU.mult),
                 reads=[mean], writes=[m2])
            S.op("dve", lambda e: e.scalar_tensor_tensor(out=rstd[:, :], in0=ps_q[:, :], scalar=1.0 / 1024, in1=m2[:, :],
                                                        op0=ALU.mult, op1=ALU.subtract), reads=[ps_q, m2], writes=[rstd])
            S.op("act", lambda e: e.activation(out=rstd[:, :], in_=rstd[:, :], func=AF.Ln, bias=EPS, scale=1.0),
                 reads=[rstd], writes=[rstd])
            S.op("act", lambda e: e.activation(out=rstd[:, :], in_=rstd[:, :], func=AF.Exp, scale=-0.5),
                 reads=[rstd], writes=[rstd])
            for k in range(8):
                tmp = lnsq.next()
                S.op("dve", lambda e, k=k, tmp=tmp: e.tensor_tensor(out=tmp[:, :], in0=XF[:, k, tok], in1=mean[:, :], op=ALU.subtract),
                     reads=[XFv[k][tt], mean], writes=[tmp])
                S.op("dve", lambda e, tmp=tmp: e.tensor_tensor(out=tmp[:, :], in0=tmp[:, :], in1=rstd[:, :], op=ALU.mult),
                     reads=[tmp, rstd], writes=[tmp])
                S.op("act", lambda e, k=k, tmp=tmp: e.activation(out=XF[:, k, tok], in_=tmp[:, :], func=AF.Identity,
                                                                scale=LNG[:, li, k:k + 1], bias=LNB[:, li, k:k + 1]),
                     reads=[tmp, CST], writes=[XFv[k][tt]])
                if xb_t is not None:
                    S.op("dve", lambda e, k=k: e.tensor_copy(out=xb_t[:, k, tok], in_=XF[:, k, tok]),
                         reads=[XFv[k][tt]], writes=[xbv[k][tt]])

        with contextlib.ExitStack() as pa:
            S.stack = pa
            psA = Rot([S.psum("psA%d" % i, [128, 512], F32) for i in range(3)])
            psB = Rot([S.psum("psB%d" % i, [128, 512], F32) for i in range(2)])
            psW = S.psum("psW", [128, 1024], F32)
            psT_t = S.psum("psT", [128, 1024], BF16)
            psTv = S.view("psTv", psT_t[:, 0:512])
            psTv.psum = True
            psT = Rot([psTv])
            scr = Rot([S.sbuf("scr%d" % i, [128, 512], F32) for i in range(4)])
            lnsq = scr
            lnst = Rot([S.sbuf("lnst%d" % i, [128, 512], F32) for i in range(2)])
            xtm = Rot([S.sbuf("xtm%d" % i, [128, 1024], F32) for i in range(2)])
            xTb = S.sbuf("xTb", [128, 8, 512], BF16)
            wpan = Rot([S.sbuf("wpan%d" % i, [128, 8, 256], BF16) for i in range(3)])
            wba = S.sbuf("wba", [128, 8, 16], BF16)
            qT = S.sbuf("qT", [128, 8, 512], BF16)
            kT = S.sbuf("kT", [128, 8, 512], BF16)
            vT = S.sbuf("vT", [128, 8, 512], BF16)
            OT = vT
            OG = qT
            halo = S.sbuf("halo", [128, 24, 3], F32)
            cwt = S.sbuf("cwt", [128, 24, 4], F32)
            ubuf = Rot([S.sbuf("ubuf%d" % i, [128, 515], F32) for i in range(2)])
            ycv = scr
            sqb = Rot([S.sbuf("sqb%d" % i, [128, 512], BF16) for i in range(1)])
            alog = S.sbuf("alog", [128, 8], F32)
            dtb = S.sbuf("dtb", [128, 8], F32)
            negA = S.sbuf("negA", [128, 8], F32)
            normg = S.sbuf("normg", [128, 1], F32)
            PB = []
            for par in range(2):
                PB.append((S.sbuf("ba%d" % par, [128, 16], F32), S.sbuf("beta%d" % par, [128, 8], F32), S.sbuf("gt%d" % par, [128, 8], F32),
                           S.sbuf("gcum%d" % par, [128, 8], F32), S.sbuf("glast%d" % par, [128, 8], F32), S.sbuf("bexp%d" % par, [128, 8], F32),
                           S.sbuf("kdsc%d" % par, [128, 8], F32), S.sbuf("DECB%d" % par, [128, 8, 128], BF16), S.sbuf("DECT%d" % par, [128, 8, 128], BF16),
                           S.sbuf("EROW%d" % par, [128, 8, 128], BF16), S.sbuf("GLS%d" % par, [128, 8, 2], F32)))
            tmpW = S.sbuf("tmpW", [128, 8, 128], F32)
            Dm = tmpW
            Sf = [S.sbuf("Sf%d" % g, [128, 4, 128], F32) for g in range(2)]
            Sb = [S.sbuf("Sb%d" % g, [128, 4, 128], BF16) for g in range(2)]
            ppc = [[S.sbuf("pp%d_%d" % (g, i), [128, 4, 128], BF16) for i in range(4)] for g in range(2)]
            gbufc = [{n: S.sbuf("g%d_%s" % (g, n), [128, 4, 128], BF16) for n in ("Tt", "Xw", "kd", "Xu", "wT", "QKT", "qd", "VN0", "VN1")} for g in range(2)]
            ufc = [S.sbuf("uf%d" % g, [128, 4, 128], F32) for g in range(2)]

            def run_chains(gens):
                gens = list(gens)
                while gens:
                    for gc_ in list(gens):
                        try:
                            next(gc_)
                        except StopIteration:
                            gens.remove(gc_)

            S.dma("sp", cwt[:], cw_d[:, :, :], writes=[CST], owner=CST)
            S.dma("sp", alog[:], alog_d[:, :], writes=[CST], owner=CST)
            S.dma("sp", dtb[:], dtb_d[:, :], writes=[CST], owner=CST)
            S.dma("sp", normg[:], normg_d[:, :], writes=[CST], owner=CST)
            S.dma("pool", wba[:], w_in[:, 4096:4112].rearrange("(k p) m -> p k m", p=128), writes=[wba])
            S.op("act", lambda e: e.activation(out=negA[:], in_=alog[:], func=AF.Exp), reads=[CST], writes=[CST])
            S.op("dve", lambda e: e.tensor_scalar(out=negA[:], in0=negA[:], scalar1=-1.0, scalar2=None, op0=ALU.mult),
                 reads=[CST], writes=[CST])
            S.op("dve", lambda e: e.memset(halo[:], 0.0), writes=[halo])
            for g in range(2):
                S.op("dve", lambda e, g=g: e.memset(Sf[g][:], 0.0), writes=[Sf[g]])
                S.op("dve", lambda e, g=g: e.memset(Sb[g][:], 0.0), writes=[Sb[g]])

            def load_panel(col0, ncols=256, src=w_in):
                wp = wpan.next()
                S.dma("pool", wp[:, :, 0:ncols], src[:, col0:col0 + ncols].rearrange("(k p) m -> p k m", p=128), writes=[wp])
                return wp

            stop = False
            for st in range(8):
                if stop:
                    break
                full = st >= 3
                ft = st - 3
                for tq in range(4):
                    xt = xtm.next()
                    r0 = st * 512 + tq * 128
                    S.dma("sp", xt[:], x_win[r0:r0 + 128, :], writes=[xt])
                    for half in range(2):
                        ps = psA.next()
                        S.group("pe", [lambda e, j=j, ps=ps, xt=xt, half=half: e.transpose(
                            out=ps[:, j * 128:(j + 1) * 128], in_=xt[:, (half * 4 + j) * 128:(half * 4 + j + 1) * 128], identity=IDF)
                            for j in range(4)], reads=[xt, CST], writes=[ps])
                        S.op("act", lambda e, ps=ps, half=half, tq=tq: e.activation(
                            out=xTb[:, half * 4:half * 4 + 4, tq * 128:(tq + 1) * 128],
                            in_=ps[:, :].rearrange("p (j t) -> p j t", j=4), func=AF.Copy), reads=[ps], writes=[xTb])
                        if full:
                            S.op("dve", lambda e, ps=ps, half=half, tq=tq: e.tensor_scalar(
                                out=XF[:, half * 4:half * 4 + 4, ft * 512 + tq * 128:ft * 512 + (tq + 1) * 128],
                                in0=ps[:, :].rearrange("p (j t) -> p j t", j=4), scalar1=ALPHA, scalar2=None, op0=ALU.mult),
                                reads=[ps], writes=[XFv[k][ft] for k in range(half * 4, half * 4 + 4)])
                if stage == 1:
                    stop = True
                    continue
                secs = [("k", 1024, kT, 8), ("v", 2048, vT, 16)] + ([("q", 0, qT, 0)] if st >= 2 else [])
                pend = []
                for (nm, c0, dst, m0) in secs:
                    for pn in range(4):
                        wp = load_panel(c0 + pn * 256)
                        for j in range(2):
                            hh = pn * 2 + j
                            ps = psA.next()
                            S.group("pe", [lambda e, k=k, ps=ps, wp=wp, j=j: e.matmul(
                                ps[:, :], lhsT=wp[:, k, j * 128:(j + 1) * 128], rhs=xTb[:, k, :], start=(k == 0), stop=(k == 7))
                                for k in range(8)], reads=[wp, xTb], writes=[ps])
                            ub = ubuf.next()
                            m = m0 + hh
                            S.op("act", lambda e, ub=ub, ps=ps: e.activation(out=ub[:, 3:515], in_=ps[:, :], func=AF.Copy),
                                 reads=[ps], writes=[ub])
                            S.op("act", lambda e, ub=ub, m=m: e.activation(out=ub[:, 0:3], in_=halo[:, m, :], func=AF.Copy),
                                 reads=[halo], writes=[ub])
                            S.op("act", lambda e, ub=ub, m=m: e.activation(out=halo[:, m, :], in_=ub[:, 512:515], func=AF.Copy),
                                 reads=[ub], writes=[halo])
                            if nm == "q" and not full:
                                continue
                            y = ycv.next()
                            S.op("act", lambda e, ps=ps, y=y, m=m: e.activation(out=y[:, :], in_=ps[:, :], func=AF.Copy, scale=cwt[:, m, 3:4]),
                                 reads=[ps, CST], writes=[y])
                            for jj in range(3):
                                S.op("dve", lambda e, ub=ub, y=y, m=m, jj=jj: e.scalar_tensor_tensor(
                                    out=y[:, :], in0=ub[:, jj:jj + 512], scalar=cwt[:, m, jj:jj + 1], in1=y[:, :],
                                    op0=ALU.mult, op1=ALU.add), reads=[ub, y, CST], writes=[y])
                            pend.append((nm, dst, hh, y))
                            if len(pend) == 4:
                                for (nm2, dst2, h2, y2) in pend:
                                    if nm2 == "v":
                                        S.op("act", lambda e, dst2=dst2, h2=h2, y2=y2: e.activation(
                                            out=dst2[:, h2, :], in_=y2[:, :], func=AF.Silu), reads=[y2], writes=[dst2])
                                    else:
                                        S.op("act", lambda e, y2=y2: e.activation(out=y2[:, :], in_=y2[:, :], func=AF.Silu),
                                             reads=[y2], writes=[y2])
                                for (nm2, dst2, h2, y2) in pend:
                                    if nm2 == "v":
                                        continue
                                    sq = sqb.next()
                                    S.op("act", lambda e, sq=sq, y2=y2: e.activation(out=sq[:, :], in_=y2[:, :], func=AF.Square),
                                         reads=[y2], writes=[sq])
                                    pss = psB.next()
                                    S.op("pe", lambda e, sq=sq, pss=pss: e.matmul(pss[:, :], lhsT=ONEB, rhs=sq[:, :], start=True, stop=True),
                                         reads=[sq, CST], writes=[pss])
                                    rn = lnst.next()
                                    S.op("act", lambda e, rn=rn, pss=pss: e.activation(out=rn[:, :], in_=pss[:, :], func=AF.Ln, bias=EPS, scale=1.0),
                                         reads=[pss], writes=[rn])
                                    S.op("act", lambda e, rn=rn: e.activation(out=rn[:, :], in_=rn[:, :], func=AF.Exp, scale=-0.5),
                                         reads=[rn], writes=[rn])
                                    if nm2 == "q":
                                        S.op("dve", lambda e, dst2=dst2, h2=h2, y2=y2, rn=rn: e.scalar_tensor_tensor(
                                            out=dst2[:, h2, :], in0=y2[:, :], scalar=128.0 ** -0.5, in1=rn[:, :],
                                            op0=ALU.mult, op1=ALU.mult), reads=[y2, rn], writes=[dst2])
                                    else:
                                        S.op("dve", lambda e, dst2=dst2, h2=h2, y2=y2, rn=rn: e.tensor_tensor(
                                            out=dst2[:, h2, :], in0=y2[:, :], in1=rn[:, :], op=ALU.mult), reads=[y2, rn], writes=[dst2])
                                pend = []
                if stage == 2:
                    stop = True
                    continue
                def pre(tq):
                    ba, beta, gt, gcum, glast, bexp, kdsc, DECB, DECT, EROW, GLS = PB[tq % 2]
                    tcol = slice(tq * 128, (tq + 1) * 128)
                    psb = psB.next()
                    S.group("pe", [lambda e, k=k, psb=psb: e.matmul(psb[:, 0:16], lhsT=xTb[:, k, tcol], rhs=wba[:, k, :],
                                                                  start=(k == 0), stop=(k == 7)) for k in range(8)],
                            reads=[xTb, wba], writes=[psb])
                    S.op("act", lambda e, psb=psb: e.activation(out=ba[:, :], in_=psb[:, 0:16], func=AF.Copy), reads=[psb], writes=[ba])
                    S.op("act", lambda e: e.activation(out=beta[:, :], in_=ba[:, 0:8], func=AF.Exp, scale=-1.0), reads=[ba], writes=[beta])
                    S.op("dve", lambda e: e.tensor_scalar(out=beta[:, :], in0=beta[:, :], scalar1=1.0, scalar2=None, op0=ALU.add),
                         reads=[beta], writes=[beta])
                    S.op("dve", lambda e: e.reciprocal(out=beta[:, :], in_=beta[:, :]), reads=[beta], writes=[beta])
                    yield
                    S.op("dve", lambda e: e.tensor_tensor(out=gt[:, :], in0=ba[:, 8:16], in1=dtb[:, :], op=ALU.add), reads=[ba, CST], writes=[gt])
                    S.op("act", lambda e: e.activation(out=gt[:, :], in_=gt[:, :], func=AF.Exp), reads=[gt], writes=[gt])
                    S.op("act", lambda e: e.activation(out=gt[:, :], in_=gt[:, :], func=AF.Ln, bias=1.0, scale=1.0), reads=[gt], writes=[gt])
                    S.op("dve", lambda e: e.tensor_tensor(out=gt[:, :], in0=gt[:, :], in1=negA[:, :], op=ALU.mult), reads=[gt, CST], writes=[gt])
                    yield
                    psb = psB.next()
                    S.op("pe", lambda e, psb=psb: e.matmul(psb[:, 0:8], lhsT=TRIF, rhs=gt[:, :], start=True, stop=True), reads=[gt, CST], writes=[psb])
                    S.op("pe", lambda e, psb=psb: e.matmul(psb[:, 8:16], lhsT=BLKF, rhs=gt[:, :], start=True, stop=True), reads=[gt, CST, psb], writes=[psb])
                    S.op("act", lambda e, psb=psb: e.activation(out=gcum[:, :], in_=psb[:, 0:8], func=AF.Copy), reads=[psb], writes=[gcum])
                    S.op("act", lambda e, psb=psb: e.activation(out=glast[:, :], in_=psb[:, 8:16], func=AF.Copy), reads=[psb], writes=[glast])
                    yield
                    S.op("act", lambda e: e.activation(out=bexp[:, :], in_=gcum[:, :], func=AF.Exp), reads=[gcum], writes=[bexp])
                    S.op("dve", lambda e: e.tensor_tensor(out=bexp[:, :], in0=bexp[:, :], in1=beta[:, :], op=ALU.mult), reads=[bexp, beta], writes=[bexp])
                    S.op("dve", lambda e: e.tensor_tensor(out=kdsc[:, :], in0=glast[:, :], in1=gcum[:, :], op=ALU.subtract), reads=[glast, gcum], writes=[kdsc])
                    S.op("act", lambda e: e.activation(out=kdsc[:, :], in_=kdsc[:, :], func=AF.Exp), reads=[kdsc], writes=[kdsc])
                    yield
                    S.op("dve", lambda e: e.tensor_tensor(out=Dm[:, :, :], in0=gcum[:, :].unsqueeze(2).to_broadcast([128, 8, 128]),
                                                          in1=IDF.unsqueeze(1).to_broadcast([128, 8, 128]), op=ALU.mult),
                         reads=[gcum, CST], writes=[Dm])
                    S.group("pe", [lambda e, hf=hf: e.matmul(psW[:, hf * 512:(hf + 1) * 512], lhsT=ONEF,
                                                           rhs=Dm[:, hf * 4:hf * 4 + 4, :].rearrange("p h j -> p (h j)"), start=True, stop=True)
                                   for hf in range(2)], reads=[Dm, CST], writes=[psW])
                    R3 = psW[:, :].rearrange("p (h j) -> p h j", h=8)
                    gc_b = gcum[:, :].unsqueeze(2).to_broadcast([128, 8, 128])
                    S.op("dve", lambda e: e.tensor_tensor(out=tmpW[:, :, :], in0=gc_b, in1=R3, op=ALU.subtract), reads=[gcum, psW], writes=[tmpW])
                    S.op("dve", lambda e: e.tensor_tensor(out=tmpW[:, :, :], in0=tmpW[:, :, :], in1=MBL.unsqueeze(1).to_broadcast([128, 8, 128]), op=ALU.add),
                         reads=[tmpW, CST], writes=[tmpW])
                    S.op("act", lambda e: e.activation(out=tmpW[:, :, :], in_=tmpW[:, :, :], func=AF.Exp), reads=[tmpW], writes=[tmpW])
                    S.op("dve", lambda e: e.tensor_tensor(out=DECB[:, :, :], in0=tmpW[:, :, :], in1=beta[:, :].unsqueeze(2).to_broadcast([128, 8, 128]), op=ALU.mult),
                         reads=[tmpW, beta], writes=[DECB])
                    yield
                    S.op("dve", lambda e: e.tensor_tensor(out=tmpW[:, :, :], in0=R3, in1=gc_b, op=ALU.subtract), reads=[gcum, psW, tmpW], writes=[tmpW])
                    S.op("dve", lambda e: e.tensor_tensor(out=tmpW[:, :, :], in0=tmpW[:, :, :], in1=MBU.unsqueeze(1).to_broadcast([128, 8, 128]), op=ALU.add),
                         reads=[tmpW, CST], writes=[tmpW])
                    S.op("act", lambda e: e.activation(out=DECT[:, :, :], in_=tmpW[:, :, :], func=AF.Exp), reads=[tmpW], writes=[DECT])
                    yield
                    S.op("act", lambda e: e.activation(out=EROW[:, :, :], in_=R3, func=AF.Exp), reads=[psW], writes=[EROW])
                    S.op("act", lambda e: e.activation(out=GLS[:, :, :], in_=R3[:, :, 63:128:64], func=AF.Exp), reads=[psW], writes=[GLS])

                    yield

                def unit(g, tq):
                    tcol = slice(tq * 128, (tq + 1) * 128)
                    ba, beta, gt, gcum, glast, bexp, kdsc, DECB, DECT, EROW, GLS = PB[tq % 2]
                    pp = ppc[g]
                    gbuf = gbufc[g]
                    hs = slice(g * 4, g * 4 + 4)

                    def mm4(ps, lfn, rfn, reads):
                        S.group("pe", [lambda e, a=a: e.matmul(ps[:, a * 128:(a + 1) * 128], lhsT=lfn(a), rhs=rfn(a), start=True, stop=True)
                                       for a in range(4)], reads=reads, writes=[ps])

                    def tr4(ps, ifn, reads):
                        S.group("pe", [lambda e, a=a: e.transpose(out=ps[:, a * 128:(a + 1) * 128], in_=ifn(a), identity=IDB)
                                       for a in range(4)], reads=reads + [CST], writes=[ps])

                    def ev(dst, ps, eng="act"):
                        if eng == "act":
                            S.op("act", lambda e: e.activation(out=dst[:, :, :].rearrange("p a j -> p (a j)"), in_=ps[:, :], func=AF.Copy),
                                 reads=[ps], writes=[dst])
                        else:
                            S.op("dve", lambda e: e.tensor_copy(out=dst[:, :, :].rearrange("p a j -> p (a j)"), in_=ps[:, :]),
                                 reads=[ps], writes=[dst])

                    def p3(ps):
                        return ps[:, :].rearrange("p (a j) -> p a j", a=4)

                    ps = psA.next()
                    mm4(ps, lambda a: kT[:, g * 4 + a, tcol], lambda a: kT[:, g * 4 + a, tcol], [kT])
                    A = pp[0]
                    S.op("dve", lambda e: e.tensor_tensor(out=A[:, :, :], in0=p3(ps), in1=DECB[:, hs, :], op=ALU.mult), reads=[ps, DECB], writes=[A])
                    yield
                    pst = psT.next()
                    tr4(pst, lambda a: A[:, a, :], [A])
                    AT = pp[1]
                    ev(AT, pst, "act")
                    Tt = gbuf["Tt"]
                    S.op("dve", lambda e: e.tensor_tensor(out=Tt[:, :, :], in0=IDF.unsqueeze(1).to_broadcast([128, 4, 128]),
                                                          in1=pst[:, :].rearrange("p (a j) -> p a j", a=4), op=ALU.subtract),
                         reads=[pst, CST], writes=[Tt])
                    yield
                    P, PT = A, AT
                    for lvl in range(5):
                        ps = psA.next()
                        mm4(ps, lambda a: PT[:, a, :], lambda a: P[:, a, :], [P, PT])
                        P2 = pp[2] if P is pp[0] else pp[0]
                        ev(P2, ps, "act")
                        yield
                        P2T = None
                        if lvl < 4:
                            ps2 = psA.next()
                            mm4(ps2, lambda a: P[:, a, :], lambda a: PT[:, a, :], [P, PT])
                            P2T = pp[3] if PT is pp[1] else pp[1]
                            ev(P2T, ps2, "act")
                            yield
                        ps3 = psA.next()
                        mm4(ps3, lambda a: P2[:, a, :], lambda a: Tt[:, a, :], [P2, Tt])
                        S.op("dve", lambda e: e.tensor_tensor(out=Tt[:, :, :], in0=Tt[:, :, :], in1=p3(ps3), op=ALU.add), reads=[ps3, Tt], writes=[Tt])
                        yield
                        P, PT = P2, P2T
                    pst = psT.next()
                    tr4(pst, lambda a: kT[:, g * 4 + a, tcol], [kT])
                    Xw = gbuf["Xw"]
                    kd = gbuf["kd"]
                    pk3 = pst[:, :].rearrange("p (a j) -> p a j", a=4)
                    S.op("dve", lambda e: e.tensor_tensor(out=Xw[:, :, :], in0=pk3, in1=bexp[:, hs].unsqueeze(2).to_broadcast([128, 4, 128]), op=ALU.mult),
                         reads=[pst, bexp], writes=[Xw])
                    S.op("dve", lambda e: e.tensor_tensor(out=kd[:, :, :], in0=pk3, in1=kdsc[:, hs].unsqueeze(2).to_broadcast([128, 4, 128]), op=ALU.mult),
                         reads=[pst, kdsc], writes=[kd])
                    yield
                    pst = psT.next()
                    tr4(pst, lambda a: vT[:, g * 4 + a, tcol], [vT])
                    Xu = gbuf["Xu"]
                    pv3 = pst[:, :].rearrange("p (a j) -> p a j", a=4)
                    S.op("dve", lambda e: e.tensor_tensor(out=Xu[:, :, :], in0=pv3, in1=beta[:, hs].unsqueeze(2).to_broadcast([128, 4, 128]), op=ALU.mult),
                         reads=[pst, beta], writes=[Xu])
                    yield
                    ps = psA.next()
                    mm4(ps, lambda a: Tt[:, a, :], lambda a: Xu[:, a, :], [Tt, Xu])
                    u = ufc[g]
                    ev(u, ps, "act")
                    yield
                    ps = psA.next()
                    mm4(ps, lambda a: Xw[:, a, :], lambda a: Tt[:, a, :], [Tt, Xw])
                    wT = gbuf["wT"]
                    ev(wT, ps, "act")
                    yield
                    if full:
                        ps = psA.next()
                        mm4(ps, lambda a: kT[:, g * 4 + a, tcol], lambda a: qT[:, g * 4 + a, tcol], [kT, qT])
                        QKT = gbuf["QKT"]
                        S.op("dve", lambda e: e.tensor_tensor(out=QKT[:, :, :], in0=p3(ps), in1=DECT[:, hs, :], op=ALU.mult), reads=[ps, DECT], writes=[QKT])
                        qd = gbuf["qd"]
                        S.op("pool", lambda e: e.tensor_tensor(out=qd[:, :, :], in0=qT[:, hs, tcol], in1=EROW[:, hs, :], op=ALU.mult),
                             reads=[qT, EROW], writes=[qd])
                        yield
                    for c in range(2):
                        pr = slice(c * 64, c * 64 + 64)
                        ps = psA.next()
                        mm4(ps, lambda a: wT[:, a, :], lambda a: Sb[g][:, a, :], [wT, Sb[g]])
                        VN = gbuf["VN%d" % c]
                        S.op("dve", lambda e: e.tensor_tensor(out=VN[pr, :, :], in0=u[pr, :, :], in1=p3(ps)[pr, :, :], op=ALU.subtract),
                             reads=[u, ps], writes=[VN])
                        yield
                        if full:
                            pso = psB.next()
                            fns = []
                            for a in range(4):
                                fns.append(lambda e, a=a: e.matmul(pso[:, a * 64:(a + 1) * 64], lhsT=Sb[g][:, a, :], rhs=qd[:, a, pr], start=True, stop=False))
                                fns.append(lambda e, a=a: e.matmul(pso[:, a * 64:(a + 1) * 64], lhsT=VN[pr, a, :], rhs=QKT[pr, a, pr], start=False, stop=True))
                            S.group("pe", fns, reads=[Sb[g], qd, VN, QKT], writes=[pso])
                            S.op("act", lambda e: e.activation(
                                out=OT[:, hs, tq * 128 + c * 64:tq * 128 + c * 64 + 64],
                                in_=pso[:, 0:256].rearrange("p (a t) -> p a t", a=4), func=AF.Copy), reads=[pso], writes=[OT])
                            yield
                        ps = psA.next()
                        mm4(ps, lambda a: kd[pr, a, :], lambda a: VN[pr, a, :], [kd, VN])
                        gl = GLS[:, hs, c:c + 1].to_broadcast([128, 4, 128])
                        S.op("dve", lambda e: e.tensor_tensor(out=Sf[g][:, :, :], in0=Sf[g][:, :, :], in1=gl, op=ALU.mult), reads=[Sf[g], GLS], writes=[Sf[g]])
                        S.op("dve", lambda e: e.tensor_tensor(out=Sf[g][:, :, :], in0=Sf[g][:, :, :], in1=p3(ps), op=ALU.add), reads=[Sf[g], ps], writes=[Sf[g]])
                        S.op("act", lambda e: e.activation(out=Sb[g][:, :, :], in_=Sf[g][:, :, :], func=AF.Copy), reads=[Sf[g]], writes=[Sb[g]])
                        yield

                pre_done = [False] * 4
                unit_done = [[False] * 4 for _ in range(2)]

                def pre_all():
                    for tq in range(4):
                        while tq >= 2 and not (unit_done[0][tq - 2] and unit_done[1][tq - 2]):
                            yield
                        yield from pre(tq)
                        pre_done[tq] = True

                def units(g):
                    for tq in range(4):
                        while not pre_done[tq]:
                            yield
                        yield from unit(g, tq)
                        unit_done[g][tq] = True

                run_chains([pre_all(), units(0), units(1)])
                if stage == 5:
                    stop = True
                    continue
                if not full:
                    continue
                rstds = []
                for h in range(8):
                    sq = sqb.next()
                    S.op("act", lambda e, sq=sq, h=h: e.activation(out=sq[:, :], in_=OT[:, h, :], func=AF.Square), reads=[OT], writes=[sq])
                    pss = psB.next()
                    S.op("pe", lambda e, sq=sq, pss=pss: e.matmul(pss[:, :], lhsT=ONEB, rhs=sq[:, :], start=True, stop=True), reads=[sq, CST], writes=[pss])
                    rn = lnst.next()
                    S.op("act", lambda e, rn=rn, pss=pss: e.activation(out=rn[:, :], in_=pss[:, :], func=AF.Ln, bias=EPS, scale=1.0 / 128), reads=[pss], writes=[rn])
                    S.op("act", lambda e, rn=rn: e.activation(out=rn[:, :], in_=rn[:, :], func=AF.Exp, scale=-0.5), reads=[rn], writes=[rn])
                    S.op("dve", lambda e, rn=rn, h=h: e.scalar_tensor_tensor(out=OT[:, h, :], in0=OT[:, h, :], scalar=normg[:, 0:1], in1=rn[:, :],
                                                                          op0=ALU.mult, op1=ALU.mult), reads=[OT, rn, CST], writes=[OT])
                for pn in range(4):
                    wp = load_panel(3072 + pn * 256)
                    for j in range(2):
                        h = pn * 2 + j
                        ps = psA.next()
                        S.group("pe", [lambda e, k=k, ps=ps, wp=wp, j=j: e.matmul(
                            ps[:, :], lhsT=wp[:, k, j * 128:(j + 1) * 128], rhs=xTb[:, k, :], start=(k == 0), stop=(k == 7))
                            for k in range(8)], reads=[wp, xTb], writes=[ps])
                        zs = ycv.next()
                        S.op("act", lambda e, zs=zs, ps=ps: e.activation(out=zs[:, :], in_=ps[:, :], func=AF.Silu), reads=[ps], writes=[zs])
                        S.op("dve", lambda e, zs=zs, h=h: e.tensor_tensor(out=OG[:, h, :], in0=OT[:, h, :], in1=zs[:, :], op=ALU.mult),
                             reads=[OT, zs], writes=[OG])
                tok = slice(ft * 512, (ft + 1) * 512)
                for pn in range(4):
                    wp = load_panel(pn * 256, src=w_oa)
                    for j in range(2):
                        m = pn * 2 + j
                        ps = psA.next()
                        S.group("pe", [lambda e, k=k, ps=ps, wp=wp, j=j: e.matmul(
                            ps[:, :], lhsT=wp[:, k, j * 128:(j + 1) * 128], rhs=OG[:, k, :], start=(k == 0), stop=(k == 7))
                            for k in range(8)], reads=[wp, OG], writes=[ps])
                        S.op("dve", lambda e, ps=ps, m=m: e.tensor_tensor(out=XF[:, m, tok], in0=XF[:, m, tok], in1=ps[:, :], op=ALU.add),
                             reads=[ps, XFv[m][ft]], writes=[XFv[m][ft]])
                layer_norm(0, ft)
                if stage == 6:
                    stop = True
            S.barrier()
        S.stack = top


        def finish_dbg():
            allx = [XFv[k][t] for k in range(8) for t in range(5)]
            S.dma("sp", dbg_d[:, :, :], XF[:], reads=allx, owner=CST)
            S._wait("sp", CST.dkey, CST.dcnt)

        if stage < 10:
            finish_dbg()
            return nc

        XB = S.sbuf("XB", [128, 8, 2560], BF16)
        XBv = [[S.view("XB%d_%d" % (k, t), None) for t in range(5)] for k in range(8)]

        def tokc(t):
            return slice(t * 512, (t + 1) * 512)

        def make_xb(tts):
            for t in tts:
                for k in range(8):
                    if k % 2 == 0:
                        S.op("act", lambda e, k=k, t=t: e.activation(out=XB[:, k, tokc(t)], in_=XF[:, k, tokc(t)], func=AF.Copy),
                             reads=[XFv[k][t]], writes=[XBv[k][t]])
                    else:
                        S.op("dve", lambda e, k=k, t=t: e.tensor_copy(out=XB[:, k, tokc(t)], in_=XF[:, k, tokc(t)]),
                             reads=[XFv[k][t]], writes=[XBv[k][t]])

        def scale_xf(tts):
            for t in tts:
                for k in range(8):
                    S.op("dve", lambda e, k=k, t=t: e.tensor_scalar(out=XF[:, k, tokc(t)], in0=XF[:, k, tokc(t)], scalar1=ALPHA, scalar2=None, op0=ALU.mult),
                         reads=[XFv[k][t]], writes=[XFv[k][t]])

        def expert(up_src, dn_src, F, tts, gate=None, hook=None):
            nf = F // 128
            for c0 in range(0, nf, 4):
                cf = min(4, nf - c0)
                for p0 in range(c0, c0 + cf, 2):
                    npn = min(2, c0 + cf - p0)
                    wp = uppan.next()
                    S.dma("pool", wp[:, :, 0:npn * 128], up_src[:, p0 * 128:(p0 + npn) * 128].rearrange("(k p) m -> p k m", p=128), writes=[wp])
                    S.dma("pool", wp[:, :, 256:256 + npn * 128], up_src[:, F + p0 * 128:F + (p0 + npn) * 128].rearrange("(k p) m -> p k m", p=128), writes=[wp])
                    for ti, t in enumerate(tts):
                        for j in range(npn):
                            fj = p0 + j - c0
                            psg = psA.next()
                            S.group("pe", [lambda e, k=k, psg=psg, wp=wp, j=j, t=t: e.matmul(
                                psg[:, :], lhsT=wp[:, k, j * 128:(j + 1) * 128], rhs=XB[:, k, tokc(t)], start=(k == 0), stop=(k == 7))
                                for k in range(8)], reads=[wp] + [XBv[k][t] for k in range(8)], writes=[psg])
                            psu = psA.next()
                            S.group("pe", [lambda e, k=k, psu=psu, wp=wp, j=j, t=t: e.matmul(
                                psu[:, :], lhsT=wp[:, k, 256 + j * 128:256 + (j + 1) * 128], rhs=XB[:, k, tokc(t)], start=(k == 0), stop=(k == 7))
                                for k in range(8)], reads=[wp] + [XBv[k][t] for k in range(8)], writes=[psu])
                            sg = sgp.next()
                            S.op("act", lambda e, sg=sg, psg=psg: e.activation(out=sg[:, :], in_=psg[:, :], func=AF.Silu), reads=[psg], writes=[sg])
                            if gate is not None:
                                S.op("dve", lambda e, sg=sg, ti=ti: e.tensor_tensor(out=sg[:, :], in0=sg[:, :], in1=gate[0][:, ti * 512:(ti + 1) * 512], op=ALU.mult),
                                     reads=[sg, gate[1][ti]], writes=[sg])
                            S.op("dve", lambda e, sg=sg, psu=psu, fj=fj, ti=ti: e.tensor_tensor(
                                out=ACT_[:, fj, ti * 512:(ti + 1) * 512], in0=sg[:, :], in1=psu[:, :], op=ALU.mult),
                                reads=[sg, psu], writes=[ACTv[fj][ti]])
                wd = dnpan.next()
                S.dma("pool", wd[:, 0:cf, :], dn_src[c0 * 128:(c0 + cf) * 128, :].rearrange("(f p) m -> p f m", p=128), writes=[wd])
                for ti, t in enumerate(tts):
                    for m in range(8):
                        ps = psB.next()
                        S.group("pe", [lambda e, j=j, ps=ps, wd=wd, m=m, ti=ti: e.matmul(
                            ps[:, :], lhsT=wd[:, j, m * 128:(m + 1) * 128], rhs=ACT_[:, j, ti * 512:(ti + 1) * 512], start=(j == 0), stop=(j == cf - 1))
                            for j in range(cf)], reads=[wd] + [ACTv[j][ti] for j in range(cf)], writes=[ps])
                        S.op("dve", lambda e, ps=ps, m=m, t=t: e.tensor_tensor(out=XF[:, m, tokc(t)], in0=XF[:, m, tokc(t)], in1=ps[:, :], op=ALU.add),
                             reads=[ps, XFv[m][t]], writes=[XFv[m][t]])
                if c0 == 0 and hook is not None:
                    hook()

        with contextlib.ExitStack() as pb:
            S.stack = pb
            psA = Rot([S.psum("bpsA%d" % i, [128, 512], F32) for i in range(4)])
            psB = Rot([S.psum("bpsB%d" % i, [128, 512], F32) for i in range(4)])
            scr = Rot([S.sbuf("bscr%d" % i, [128, 512], F32) for i in range(4)])
            lnsq = scr
            lnst = Rot([S.sbuf("blnst%d" % i, [128, 512], F32) for i in range(3)])
            uppan = Rot([S.sbuf("buppan%d" % i, [128, 8, 512], BF16) for i in range(2)])
            dnpan = Rot([S.sbuf("bdnpan%d" % i, [128, 4, 1024], BF16) for i in range(2)])
            sgp = Rot([S.sbuf("bsg%d" % i, [128, 512], F32) for i in range(2)])
            ACT_ = S.sbuf("bact", [128, 4, 2560], BF16)
            ACTv = [[S.view("bact%d_%d" % (j, t), None) for t in range(5)] for j in range(4)]
            make_xb(range(5))
            scale_xf(range(5))
            expert(ffn_up, ffn_dn, 2816, list(range(5)))
            for t in range(5):
                layer_norm(1, t, xb_t=XB, xbv=XBv)
            S.barrier()
        S.stack = top
        if stage == 10:
            finish_dbg()
            return nc

        with contextlib.ExitStack() as pc:
            S.stack = pc
            scale_xf(range(1, 5))
            psS = Rot([S.psum("cpsS%d" % i, [128, 1024], F32) for i in range(3)])
            psT_t = S.psum("cpsT", [128, 1024], BF16)
            psTv = S.view("cpsTv", psT_t[:, 0:640])
            psTv.psum = True
            psO1 = S.psum("cpsO", [128, 512], F32)
            psO = psS
            cpan = Rot([S.sbuf("cpan%d" % i, [128, 8, 256], BF16) for i in range(3)])
            KT2 = S.sbuf("KT2", [128, 2, 2560], BF16)
            QT2 = S.sbuf("QT2", [128, 2, 2048], BF16)
            VT = S.sbuf("VT", [128, 20, 256], BF16)
            BM = S.sbuf("BM", [128, 4, 640], BF16)
            WM = S.sbuf("WM", [128, 640], BF16)
            KV1 = S.sbuf("KV1", [1, 2560], BF16)
            ONE1 = S.sbuf("ONE1", [1, 128], BF16)
            pexp = Rot([S.sbuf("cpe%d" % i, [128, 640], BF16) for i in range(3)])
            PTs = Rot([S.sbuf("cPT%d" % i, [128, 5, 128], BF16) for i in range(2)])
            OTM = Rot([S.sbuf("cOTM%d" % i, [128, 256], BF16) for i in range(2)])
            OTF = S.sbuf("cOTF", [128, 2, 2048], BF16)
            wob = S.sbuf("cwob", [128, 2, 1024], BF16)
            st4 = Rot([S.sbuf("cst%d" % i, [128, 4], F32) for i in range(6)])
            S.dma("pool", WM[:], wmask_d[:, :], writes=[WM])
            S.dma("pool", KV1[:], kvalid_d[:, :], writes=[KV1])
            S.op("dve", lambda e: e.memset(ONE1[:], 1.0), writes=[ONE1])
            for hg in range(4):
                wk = cpan.next()
                S.dma("pool", wk[:], kv_w[:, hg * 256:(hg + 1) * 256].rearrange("(k p) m -> p k m", p=128), writes=[wk])
                wv = cpan.next()
                S.dma("pool", wv[:], kv_w[:, 1024 + hg * 256:1024 + (hg + 1) * 256].rearrange("(k p) m -> p k m", p=128), writes=[wv])
                wq = cpan.next()
                S.dma("pool", wq[:], w_q[:, hg * 256:(hg + 1) * 256].rearrange("(k p) m -> p k m", p=128), writes=[wq])
                S.dma("pool", wob[:], w_ob[hg * 256:(hg + 1) * 256, :].rearrange("(f p) m -> p f m", p=128), writes=[wob])
                S.dma("pool", BM[:], biasT[hg * 4:(hg + 1) * 4, :, :].rearrange("a q k -> q a k"), writes=[BM])
                S.op("dve", lambda e: e.tensor_tensor(out=BM[:, :, :], in0=BM[:, :, :], in1=WM[:, :].unsqueeze(1).to_broadcast([128, 4, 640]), op=ALU.add),
                     reads=[BM, WM], writes=[BM])
                for t in range(5):
                    for pi in range(2):
                        ps = psO.next()
                        S.group("pe", [lambda e, k=k, ps=ps, pi=pi, t=t: e.matmul(ps[:, 0:512], lhsT=wk[:, k, pi * 128:(pi + 1) * 128], rhs=XB[:, k, tokc(t)],
                                                                                start=(k == 0), stop=(k == 7)) for k in range(8)],
                                reads=[wk] + [XBv[k][t] for k in range(8)], writes=[ps])
                        S.op("act", lambda e, ps=ps, pi=pi, t=t: e.activation(out=KT2[:, pi, tokc(t)], in_=ps[:, 0:512], func=AF.Copy), reads=[ps], writes=[KT2])
                        if t >= 1:
                            ps = psO.next()
                            S.group("pe", [lambda e, k=k, ps=ps, pi=pi, t=t: e.matmul(ps[:, 0:512], lhsT=wq[:, k, pi * 128:(pi + 1) * 128], rhs=XB[:, k, tokc(t)],
                                                                                    start=(k == 0), stop=(k == 7)) for k in range(8)],
                                    reads=[wq] + [XBv[k][t] for k in range(8)], writes=[ps])
                            S.op("act", lambda e, ps=ps, pi=pi, t=t: e.activation(out=QT2[:, pi, tokc(t - 1)], in_=ps[:, 0:512], func=AF.Copy, scale=0.125),
                                 reads=[ps], writes=[QT2])
                    for q4 in range(4):
                        kt = t * 4 + q4
                        ps = psO.next()
                        S.group("pe", [lambda e, k=k, ps=ps, kt=kt: e.matmul(ps[:, 0:256], lhsT=XB[:, k, kt * 128:(kt + 1) * 128], rhs=wv[:, k, :],
                                                                            start=(k == 0), stop=(k == 7)) for k in range(8)],
                                reads=[wv] + [XBv[k][t] for k in range(8)], writes=[ps])
                        S.op("dve", lambda e, ps=ps, kt=kt: e.tensor_copy(out=VT[:, kt, :], in_=ps[:, 0:256]), reads=[ps], writes=[VT])
                chains = [(T, a) for T in range(16) for a in range(4)]
                cst_ = {}
                otms = {}

                def s1(T, a):
                    pi = a // 2
                    pr = slice((a % 2) * 64, (a % 2) * 64 + 64)
                    ps2 = psS.next()
                    qs = slice(T * 128, (T + 1) * 128)
                    fns = []
                    for (c0, c1) in ((0, 512), (512, 640)):
                        ks = slice(T * 128 + c0, T * 128 + c1)
                        fns.append(lambda e, c0=c0, c1=c1, ks=ks: e.matmul(ps2[:, c0:c1], lhsT=QT2[pr, pi, qs], rhs=KT2[pr, pi, ks], start=True, stop=False))
                        if T < 4:
                            fns.append(lambda e, c0=c0, c1=c1, ks=ks: e.matmul(ps2[:, c0:c1], lhsT=ONE1[0:1, :], rhs=KV1[0:1, ks], start=False, stop=False))
                        fns.append(lambda e, c0=c0, c1=c1: e.matmul(ps2[:, c0:c1], lhsT=IDB, rhs=BM[:, a, c0:c1], start=False, stop=True))
                    S.group("pe", fns, reads=[QT2, KT2, ONE1, KV1, BM, CST], writes=[ps2])
                    stt_ = st4.next()
                    S.op("dve", lambda e: e.tensor_reduce(out=stt_[:, 1:2], in_=ps2[:, 0:640], axis=AX.X, op=ALU.max, negate=True), reads=[ps2], writes=[stt_])
                    pe_ = pexp.next()
                    S.op("act", lambda e: e.activation(out=pe_[:, :], in_=ps2[:, 0:640], func=AF.Exp, bias=stt_[:, 1:2], scale=1.0, accum_out=stt_[:, 2:3]),
                         reads=[ps2, stt_], writes=[pe_, stt_])
                    S.op("dve", lambda e: e.reciprocal(out=stt_[:, 3:4], in_=stt_[:, 2:3]), reads=[stt_], writes=[stt_])
                    cst_[(T, a)] = [pe_, stt_, None]

                def s2(T, a):
                    pe_, stt_, _ = cst_[(T, a)]
                    S.group("pe", [lambda e, kt=kt: e.transpose(out=psTv[:, kt * 128:(kt + 1) * 128], in_=pe_[:, kt * 128:(kt + 1) * 128], identity=IDB)
                                   for kt in range(5)], reads=[pe_, CST], writes=[psTv])
                    pt = PTs.next()
                    if (T * 4 + a) % 2 == 0:
                        S.op("dve", lambda e: e.tensor_copy(out=pt[:, :, :].rearrange("p k q -> p (k q)"), in_=psTv[:, :]), reads=[psTv], writes=[pt])
                    else:
                        S.op("act", lambda e: e.activation(out=pt[:, :, :].rearrange("p k q -> p (k q)"), in_=psTv[:, :], func=AF.Copy), reads=[psTv], writes=[pt])
                    cst_[(T, a)][2] = pt

                def s3(T, a):
                    pe_, stt_, pt = cst_.pop((T, a))
                    if a == 0:
                        otms[T] = OTM.next()
                    otm = otms[T]
                    pso = psO1
                    S.group("pe", [lambda e, kt=kt: e.matmul(pso[:, 0:64], lhsT=pt[:, kt, :], rhs=VT[:, T + kt, a * 64:(a + 1) * 64], start=(kt == 0), stop=(kt == 4))
                                   for kt in range(5)], reads=[pt, VT], writes=[pso])
                    S.op("act", lambda e: e.activation(out=otm[:, a * 64:(a + 1) * 64], in_=pso[:, 0:64], func=AF.Copy, scale=stt_[:, 3:4]),
                         reads=[pso, stt_], writes=[otm])
                    if a == 3:
                        S.group("pe", [lambda e, j=j: e.transpose(out=psTv[:, j * 128:(j + 1) * 128], in_=otm[:, j * 128:(j + 1) * 128], identity=IDB)
                                       for j in range(2)], reads=[otm, CST], writes=[psTv])
                        S.op("act", lambda e: e.activation(out=OTF[:, :, T * 128:(T + 1) * 128], in_=psTv[:, 0:256].rearrange("p (j q) -> p j q", j=2), func=AF.Copy),
                             reads=[psTv], writes=[OTF])

                s1(*chains[0])
                s1(*chains[1])
                for ci in range(len(chains)):
                    s2(*chains[ci])
                    if ci + 2 < len(chains):
                        s1(*chains[ci + 2])
                    s3(*chains[ci])
                for t in range(1, 5):
                    for m in range(8):
                        ps = psO.next()
                        S.group("pe", [lambda e, j=j, ps=ps, m=m, t=t: e.matmul(ps[:, 0:512], lhsT=wob[:, j, m * 128:(m + 1) * 128], rhs=OTF[:, j, tokc(t - 1)],
                                                                               start=(j == 0), stop=(j == 1)) for j in range(2)], reads=[wob, OTF], writes=[ps])
                        S.op("dve", lambda e, ps=ps, m=m, t=t: e.tensor_tensor(out=XF[:, m, tokc(t)], in0=XF[:, m, tokc(t)], in1=ps[:, 0:512], op=ALU.add),
                             reads=[ps, XFv[m][t]], writes=[XFv[m][t]])
            S.barrier()
        S.stack = top

        with contextlib.ExitStack() as pd:
            S.stack = pd
            psA = Rot([S.psum("dpsA%d" % i, [128, 512], F32) for i in range(4)])
            psB = Rot([S.psum("dpsB%d" % i, [128, 512], F32) for i in range(4)])
            scr = Rot([S.sbuf("dscr%d" % i, [128, 512], F32) for i in range(4)])
            lnsq = scr
            lnst = Rot([S.sbuf("dlnst%d" % i, [128, 512], F32) for i in range(3)])
            for t in range(1, 5):
                layer_norm(2, t, xb_t=XB, xbv=XBv)
            if stage == 11:
                S.barrier()
                S.stack = top
                finish_dbg()
                return nc
            uppan = Rot([S.sbuf("duppan%d" % i, [128, 8, 512], BF16) for i in range(2)])
            dnpan = Rot([S.sbuf("ddnpan%d" % i, [128, 4, 1024], BF16) for i in range(2)])
            sgp = Rot([S.sbuf("dsg%d" % i, [128, 512], F32) for i in range(2)])
            ACT_ = S.sbuf("dact", [128, 4, 2048], BF16)
            ACTv = [[S.view("dact%d_%d" % (j, t), None) for t in range(4)] for j in range(4)]
            GB = S.sbuf("GB", [128, 2048], F32)
            GBv = [S.view("GB%d" % t, None) for t in range(4)]
            GTM = S.sbuf("GTM", [128, 16, 8], F32)
            WR = S.sbuf("WR", [128, 8, 8], F32)
            rt = Rot([S.sbuf("drt%d" % i, [128, 8], F32) for i in range(6)])
            rs = Rot([S.sbuf("drs%d" % i, [128, 8], F32) for i in range(4)])
            Dg = Rot([S.sbuf("dDg%d" % i, [128, 128], F32) for i in range(2)])
            S.dma("sp", WR[:], router_d.rearrange("(k p) e -> p k e", p=128), writes=[WR])
            for i in range(16):
                t, q4 = 1 + i // 4, i % 4
                ts_ = slice(t * 512 + q4 * 128, t * 512 + (q4 + 1) * 128)
                ps = psA.next()
                S.group("pe", [lambda e, k=k, ps=ps, ts_=ts_: e.matmul(ps[:, 0:8], lhsT=XF[:, k, ts_], rhs=WR[:, k, :], start=(k == 0), stop=(k == 7))
                               for k in range(8)], reads=[WR] + [XFv[k][t] for k in range(8)], writes=[ps])
                lg = rt.next()
                S.op("act", lambda e, lg=lg, ps=ps: e.activation(out=lg[:, :], in_=ps[:, 0:8], func=AF.Copy), reads=[ps], writes=[lg])
                sc_ = rs.next()
                S.op("dve", lambda e, lg=lg, sc_=sc_: e.tensor_reduce(out=sc_[:, 0:1], in_=lg[:, :], axis=AX.X, op=ALU.max), reads=[lg], writes=[sc_])
                eq1 = rt.next()
                S.op("dve", lambda e, lg=lg, sc_=sc_, eq1=eq1: e.tensor_scalar(out=eq1[:, :], in0=lg[:, :], scalar1=sc_[:, 0:1], scalar2=None, op0=ALU.is_equal),
                     reads=[lg, sc_], writes=[eq1])
                l2 = rt.next()
                S.op("dve", lambda e, lg=lg, eq1=eq1, l2=l2: e.scalar_tensor_tensor(out=l2[:, :], in0=eq1[:, :], scalar=-1e30, in1=lg[:, :], op0=ALU.mult, op1=ALU.add),
                     reads=[lg, eq1], writes=[l2])
                S.op("dve", lambda e, l2=l2, sc_=sc_: e.tensor_reduce(out=sc_[:, 1:2], in_=l2[:, :], axis=AX.X, op=ALU.max), reads=[l2, sc_], writes=[sc_])
                eq2 = rt.next()
                S.op("dve", lambda e, l2=l2, sc_=sc_, eq2=eq2: e.tensor_scalar(out=eq2[:, :], in0=l2[:, :], scalar1=sc_[:, 1:2], scalar2=None, op0=ALU.is_equal),
                     reads=[l2, sc_], writes=[eq2])
                S.op("dve", lambda e, sc_=sc_: e.tensor_tensor(out=sc_[:, 2:3], in0=sc_[:, 1:2], in1=sc_[:, 0:1], op=ALU.subtract), reads=[sc_], writes=[sc_])
                S.op("act", lambda e, sc_=sc_: e.activation(out=sc_[:, 2:3], in_=sc_[:, 2:3], func=AF.Exp), reads=[sc_], writes=[sc_])
                S.op("dve", lambda e, sc_=sc_: e.tensor_scalar(out=sc_[:, 3:4], in0=sc_[:, 2:3], scalar1=1.0, scalar2=None, op0=ALU.add), reads=[sc_], writes=[sc_])
                S.op("dve", lambda e, sc_=sc_: e.reciprocal(out=sc_[:, 3:4], in_=sc_[:, 3:4]), reads=[sc_], writes=[sc_])
                S.op("dve", lambda e, sc_=sc_: e.tensor_tensor(out=sc_[:, 4:5], in0=sc_[:, 2:3], in1=sc_[:, 3:4], op=ALU.mult), reads=[sc_], writes=[sc_])
                S.op("dve", lambda e, eq1=eq1, sc_=sc_: e.tensor_scalar(out=eq1[:, :], in0=eq1[:, :], scalar1=sc_[:, 3:4], scalar2=None, op0=ALU.mult),
                     reads=[eq1, sc_], writes=[eq1])
                S.op("dve", lambda e, eq1=eq1, eq2=eq2, sc_=sc_, i=i: e.scalar_tensor_tensor(out=GTM[:, i, :], in0=eq2[:, :], scalar=sc_[:, 4:5], in1=eq1[:, :],
                                                                                           op0=ALU.mult, op1=ALU.add), reads=[eq1, eq2, sc_], writes=[GTM])
            scale_xf(range(1, 5))
            GB2 = S.sbuf("GB2", [128, 2048], F32)
            GBs = [GB, GB2]
            GBvs = [GBv, [S.view("GB2_%d" % t, None) for t in range(4)]]

            def make_gb(ex):
                gb, gbv = GBs[ex % 2], GBvs[ex % 2]
                for t4 in range(4):
                    ps = psA.next()
                    for q4 in range(4):
                        i = t4 * 4 + q4
                        dg = Dg.next()
                        S.op("dve", lambda e, dg=dg, i=i: e.tensor_scalar(out=dg[:, :], in0=IDF, scalar1=GTM[:, i, ex:ex + 1], scalar2=None, op0=ALU.mult),
                             reads=[GTM, CST], writes=[dg])
                        S.op("pe", lambda e, dg=dg, ps=ps, q4=q4: e.matmul(ps[:, q4 * 128:(q4 + 1) * 128], lhsT=ONEF, rhs=dg[:, :], start=True, stop=True),
                             reads=[dg, CST, ps], writes=[ps])
                    S.op("act", lambda e, ps=ps, t4=t4: e.activation(out=gb[:, t4 * 512:(t4 + 1) * 512], in_=ps[:, :], func=AF.Copy), reads=[ps], writes=[gbv[t4]])

            make_gb(0)
            for ex in range(8):
                hook = (lambda ex=ex: make_gb(ex + 1)) if ex < 7 else None
                expert(moe_up[ex], moe_dn[ex], 3584, [1, 2, 3, 4], gate=(GBs[ex % 2], GBvs[ex % 2]), hook=hook)
            for t in range(1, 5):
                layer_norm(3, t)
            S.barrier()
            xo = Rot([S.view("dxo%d" % i, GB[:, i * 1024:(i + 1) * 1024]) for i in range(2)])
            OUTB = S.view("OUTB", None)
            for i in range(16):
                t, q4 = 1 + i // 4, i % 4
                ts_ = slice(t * 512 + q4 * 128, t * 512 + (q4 + 1) * 128)
                xb_ = xo.next()
                for half in range(2):
                    ps = psA.next()
                    S.group("pe", [lambda e, j=j, ps=ps, half=half, ts_=ts_: e.transpose(out=ps[:, j * 128:(j + 1) * 128], in_=XF[:, half * 4 + j, ts_], identity=IDF)
                                   for j in range(4)], reads=[CST] + [XFv[k][t] for k in range(half * 4, half * 4 + 4)], writes=[ps])
                    if half == 0:
                        S.op("act", lambda e, ps=ps, xb_=xb_: e.activation(out=xb_[:, 0:512], in_=ps[:, :], func=AF.Copy), reads=[ps], writes=[xb_])
                    else:
                        S.op("dve", lambda e, ps=ps, xb_=xb_: e.tensor_copy(out=xb_[:, 512:1024], in_=ps[:, :]), reads=[ps], writes=[xb_])
                S.dma("sp", out_d[i * 128:(i + 1) * 128, :], xb_[:, :], reads=[xb_], owner=xb_)
            S.barrier()
        S.stack = top
        S.barrier()
    return nc


def _consts():
    i = np.arange(128)
    same = (i[:, None] // 64) == (i[None, :] // 64)
    ident = np.eye(128, dtype=np.float32)
    tri = (same & (i[:, None] <= i[None, :])).astype(np.float32)
    blk = same.astype(np.float32)
    mbl = np.where(same & (i[None, :] < i[:, None]), 0.0, -1e5).astype(np.float32)
    mbu = np.where(same & (i[None, :] >= i[:, None]), 0.0, -1e5).astype(np.float32)
    ones = np.ones((128, 128), np.float32)
    return np.ascontiguousarray(np.stack([ident, tri, blk, mbl, mbu, ones], axis=1))


def make_in_maps(inputs):
    f = lambda a: np.ascontiguousarray(np.asarray(a, dtype=np.float32))
    x = f(inputs["x"])
    q = np.arange(128)[:, None]
    k = np.arange(640)[None, :]
    idx = np.clip(512 + q - k, -128, 128) + 128
    biasT = f(np.asarray(inputs["b_rel_bias"])[0][:, idx])
    cq = q // 64
    wmask = np.where((k >= 64 * cq) & (k < 64 * cq + 576), 0.0, NEG).astype(np.float32)
    lng = np.stack([np.asarray(inputs[n])[l].reshape(8, 128).T for (n, l) in
                    (("ln1_g", 0), ("ln2_g", 0), ("ln1_g", 1), ("ln2_g", 1))], axis=1)
    lnb = np.stack([np.asarray(inputs[n])[l].reshape(8, 128).T for (n, l) in
                    (("ln1_b", 0), ("ln2_b", 0), ("ln1_b", 1), ("ln2_b", 1))], axis=1)
    shared = {
        "w_in": f(inputs["a_w_in"][0]),
        "cw": f(np.asarray(inputs["a_conv_w"])[0].reshape(4, 24, 128).transpose(2, 1, 0)),
        "alog": f(np.broadcast_to(np.asarray(inputs["a_A_log"])[0][None, :], (128, 8))),
        "dtb": f(np.broadcast_to(np.asarray(inputs["a_dt_bias"])[0][None, :], (128, 8))),
        "normg": f(np.asarray(inputs["a_norm_g"])[0].reshape(128, 1)),
        "w_oa": f(inputs["a_w_o"][0]),
        "kv_w": f(inputs["kv_w"]),
        "w_q": f(inputs["b_w_q"][0]),
        "biasT": biasT,
        "wmask": wmask,
        "w_ob": f(inputs["b_w_o"][0]),
        "ffn_up": f(inputs["ffn_w_up"][0]),
        "ffn_dn": f(inputs["ffn_w_down"][0]),
        "router": f(inputs["moe_router"][0]),
        "moe_up": f(inputs["moe_w_up"][0]),
        "moe_dn": f(inputs["moe_w_down"][0]),
        "lng": f(lng),
        "lnb": f(lnb),
        "cmat": _consts(),
    }
    maps = []
    for c in range(8):
        b, half = c // 2, c % 2
        if half == 0:
            xw = np.concatenate([np.zeros((2048, 1024), np.float32), x[b, 0:2048]], axis=0)
            kvalid = np.concatenate([np.full((1, 512), NEG, np.float32), np.zeros((1, 2048), np.float32)], axis=1)
        else:
            xw = x[b]
            kvalid = np.zeros((1, 2560), np.float32)
        m = dict(shared)
        m["x_win"] = np.ascontiguousarray(xw)
        m["kvalid"] = kvalid
        maps.append(m)
    return maps


def kernel(**inputs):
    nc = build()
    maps = make_in_maps(inputs)
    res = run_bass_kernel_spmd(nc, maps, core_ids=list(range(8)))
    out = np.zeros((4, 4096, 1024), np.float32)
    for c in range(8):
        b, half = c // 2, c % 2
        out[b, half * 2048:(half + 1) * 2048] = res.results[c]["out"]
    return out
```
